# Optimizing a Trainium2 kernel written in Bass

```python
import jax, jax.numpy as jnp
from jax import lax
import numpy as np

D_MODEL = 1024
BATCH = 32
SEQ = 2048
DEPTH = 1

D_MIX = 2 * D_MODEL
GM_WIDTH = D_MIX // 2
GM_HEADS = 8
GM_HEAD_DIM = GM_WIDTH // GM_HEADS
GM_CHUNK = 128
SSD_WIDTH = D_MIX - GM_WIDTH
SSD_HEAD_DIM = 64
SSD_HEADS = SSD_WIDTH // SSD_HEAD_DIM
SSD_GROUPS = 2
SSD_HPG = SSD_HEADS // SSD_GROUPS
SSD_STATE = 128
SSD_CONV = 4
SSD_CHUNK = 128
SSD_CONV_DIM = SSD_WIDTH + 2 * SSD_GROUPS * SSD_STATE
D_IN_PROJ = 2 * GM_WIDTH + SSD_WIDTH + SSD_CONV_DIM + SSD_HEADS
PEER_HEADS = 8
PEER_NKEYS = 128
PEER_EXPERTS = PEER_NKEYS * PEER_NKEYS
PEER_DKEY = 256
PEER_TOPK = 16
PEER_TOKEN_BLOCK = 128
EPS = 1e-6

kernel_name = "hybrid_gmlp_ssd_peer_block"


def rmsnorm(x, g):
    xf = x.astype(jnp.float32)
    y = xf * lax.rsqrt(jnp.mean(xf * xf, axis=-1, keepdims=True) + EPS)
    return (y * g.astype(jnp.float32)).astype(x.dtype)


def layernorm(x, g, b):
    xf = x.astype(jnp.float32)
    mu = jnp.mean(xf, axis=-1, keepdims=True)
    var = jnp.mean(jnp.square(xf - mu), axis=-1, keepdims=True)
    y = (xf - mu) * lax.rsqrt(var + EPS)
    return (y * g.astype(jnp.float32) + b.astype(jnp.float32)).astype(x.dtype)


def chunked_spatial_gating(u_raw, v_raw, ln_g, ln_b, ws, bs):
    b_, L, _ = u_raw.shape
    nc = L // GM_CHUNK
    u = jax.nn.gelu(u_raw, approximate=False)
    v = layernorm(jax.nn.gelu(v_raw, approximate=False), ln_g, ln_b)
    v = v.reshape(b_, nc, GM_CHUNK, GM_HEADS, GM_HEAD_DIM)
    causal = jnp.tril(jnp.ones((GM_CHUNK, GM_CHUNK), dtype=bool))
    w = jnp.where(causal[None], ws, jnp.zeros_like(ws))
    s = jnp.einsum('hts,bcshd->bcthd', w, v) + bs.T[None, None, :, :, None]
    return u * s.reshape(b_, L, GM_WIDTH)


def causal_depthwise_conv(x, w, b):
    C = x.shape[-1]
    y = lax.conv_general_dilated(
        x, w[:, None, :].astype(x.dtype), window_strides=(1,),
        padding=[(SSD_CONV - 1, 0)],
        dimension_numbers=('NWC', 'WIO', 'NWC'),
        feature_group_count=C)
    return y + b.astype(x.dtype)


def ssd_chunked(xs, dt, A, Bm, Cm):
    b_, L = xs.shape[0], xs.shape[1]
    nc = L // SSD_CHUNK
    Q = SSD_CHUNK
    xc = xs.reshape(b_, nc, Q, SSD_GROUPS, SSD_HPG, SSD_HEAD_DIM)
    dtc = dt.reshape(b_, nc, Q, SSD_GROUPS, SSD_HPG)
    Bc = Bm.reshape(b_, nc, Q, SSD_GROUPS, SSD_STATE)
    Cc = Cm.reshape(b_, nc, Q, SSD_GROUPS, SSD_STATE)
    a_cs = jnp.cumsum(dtc * A, axis=2)
    xdt = xc * dtc[..., None]
    acs_t = jnp.moveaxis(a_cs, 2, -1)
    diff = acs_t[..., :, None] - acs_t[..., None, :]
    causal = jnp.tril(jnp.ones((Q, Q), dtype=bool))
    Lmat = jnp.exp(jnp.where(causal, diff, -jnp.inf))
    cb = jnp.einsum('bctgn,bcsgn->bcgts', Cc, Bc)
    y_diag = jnp.einsum('bcgkts,bcsgkp->bctgkp', cb[:, :, :, None] * Lmat, xdt)
    decay_states = jnp.exp(a_cs[:, :, -1:] - a_cs)
    states = jnp.einsum('bcsgn,bcsgkp->bcgkpn', Bc, xdt * decay_states[..., None])
    chunk_decay = jnp.exp(a_cs[:, :, -1])

    def step(h, inp):
        st, dec = inp
        return dec[..., None, None] * h + st, h

    h0 = jnp.zeros((b_, SSD_GROUPS, SSD_HPG, SSD_HEAD_DIM, SSD_STATE), jnp.float32)
    _, prev = lax.scan(step, h0, (jnp.moveaxis(states, 1, 0), jnp.moveaxis(chunk_decay, 1, 0)))
    prev = jnp.moveaxis(prev, 0, 1)
    y_off = jnp.einsum('bctgn,bcgkpn->bctgkp', Cc, prev) * jnp.exp(a_cs)[..., None]
    return (y_diag + y_off).reshape(b_, L, SSD_GROUPS, SSD_HPG, SSD_HEAD_DIM)


def peer(h, w_query, sub_keys1, sub_keys2, expert_u, expert_v):
    b_, L, D = h.shape
    T = b_ * L
    hf = h.reshape(T, D)
    q = (hf @ w_query).astype(jnp.float32).reshape(T, PEER_HEADS, 2, PEER_DKEY // 2)
    s1 = jnp.einsum('thd,nd->thn', q[:, :, 0], sub_keys1.astype(jnp.float32))
    s2 = jnp.einsum('thd,nd->thn', q[:, :, 1], sub_keys2.astype(jnp.float32))
    v1, i1 = lax.top_k(s1, PEER_TOPK)
    v2, i2 = lax.top_k(s2, PEER_TOPK)
    cand = (v1[..., :, None] + v2[..., None, :]).reshape(T, PEER_HEADS, PEER_TOPK * PEER_TOPK)
    cv, ci = lax.top_k(cand, PEER_TOPK)
    e1 = jnp.take_along_axis(i1, ci // PEER_TOPK, axis=-1)
    e2 = jnp.take_along_axis(i2, ci % PEER_TOPK, axis=-1)
    idx = e1 * PEER_NKEYS + e2
    gate = jax.nn.softmax(cv, axis=-1)
    nb = T // PEER_TOKEN_BLOCK
    HK = PEER_HEADS * PEER_TOPK
    hb = hf.reshape(nb, PEER_TOKEN_BLOCK, D)
    ib = idx.reshape(nb, PEER_TOKEN_BLOCK, HK)
    gb = gate.reshape(nb, PEER_TOKEN_BLOCK, HK).astype(h.dtype)

    def block(args):
        hx, ix, gx = args
        u = expert_u[ix]
        a = jax.nn.gelu(jnp.einsum('tkd,td->tk', u, hx), approximate=False) * gx
        return jnp.einsum('tk,tkd->td', a, expert_v[ix])

    out = lax.map(block, (hb, ib, gb))
    return out.reshape(b_, L, D)


def setup_inputs(seed: int = 0) -> dict:
    key = jax.random.key(seed)
    ks = jax.random.split(key, 24)
    f32 = jnp.float32
    nrm = lambda k, shape, s: jax.random.normal(k, shape, f32) * s
    gain = lambda k, shape: 1.0 + 0.1 * jax.random.normal(k, shape, f32)
    dt0 = jnp.exp(jax.random.uniform(ks[10], (DEPTH, SSD_HEADS), f32,
                                     np.float32(np.log(1e-3)), np.float32(np.log(1e-1))))
    dt_bias = dt0 + jnp.log(-jnp.expm1(-dt0))
    return {
        "x": nrm(ks[0], (BATCH, SEQ, D_MODEL), 1.0),
        "norm1_g": gain(ks[1], (DEPTH, D_MODEL)),
        "w_in": nrm(ks[2], (DEPTH, D_MODEL, D_IN_PROJ), D_MODEL ** -0.5),
        "gmlp_ln_g": gain(ks[3], (DEPTH, GM_WIDTH)),
        "gmlp_ln_b": nrm(ks[4], (DEPTH, GM_WIDTH), 0.02),
        "gmlp_ws": nrm(ks[5], (DEPTH, GM_HEADS, GM_CHUNK, GM_CHUNK), GM_CHUNK ** -0.5),
        "gmlp_bs": gain(ks[6], (DEPTH, GM_HEADS, GM_CHUNK)),
        "gmlp_out_g": gain(ks[7], (DEPTH, GM_WIDTH)),
        "conv_w": nrm(ks[8], (DEPTH, SSD_CONV, SSD_CONV_DIM), SSD_CONV ** -0.5),
        "conv_b": nrm(ks[9], (DEPTH, SSD_CONV_DIM), 0.02),
        "dt_bias": dt_bias,
        "a_log": jnp.log(jax.random.uniform(ks[11], (DEPTH, SSD_HEADS), f32, 1.0, 16.0)),
        "d_skip": gain(ks[12], (DEPTH, SSD_HEADS)),
        "ssd_norm_g": gain(ks[13], (DEPTH, SSD_WIDTH)),
        "w_out": nrm(ks[14], (DEPTH, D_MIX, D_MODEL), D_MIX ** -0.5),
        "norm2_g": gain(ks[15], (DEPTH, D_MODEL)),
        "w_query": nrm(ks[16], (DEPTH, D_MODEL, PEER_HEADS * PEER_DKEY), D_MODEL ** -0.5),
        "sub_keys1": nrm(ks[17], (DEPTH, PEER_NKEYS, PEER_DKEY // 2), (PEER_DKEY // 2) ** -0.5),
        "sub_keys2": nrm(ks[18], (DEPTH, PEER_NKEYS, PEER_DKEY // 2), (PEER_DKEY // 2) ** -0.5),
        "expert_u": nrm(ks[19], (DEPTH, PEER_EXPERTS, D_MODEL), D_MODEL ** -0.5),
        "expert_v": nrm(ks[20], (DEPTH, PEER_EXPERTS, D_MODEL), PEER_HEADS ** -0.5),
        "final_g": gain(ks[21], (D_MODEL,)),
    }


def reference(x, norm1_g, w_in, gmlp_ln_g, gmlp_ln_b, gmlp_ws, gmlp_bs, gmlp_out_g,
              conv_w, conv_b, dt_bias, a_log, d_skip, ssd_norm_g, w_out, norm2_g,
              w_query, sub_keys1, sub_keys2, expert_u, expert_v, final_g):
    b_, L, _ = x.shape
    splits = [GM_WIDTH, 2 * GM_WIDTH, 2 * GM_WIDTH + SSD_WIDTH,
              2 * GM_WIDTH + SSD_WIDTH + SSD_CONV_DIM]
    for l in range(DEPTH):
        h = rmsnorm(x, norm1_g[l])
        proj = h @ w_in[l]
        u_raw, v_raw, z, xbc, dt_raw = jnp.split(proj, splits, axis=-1)
        gm = chunked_spatial_gating(u_raw, v_raw, gmlp_ln_g[l], gmlp_ln_b[l],
                                    gmlp_ws[l], gmlp_bs[l])
        gm = rmsnorm(gm, gmlp_out_g[l])
        xbc = jax.nn.silu(causal_depthwise_conv(xbc, conv_w[l], conv_b[l])).astype(jnp.float32)
        xs, Bm, Cm = jnp.split(xbc, [SSD_WIDTH, SSD_WIDTH + SSD_GROUPS * SSD_STATE], axis=-1)
        xs = xs.reshape(b_, L, SSD_GROUPS, SSD_HPG, SSD_HEAD_DIM)
        Bm = Bm.reshape(b_, L, SSD_GROUPS, SSD_STATE)
        Cm = Cm.reshape(b_, L, SSD_GROUPS, SSD_STATE)
        dt = jax.nn.softplus(dt_raw.astype(jnp.float32) + dt_bias[l].astype(jnp.float32))
        dt = dt.reshape(b_, L, SSD_GROUPS, SSD_HPG)
        A = -jnp.exp(a_log[l].astype(jnp.float32)).reshape(SSD_GROUPS, SSD_HPG)
        y = ssd_chunked(xs, dt, A, Bm, Cm)
        y = y + d_skip[l].astype(jnp.float32).reshape(SSD_GROUPS, SSD_HPG)[..., None] * xs
        yg = (y.reshape(b_, L, SSD_WIDTH) * jax.nn.silu(z.astype(jnp.float32)))
        yg = yg.reshape(b_, L, SSD_GROUPS, SSD_WIDTH // SSD_GROUPS)
        yg = yg * lax.rsqrt(jnp.mean(yg * yg, axis=-1, keepdims=True) + EPS)
        ssd_out = (yg.reshape(b_, L, SSD_WIDTH) * ssd_norm_g[l].astype(jnp.float32)).astype(x.dtype)
        mix = jnp.concatenate([gm.astype(x.dtype), ssd_out], axis=-1) @ w_out[l]
        x = x + mix.astype(x.dtype)
        h2 = rmsnorm(x, norm2_g[l])
        x = x + peer(h2, w_query[l], sub_keys1[l], sub_keys2[l],
                     expert_u[l], expert_v[l]).astype(x.dtype)
    return rmsnorm(x, final_g)
```

```python
import contextlib
import numpy as np
import ml_dtypes
import concourse.bass as bass
import concourse.mybir as mybir
from concourse.bass_utils import run_bass_kernel_spmd

F32 = mybir.dt.float32
BF16 = mybir.dt.bfloat16
U32 = mybir.dt.uint32
ALU = mybir.AluOpType
AF = mybir.ActivationFunctionType
AX = mybir.AxisListType

D = 1024
GMW = 1024
SSDW = 1024
NH = 16
HP = 64
NST = 128
CONVD = 1536
DIN = 4624
NEXP = 16384
EPS = 1e-6
NCORES = 8


class Tok:
    __slots__ = ("last_write", "reads", "name")

    def __init__(self, name=""):
        self.last_write = None
        self.reads = []
        self.name = name


class Op:
    __slots__ = ("eng", "fn", "deps", "signal", "seq", "dsem", "val")

    def __init__(self, eng, fn):
        self.eng = eng
        self.fn = fn
        self.deps = []
        self.signal = False
        self.seq = 0
        self.dsem = None
        self.val = 0


class DSem:
    def __init__(self, handle, group=False):
        self.handle = handle
        self.count = 0
        self.group = group


class Prog:
    ENGS = ("pe", "act", "dve", "pool", "sp")

    def __init__(self, nc, stack):
        self.nc = nc
        self.stack = stack
        self.ops = {e: [] for e in self.ENGS}
        self.esem = {e: stack.enter_context(nc.semaphore("es_" + e)) for e in self.ENGS}
        self.nsem = 0
        self.dmas = []

    def dsem(self, name=None, group=False):
        self.nsem += 1
        return DSem(self.stack.enter_context(self.nc.semaphore(name or ("ds%d" % self.nsem))), group)

    def op(self, eng, fn, reads=(), writes=(), dsem=None):
        if dsem is not None and dsem.group:
            dsem = self.dsem()
        o = Op(eng, fn)
        deps = []
        for t in reads:
            if t.last_write is not None:
                deps.append(t.last_write)
        for t in writes:
            if t.last_write is not None:
                deps.append(t.last_write)
            deps.extend(t.reads)
        o.deps = deps
        for d in deps:
            d.signal = True
        for t in reads:
            t.reads = [r for r in t.reads if not (r.eng == eng and r.dsem is None and dsem is None)]
            t.reads.append(o)
        for t in writes:
            t.last_write = o
            t.reads = []
        if dsem is not None:
            o.dsem = dsem
            dsem.count += 16
            o.val = dsem.count
        self.ops[eng].append(o)
        if dsem is not None:
            self.dmas.append(o)
        return o

    def barrier(self):
        lasts = [self.ops[e][-1] for e in self.ENGS if self.ops[e] and self.ops[e][-1].dsem is None]
        for e in self.ENGS:
            prev = [o for o in reversed(self.ops[e]) if o.dsem is None][:1]
            lasts_e = prev
        lasts = []
        for e in self.ENGS:
            for o in reversed(self.ops[e]):
                if o.dsem is None:
                    lasts.append(o)
                    break
        deps = lasts + list(self.dmas)
        self.dmas = []
        for e in self.ENGS:
            o = Op(e, lambda eng: eng.nop())
            o.deps = list(deps)
            for d in deps:
                d.signal = True
            self.ops[e].append(o)

    def emit(self, final_waits=()):
        nc = self.nc
        for e in self.ENGS:
            n = 0
            for o in self.ops[e]:
                if o.dsem is None and o.signal:
                    n += 1
                    o.seq = n
        esem = self.esem

        def run(ename, eng):
            waited = {}
            for o in self.ops[ename]:
                need = {}
                for d in o.deps:
                    if d.dsem is not None:
                        key = ("d", id(d.dsem))
                        h, v = d.dsem.handle, d.val
                    else:
                        if d.eng == ename and ename == "pe":
                            continue
                        key = ("e", d.eng)
                        h, v = esem[d.eng], d.seq
                    if waited.get(key, 0) >= v:
                        continue
                    if key not in need or need[key][1] < v:
                        need[key] = (h, v)
                for key, (h, v) in need.items():
                    eng.wait_ge(h, v)
                    waited[key] = v
                ins = o.fn(eng)
                if o.dsem is not None:
                    ins.then_inc(o.dsem.handle, 16)
                elif o.signal:
                    ins.then_inc(esem[ename], 1)
            if ename == "sp":
                for o in final_waits:
                    if o.dsem is not None:
                        eng.wait_ge(o.dsem.handle, o.val)
                    else:
                        eng.wait_ge(esem[o.eng], o.seq)

        for o in final_waits:
            o.signal = True
        for e in self.ENGS:
            n = 0
            for o in self.ops[e]:
                if o.dsem is None and o.signal:
                    n += 1
                    o.seq = n
        with nc.Block() as block:
            @block.sync
            def _(eng):
                run("sp", eng)

            @block.scalar
            def _(eng):
                run("act", eng)

            @block.vector
            def _(eng):
                run("dve", eng)

            @block.gpsimd
            def _(eng):
                run("pool", eng)

            @block.tensor
            def _(eng):
                run("pe", eng)


def host_consts():
    c = {}
    c["ident"] = np.eye(128, dtype=np.float32)
    s = np.arange(128)[:, None]
    t = np.arange(128)[None, :]
    c["triu"] = (s <= t).astype(np.float32)
    for k in range(3):
        sh = 3 - k
        c["shift%d" % k] = (s == t - sh).astype(np.float32)
        c["shiftp%d" % k] = (s == t - sh + 128).astype(np.float32)
    c["ones"] = np.ones((128, 128), dtype=np.float32)
    c["gt"] = (s > t).astype(np.float32)
    c["iota"] = np.broadcast_to(np.arange(128, dtype=np.float32)[None, :], (128, 128)).copy()
    return c


CONST_NAMES = ["ident", "triu", "shift0", "shift1", "shift2", "shiftp0", "shiftp1", "shiftp2", "iota", "ones", "gt"]


def build(NSEQ=4, L=2048, sweeps=(1, 2, 3, 4), debug=False):
    NCH = L // 128
    T = NSEQ * L
    NT = T // 128
    nc = bass.Bass("TRN2", target_bir_lowering=False)
    scratch_kind = "ExternalOutput" if debug else "Internal"

    in_names = []

    def din(name, shape, dt=F32):
        in_names.append(name)
        return nc.dram_tensor(name, list(shape), dt, kind="ExternalInput").ap()

    x_d = din("x", [T, D])
    w_in_d = din("w_in", [D, DIN])
    g1_d = din("norm1_gc", [128, 8])
    lng_d = din("gmlp_ln_g", [GMW])
    lnb_d = din("gmlp_ln_b", [GMW])
    wsT_d = din("gmlp_wsT", [128, 8, 128])
    bsT_d = din("gmlp_bsT", [128, 8])
    consts_d = din("consts", [128, len(CONST_NAMES) * 128])
    convw_d = din("conv_w", [4 * CONVD])
    convb_d = din("conv_b", [1, CONVD])
    dtb_d = din("dt_bias", [NH])
    alog_d = din("a_log", [NH])
    dskip_d = din("d_skip", [NH])
    if 3 in sweeps or 4 in sweeps:
        wout_d = din("w_out", [2048, D])
        gcat_d = din("gcat", [128, 16])
        wqT_d = din("w_queryT", [2048, D])
        kT_d = din("sub_keysT", [128, 2, 128])
        g2c_d = din("norm2_gc", [128, 8])
        if 4 in sweeps:
            uT_d = din("expert_uT", [D, NEXP])
            ev_d = din("expert_v", [NEXP, D])
            fg_d = din("final_g", [D])
            UTb_d = nc.dram_tensor("UTb", [32, 128, 8, 512], BF16, kind="Internal").ap()
            Vb_d = nc.dram_tensor("Vb", [32, 128, 4, 1024], BF16, kind="Internal").ap()
        x1_d = nc.dram_tensor("x1s", [T, D], F32, kind=scratch_kind).ap()
        h2T_d = nc.dram_tensor("h2Ts", [NT, 128, 1024], BF16, kind=scratch_kind).ap()
        rt_d = nc.dram_tensor("rts", [NT, 128, 384], BF16, kind=scratch_kind).ap()
    cat_d = nc.dram_tensor("cat", [T, 2048], BF16, kind=scratch_kind).ap()
    out_d = nc.dram_tensor("out", [T, D], F32, kind="ExternalOutput").ap()

    with contextlib.ExitStack() as stack:
        P = Prog(nc, stack)

        def sb(name, shape, dt=F32):
            return stack.enter_context(nc.sbuf_tensor(name, list(shape), dt))

        ps = stack.enter_context(nc.psum_tensor("ps", [128, 8, 512], F32))
        pstok = [Tok("ps%d" % i) for i in range(8)]

        finals = []
        cst = sb("cst", [128, len(CONST_NAMES) * 128])
        t_cst = Tok()
        ds_c = P.dsem()
        P.op("sp", lambda e: e.dma_start(out=cst[:, :], in_=consts_d[:, :]), writes=[t_cst], dsem=ds_c)

        def cf(name):
            i = CONST_NAMES.index(name)
            return cst[:, i * 128:(i + 1) * 128]

        ident_bf = sb("ident_bf", [128, 128], BF16)
        t_ident = Tok()
        P.op("dve", lambda e: e.tensor_copy(out=ident_bf[:, :], in_=cf("ident")), reads=[t_cst], writes=[t_ident])

        nhalf = sb("nhalf", [128, 16])
        t_nhalf = Tok()
        P.op("pool", lambda e: e.memset(nhalf[:, :], -0.5), writes=[t_nhalf])

        def rsqrt(out_ap, in_ap, toks_r, toks_w):
            n = in_ap.shape[-1]
            return P.op("pool", lambda e: e.tensor_tensor(out=out_ap, in0=in_ap, in1=nhalf[:, 0:n], op=ALU.pow),
                        reads=list(toks_r) + [t_nhalf], writes=list(toks_w))

        ds_stg = P.dsem()

        def prepass_gen(sbx):
            dve = lambda fn, r=(), w=(): P.op("dve", fn, r, w)
            act = lambda fn, r=(), w=(): P.op("act", fn, r, w)
            g2c4 = sbx("g2c4", [128, 8]); t_g2c4 = Tok(); ds_s4 = P.dsem(group=True)
            P.op("sp", lambda e: e.dma_start(out=g2c4[:, :], in_=g2c_d[:, :]), writes=[t_g2c4], dsem=ds_s4)
            cin = [sbx("cin%d" % i, [128, 4096]) for i in range(2)]; t_cin = [Tok(), Tok()]; ds_cin = [P.dsem(), P.dsem()]
            cout = [sbx("cout%d" % i, [128, 4096], BF16) for i in range(2)]; t_cout = [Tok(), Tok()]; ds_cout = [P.dsem(), P.dsem()]
            uT_v = uT_d.rearrange("(k p) e -> p k e", p=128)
            ev_v = ev_d.rearrange("(g b p) d -> g p b d", b=4, p=128)
            for it in range(64):
                bb = it % 2
                g = it // 2
                if it % 2 == 0:
                    P.op("sp", lambda e, g=g, bb=bb: e.dma_start(out=cin[bb][:, :].rearrange("p (k e) -> p k e", k=8),
                                                                 in_=uT_v[:, :, g * 512:(g + 1) * 512]),
                         writes=[t_cin[bb]], dsem=ds_cin[bb])
                    dve(lambda e, bb=bb: e.tensor_tensor(out=cout[bb][:, :].rearrange("p (k e) -> p k e", k=8),
                                                         in0=cin[bb][:, :].rearrange("p (k e) -> p k e", k=8),
                                                         in1=g2c4[:, :].unsqueeze(2).to_broadcast([128, 8, 512]), op=ALU.mult),
                        [t_cin[bb], t_g2c4], [t_cout[bb]])
                    P.op("pool", lambda e, g=g, bb=bb: e.dma_start(out=UTb_d[g, :, :, :].rearrange("p k e -> p (k e)"), in_=cout[bb][:, :]),
                         reads=[t_cout[bb]], writes=[], dsem=ds_cout[bb])
                else:
                    P.op("sp", lambda e, g=g, bb=bb: e.dma_start(out=cin[bb][:, :].rearrange("p (b d) -> p b d", b=4), in_=ev_v[g, :, :, :]),
                         writes=[t_cin[bb]], dsem=ds_cin[bb])
                    act(lambda e, bb=bb: e.activation(out=cout[bb][:, :], in_=cin[bb][:, :], func=AF.Copy), [t_cin[bb]], [t_cout[bb]])
                    P.op("pool", lambda e, g=g, bb=bb: e.dma_start(out=Vb_d[g, :, :, :].rearrange("p b d -> p (b d)"), in_=cout[bb][:, :]),
                         reads=[t_cout[bb]], writes=[], dsem=ds_cout[bb])
                yield

        def _sweep1():
            with contextlib.ExitStack() as s1:
                def sb1(name, shape, dt=F32):
                    return s1.enter_context(nc.sbuf_tensor(name, list(shape), dt))

                stg = sb1("stg_1", [128, 2576]); t_stg = Tok()

                w1 = sb1("w1", [128, 8, 2048], BF16)
                t_w1 = Tok()
                g1c = sb1("g1c", [128, 8])
                t_g1c = Tok()
                ds_small = P.dsem(group=True)
                P.op("sp", lambda e: e.dma_start(out=g1c[:, :], in_=g1_d[:, :]),
                     writes=[t_g1c], dsem=ds_small)
                for k in range(8):
                    P.op("sp", lambda e, k=k: e.dma_start(out=stg[:, 0:2048], in_=w_in_d[k * 128:(k + 1) * 128, 0:2048]),
                         writes=[t_stg], dsem=ds_stg)
                    P.op("dve", lambda e, k=k: e.tensor_scalar(out=w1[:, k, :], in0=stg[:, 0:2048], scalar1=g1c[:, k:k + 1],
                                                               scalar2=None, op0=ALU.mult),
                         reads=[t_stg, t_g1c], writes=[t_w1])
                WT = sb1("WT", [128, 8, 128], BF16)
                t_WT = Tok()
                P.op("sp", lambda e: e.dma_start(out=stg[:, 0:1024], in_=wsT_d.rearrange("s h t -> s (h t)")),
                     writes=[t_stg], dsem=ds_stg)
                P.op("dve", lambda e: e.tensor_tensor(out=WT[:, :, :], in0=stg[:, 0:1024].rearrange("s (h t) -> s h t", h=8),
                                                      in1=cf("triu").unsqueeze(1).to_broadcast([128, 8, 128]), op=ALU.mult),
                     reads=[t_stg, t_cst], writes=[t_WT])
                bsT = sb1("bsT", [128, 8])
                t_bsT = Tok()
                P.op("sp", lambda e: e.dma_start(out=bsT[:, :], in_=bsT_d[:, :]), writes=[t_bsT], dsem=ds_small)
                lngb = sb1("lngb", [128, 1024])
                lnbb = sb1("lnbb", [128, 1024])
                t_ln = Tok()
                P.op("sp", lambda e: e.dma_start(out=lngb[:, :], in_=lng_d.partition_broadcast(128)), writes=[t_ln], dsem=ds_small)
                P.op("sp", lambda e: e.dma_start(out=lnbb[:, :], in_=lnb_d.partition_broadcast(128)), writes=[t_ln], dsem=ds_small)

                NB = 2
                xt = [sb1("xt%d" % i, [128, 1024]) for i in range(NB)]
                t_xt = [Tok() for _ in range(NB)]
                ds_xt = [P.dsem() for _ in range(NB)]
                junk = sb1("junk", [128, 1024], BF16)
                t_junk = Tok()
                st = sb1("st", [128, 16])
                t_st = Tok()
                hb = sb1("hb", [128, 1024], BF16)
                t_hb = Tok()
                hT = sb1("hT", [128, 8, 128], BF16)
                t_hT = Tok()
                vtmp = sb1("vtmp", [128, 1024])
                t_vtmp = Tok()
                vb = sb1("vb", [128, 1024], BF16)
                t_vb = Tok()
                gm = sb1("gm", [128, 1024])
                t_gm = Tok()
                gmn = [sb1("gmn%d" % i, [128, 1024], BF16) for i in range(NB)]
                t_gmn = [Tok() for _ in range(NB)]
                ds_gmn = [P.dsem() for _ in range(NB)]

                ug2 = [sb1("ug2_%d" % i, [128, 1024]) for i in range(2)]; t_ug2 = [Tok(), Tok()]
                vg2 = [sb1("vg2_%d" % i, [128, 1024]) for i in range(2)]; t_vg2 = [Tok(), Tok()]
                stb = [sb1("stb_%d" % i, [128, 16]) for i in range(2)]; t_stb = [Tok(), Tok()]

                def front(ci):
                    r0 = ci * 128
                    b = ci % NB
                    P.op("sp", lambda e, b=b, r0=r0: e.dma_start(out=xt[b][:, :], in_=x_d[r0:r0 + 128, :]),
                         writes=[t_xt[b]], dsem=ds_xt[b])
                    P.op("act", lambda e, b=b: e.activation(out=junk[:, :], in_=xt[b][:, :], func=AF.Square, accum_out=st[:, 0:1]),
                         reads=[t_xt[b]], writes=[t_junk, t_st])
                    P.op("dve", lambda e: e.tensor_scalar(out=st[:, 1:2], in0=st[:, 0:1], scalar1=1.0 / D, scalar2=EPS,
                                                          op0=ALU.mult, op1=ALU.add), reads=[t_st], writes=[t_st])
                    rsqrt(st[:, 2:3], st[:, 1:2], [t_st], [t_st])
                    P.op("act", lambda e, b=b: e.activation(out=hb[:, :], in_=xt[b][:, :], func=AF.Copy, scale=st[:, 2:3]),
                         reads=[t_xt[b], t_st], writes=[t_hb])
                    yield
                    psT = ps[:, 7, :].bitcast(BF16)
                    for k in range(8):
                        P.op("pe", lambda e, k=k, psT=psT: e.transpose(out=psT[:, k * 128:(k + 1) * 128], in_=hb[:, k * 128:(k + 1) * 128],
                                                                       identity=ident_bf[:, :]),
                             reads=[t_hb, t_ident], writes=[pstok[7]])
                    P.op("dve", lambda e, psT=psT: e.tensor_copy(out=hT[:, :, :].rearrange("p k t -> p (k t)"), in_=psT[:, :]),
                         reads=[pstok[7]], writes=[t_hT])
                    yield
                    for j in range(4):
                        for k in range(8):
                            P.op("pe", lambda e, j=j, k=k: e.matmul(out=ps[:, j, :], lhsT=hT[:, k, :], rhs=w1[:, k, j * 512:(j + 1) * 512],
                                                                    start=(k == 0), stop=(k == 7)),
                                 reads=[t_hT, t_w1], writes=[pstok[j]])
                    yield
                    P.op("act", lambda e, b=b: e.activation(out=ug2[b][:, :], in_=ps[:, 0:2, :].rearrange("p a b -> p (a b)"), func=AF.Gelu),
                         reads=[pstok[0], pstok[1]], writes=[t_ug2[b]])
                    P.op("act", lambda e, b=b: e.activation(out=vg2[b][:, :], in_=ps[:, 2:4, :].rearrange("p a b -> p (a b)"), func=AF.Gelu,
                                                       accum_out=stb[b][:, 3:4]),
                         reads=[pstok[2], pstok[3]], writes=[t_vg2[b], t_stb[b]])
                    yield

                def back(ci):
                    r0 = ci * 128
                    b = ci % NB
                    P.op("act", lambda e, b=b: e.activation(out=junk[:, :], in_=vg2[b][:, :], func=AF.Square, accum_out=stb[b][:, 4:5]),
                         reads=[t_vg2[b]], writes=[t_junk, t_stb[b]])
                    P.op("dve", lambda e, b=b: e.tensor_scalar(out=stb[b][:, 5:6], in0=stb[b][:, 3:4], scalar1=1.0 / GMW, scalar2=None, op0=ALU.mult),
                         reads=[t_stb[b]], writes=[t_stb[b]])
                    P.op("dve", lambda e, b=b: e.tensor_tensor(out=stb[b][:, 6:7], in0=stb[b][:, 5:6], in1=stb[b][:, 5:6], op=ALU.mult),
                         reads=[t_stb[b]], writes=[t_stb[b]])
                    P.op("dve", lambda e, b=b: e.scalar_tensor_tensor(out=stb[b][:, 7:8], in0=stb[b][:, 4:5], scalar=1.0 / GMW, in1=stb[b][:, 6:7],
                                                                 op0=ALU.mult, op1=ALU.subtract),
                         reads=[t_stb[b]], writes=[t_stb[b]])
                    P.op("dve", lambda e, b=b: e.tensor_scalar(out=stb[b][:, 12:13], in0=stb[b][:, 7:8], scalar1=EPS, scalar2=None,
                                                          op0=ALU.add), reads=[t_stb[b]], writes=[t_stb[b]])
                    rsqrt(stb[b][:, 8:9], stb[b][:, 12:13], [t_stb[b]], [t_stb[b]])
                    P.op("dve", lambda e, b=b: e.scalar_tensor_tensor(out=vtmp[:, :], in0=vg2[b][:, :], scalar=stb[b][:, 5:6], in1=lngb[:, :],
                                                                 op0=ALU.subtract, op1=ALU.mult),
                         reads=[t_vg2[b], t_stb[b], t_ln], writes=[t_vtmp])
                    P.op("dve", lambda e, b=b: e.scalar_tensor_tensor(out=vb[:, :], in0=vtmp[:, :], scalar=stb[b][:, 8:9], in1=lnbb[:, :],
                                                                 op0=ALU.mult, op1=ALU.add),
                         reads=[t_vtmp, t_stb[b], t_ln], writes=[t_vb])
                    yield
                    for h in range(8):
                        P.op("pe", lambda e, h=h: e.matmul(out=ps[:, 4 + h // 4, (h % 4) * 128:(h % 4 + 1) * 128], lhsT=WT[:, h, :],
                                                           rhs=vb[:, h * 128:(h + 1) * 128], start=True, stop=True),
                             reads=[t_WT, t_vb], writes=[pstok[4 + h // 4]])
                    P.op("dve", lambda e, b=b: e.tensor_tensor(out=gm[:, :].rearrange("p (h d) -> p h d", h=8),
                                                          in0=ps[:, 4:6, :].rearrange("p a (h d) -> p (a h) d", d=128),
                                                          in1=bsT[:, :].unsqueeze(2).to_broadcast([128, 8, 128]), op=ALU.add),
                         reads=[pstok[4], pstok[5], t_bsT], writes=[t_gm])
                    yield
                    P.op("dve", lambda e, b=b: e.tensor_tensor(out=gm[:, :], in0=gm[:, :], in1=ug2[b][:, :], op=ALU.mult),
                         reads=[t_gm, t_ug2[b]], writes=[t_gm])
                    P.op("act", lambda e, b=b: e.activation(out=junk[:, :], in_=gm[:, :], func=AF.Square, accum_out=stb[b][:, 9:10]),
                         reads=[t_gm], writes=[t_junk, t_stb[b]])
                    P.op("dve", lambda e, b=b: e.tensor_scalar(out=stb[b][:, 10:11], in0=stb[b][:, 9:10], scalar1=1.0 / GMW, scalar2=EPS,
                                                          op0=ALU.mult, op1=ALU.add), reads=[t_stb[b]], writes=[t_stb[b]])
                    rsqrt(stb[b][:, 11:12], stb[b][:, 10:11], [t_stb[b]], [t_stb[b]])
                    yield
                    P.op("act", lambda e, b=b: e.activation(out=gmn[b][:, :], in_=gm[:, :], func=AF.Copy, scale=stb[b][:, 11:12]),
                         reads=[t_gm, t_stb[b]], writes=[t_gmn[b]])
                    finals.append(P.op("pool", lambda e, b=b, r0=r0: e.dma_start(out=cat_d[r0:r0 + 128, 0:1024], in_=gmn[b][:, :]),
                                       reads=[t_gmn[b]], writes=[], dsem=ds_gmn[b]))


                    yield

                def interleave(g1, g2):
                    gens = [g for g in (g1, g2) if g is not None]
                    while gens:
                        for g in list(gens):
                            try:
                                next(g)
                            except StopIteration:
                                gens.remove(g)

                pp = prepass_gen(sb1) if 4 in sweeps else None
                interleave(front(0), None)
                for ci in range(NT):
                    interleave(back(ci), front(ci + 1) if ci + 1 < NT else None)
                    if pp is not None:
                        for _ in range(-(-64 // NT)):
                            next(pp, None)
                if pp is not None:
                    for _ in pp:
                        pass

        def _sweep2():
            P.barrier()
            with contextlib.ExitStack() as s2:
                def sb2(name, shape, dt=F32):
                    return s2.enter_context(nc.sbuf_tensor(name, list(shape), dt))

                stg = sb2("stg_2", [128, 2576]); t_stg = Tok()

                dve = lambda fn, r=(), w=(): P.op("dve", fn, r, w)
                act = lambda fn, r=(), w=(): P.op("act", fn, r, w)
                pe = lambda fn, r=(), w=(): P.op("pe", fn, r, w)
                pool = lambda fn, r=(), w=(): P.op("pool", fn, r, w)

                ds_small2 = P.dsem(group=True)
                w2 = sb2("w2", [128, 8, 2576], BF16)
                t_w2 = Tok()
                g1c2 = sb2("g1c2", [128, 8])
                t_g1c2 = Tok()
                P.op("sp", lambda e: e.dma_start(out=g1c2[:, :], in_=g1_d[:, :]), writes=[t_g1c2], dsem=ds_small2)
                for k in range(8):
                    P.op("sp", lambda e, k=k: e.dma_start(out=stg[:, 0:2576], in_=w_in_d[k * 128:(k + 1) * 128, 2048:4624]),
                         writes=[t_stg], dsem=ds_stg)
                    dve(lambda e, k=k: e.tensor_scalar(out=w2[:, k, :], in0=stg[:, 0:2576], scalar1=g1c2[:, k:k + 1],
                                                       scalar2=None, op0=ALU.mult), [t_stg, t_g1c2], [t_w2])
                shb = sb2("shb", [128, 6, 128], BF16)
                t_shb = Tok()
                i0 = CONST_NAMES.index("shift0")
                dve(lambda e: e.tensor_copy(out=shb[:, :, :].rearrange("p a b -> p (a b)"), in_=cst[:, i0 * 128:(i0 + 6) * 128]),
                    [t_cst], [t_shb])
                onesb = sb2("onesb", [128, 128], BF16)
                t_onesb = Tok()
                dve(lambda e: e.tensor_copy(out=onesb[:, :], in_=cf("ones")), [t_cst], [t_onesb])
                convb = sb2("convb", [1, CONVD], BF16)
                t_convb = Tok()
                P.op("sp", lambda e: e.dma_start(out=stg[0:1, 0:CONVD], in_=convb_d[:, :]), writes=[t_stg], dsem=ds_stg)
                dve(lambda e: e.tensor_copy(out=convb[:, :], in_=stg[0:1, 0:CONVD]), [t_stg], [t_convb])
                convw = sb2("convw", [128, 4, CONVD])
                t_convw = Tok()
                P.op("sp", lambda e: e.dma_start(out=convw[:, :, :].rearrange("p k c -> p (k c)"), in_=convw_d.partition_broadcast(128)),
                     writes=[t_convw], dsem=ds_small2)
                sm = sb2("sm", [128, 4, 16])
                t_sm = Tok()
                P.op("sp", lambda e: e.dma_start(out=sm[:, 0, :], in_=dtb_d.partition_broadcast(128)), writes=[t_sm], dsem=ds_small2)
                P.op("sp", lambda e: e.dma_start(out=sm[:, 1, :], in_=alog_d.partition_broadcast(128)), writes=[t_sm], dsem=ds_small2)
                P.op("sp", lambda e: e.dma_start(out=sm[:, 2, :], in_=dskip_d.partition_broadcast(128)), writes=[t_sm], dsem=ds_small2)
                act(lambda e: e.activation(out=sm[:, 1, :], in_=sm[:, 1, :], func=AF.Exp), [t_sm], [t_sm])
                dve(lambda e: e.tensor_scalar(out=sm[:, 1, :], in0=sm[:, 1, :], scalar1=-1.0, scalar2=None, op0=ALU.mult), [t_sm], [t_sm])

                xt = sb2("xt_2", [128, 1024]); t_xt = Tok(); ds_xt = P.dsem()
                junkF = sb2("junkF_2", [128, 1024], BF16); t_junkF = Tok()
                junkB = sb2("junkB_2", [128, 512], BF16); t_junkB = Tok()
                stF = sb2("stF_2", [128, 4]); t_stF = Tok()
                stB = sb2("stB_2", [128, 16]); t_stB = Tok()
                hb = sb2("hb_2", [128, 1024], BF16); t_hb = Tok()
                hT = sb2("hT_2", [128, 8, 128], BF16); t_hT = Tok()
                zs = [sb2("zs%d" % i, [128, 1024]) for i in range(2)]; t_zs = [Tok(), Tok()]
                xw = [sb2("xw%d" % i, [128, 4, CONVD], BF16) for i in range(2)]; t_xw = [Tok(), Tok()]
                xs = [sb2("xs%d" % i, [128, 16, 64]) for i in range(2)]; t_xs = [Tok(), Tok()]
                bc = [sb2("bc%d" % i, [128, 4, 128], BF16) for i in range(2)]; t_bc = [Tok(), Tok()]
                bcT = [sb2("bcT%d" % i, [128, 4, 128], BF16) for i in range(2)]; t_bcT = [Tok(), Tok()]
                dF = [sb2("dF%d" % i, [128, 4, 16]) for i in range(2)]; t_dF = [Tok(), Tok()]
                dtv = sb2("dtv", [128, 8, 16]); t_dtv = Tok()
                R = sb2("R", [128, 16, 128]); t_R = Tok()
                LT = sb2("LT", [128, 16, 128], BF16); t_LT = Tok()
                cbm = sb2("cbm", [128, 2, 128], BF16); t_cbm = Tok()
                MT = sb2("MT", [128, 16, 128], BF16); t_MT = Tok()
                xdt = sb2("xdt", [128, 16, 64], BF16); t_xdt = Tok()
                xdtd = sb2("xdtd", [128, 16, 64], BF16); t_xdtd = Tok()
                stT = sb2("stT", [128, 16, 64]); t_stT = Tok()
                stTb = sb2("stTb", [128, 16, 64], BF16); t_stTb = Tok()
                yy = sb2("yy", [128, 16, 64]); t_yy = Tok()
                so = [sb2("so%d" % i, [128, 1024], BF16) for i in range(2)]; t_so = [Tok(), Tok()]
                ds_so = [P.dsem(), P.dsem()]
                ps3b = ps[:, 3, :].bitcast(BF16)
                flat = lambda ap: ap.rearrange("p a b -> p (a b)")

                def front(ci):
                    r0 = ci * 128
                    par = ci % 2
                    first = (ci % NCH == 0)
                    P.op("sp", lambda e, r0=r0: e.dma_start(out=xt[:, :], in_=x_d[r0:r0 + 128, :]), writes=[t_xt], dsem=ds_xt)
                    act(lambda e: e.activation(out=junkF[:, :], in_=xt[:, :], func=AF.Square, accum_out=stF[:, 0:1]), [t_xt], [t_junkF, t_stF])
                    dve(lambda e: e.tensor_scalar(out=stF[:, 1:2], in0=stF[:, 0:1], scalar1=1.0 / D, scalar2=EPS, op0=ALU.mult, op1=ALU.add),
                        [t_stF], [t_stF])
                    rsqrt(stF[:, 2:3], stF[:, 1:2], [t_stF], [t_stF])
                    act(lambda e: e.activation(out=hb[:, :], in_=xt[:, :], func=AF.Copy, scale=stF[:, 2:3]), [t_xt, t_stF], [t_hb])
                    yield
                    for k in range(8):
                        pe(lambda e, k=k: e.transpose(out=ps3b[:, k * 128:(k + 1) * 128], in_=hb[:, k * 128:(k + 1) * 128], identity=ident_bf[:, :]),
                           [t_hb, t_ident], [pstok[3]])
                    act(lambda e: e.activation(out=hT[:, :, :].rearrange("p k t -> p (k t)"), in_=ps3b[:, :], func=AF.Copy), [pstok[3]], [t_hT])
                    yield
                    for j in range(2):
                        for k in range(8):
                            pe(lambda e, j=j, k=k: e.matmul(out=ps[:, j, :], lhsT=hT[:, k, :], rhs=w2[:, k, j * 512:(j + 1) * 512],
                                                            start=(k == 0), stop=(k == 7)), [t_hT, t_w2], [pstok[j]])
                    for k in range(8):
                        pe(lambda e, k=k: e.matmul(out=ps[:, 2, 0:16], lhsT=hT[:, k, :], rhs=w2[:, k, 2560:2576],
                                                   start=(k == 0), stop=(k == 7)), [t_hT, t_w2], [pstok[2]])
                    yield
                    act(lambda e, par=par: e.activation(out=zs[par][:, :], in_=ps[:, 0:2, :].rearrange("p a b -> p (a b)"), func=AF.Silu),
                        [pstok[0], pstok[1]], [t_zs[par]])
                    dve(lambda e, par=par: e.tensor_tensor(out=dF[par][:, 2, :], in0=ps[:, 2, 0:16], in1=sm[:, 0, :], op=ALU.add), [pstok[2], t_sm], [t_dF[par]])
                    yield
                    for j in range(3):
                        for k in range(8):
                            pe(lambda e, j=j, k=k: e.matmul(out=ps[:, j, :], lhsT=hT[:, k, :], rhs=w2[:, k, 1024 + j * 512:1024 + (j + 1) * 512],
                                                            start=(k == 0), stop=(k == 7)), [t_hT, t_w2], [pstok[j]])
                    yield
                    dve(lambda e, par=par: e.tensor_scalar(out=dF[par][:, 3, :], in0=dF[par][:, 2, :], scalar1=-1.0, scalar2=None, op0=ALU.mult), [t_dF[par]], [t_dF[par]])
                    dve(lambda e, par=par: e.tensor_tensor(out=dF[par][:, 3, :], in0=dF[par][:, 3, :], in1=dF[par][:, 2, :], op=ALU.max), [t_dF[par]], [t_dF[par]])
                    act(lambda e, par=par: e.activation(out=dF[par][:, 3, :], in_=dF[par][:, 3, :], func=AF.Exp, scale=-1.0), [t_dF[par]], [t_dF[par]])
                    act(lambda e, par=par: e.activation(out=dF[par][:, 3, :], in_=dF[par][:, 3, :], func=AF.Ln, bias=1.0), [t_dF[par]], [t_dF[par]])
                    dve(lambda e, par=par: e.scalar_tensor_tensor(out=dF[par][:, 0, :], in0=dF[par][:, 2, :], scalar=0.0, in1=dF[par][:, 3, :],
                                                                  op0=ALU.max, op1=ALU.add), [t_dF[par]], [t_dF[par]])
                    dve(lambda e, par=par: e.tensor_tensor(out=dF[par][:, 1, :], in0=dF[par][:, 0, :], in1=sm[:, 1, :], op=ALU.mult), [t_dF[par], t_sm], [t_dF[par]])
                    yield
                    for k in range(4):
                        dve(lambda e, k=k, par=par: e.tensor_tensor(out=xw[par][:, k, :], in0=ps[:, 0:3, :].rearrange("p a b -> p (a b)"),
                                                                    in1=convw[:, k, :], op=ALU.mult),
                            [pstok[0], pstok[1], pstok[2], t_convw], [t_xw[par]])
                        if k == 1:
                            yield
                    yield
                    for j in range(3):
                        sl = slice(j * 512, (j + 1) * 512)
                        pe(lambda e, j=j, sl=sl, par=par: e.matmul(out=ps[:, j, :], lhsT=ident_bf[:, :], rhs=xw[par][:, 3, sl], start=True, stop=False),
                           [t_ident, t_xw[par]], [pstok[j]])
                        for k in range(3):
                            pe(lambda e, j=j, sl=sl, k=k, par=par: e.matmul(out=ps[:, j, :], lhsT=shb[:, k, :], rhs=xw[par][:, k, sl], start=False, stop=False),
                               [t_shb, t_xw[par]], [pstok[j]])
                        if not first:
                            for k in range(3):
                                pe(lambda e, j=j, sl=sl, k=k, par=par: e.matmul(out=ps[:, j, :], lhsT=shb[:, 3 + k, :], rhs=xw[1 - par][:, k, sl],
                                                                                start=False, stop=False),
                                   [t_shb, t_xw[1 - par]], [pstok[j]])
                        pe(lambda e, j=j, sl=sl: e.matmul(out=ps[:, j, :], lhsT=onesb[0:1, :], rhs=convb[0:1, sl], start=False, stop=True),
                           [t_onesb, t_convb], [pstok[j]])
                    yield
                    act(lambda e, par=par: e.activation(out=flat(xs[par][:, :, :]), in_=ps[:, 0:2, :].rearrange("p a b -> p (a b)"), func=AF.Silu),
                        [pstok[0], pstok[1]], [t_xs[par]])
                    act(lambda e, par=par: e.activation(out=flat(bc[par][:, :, :]), in_=ps[:, 2, :], func=AF.Silu), [pstok[2]], [t_bc[par]])
                    yield
                    for i in range(4):
                        pe(lambda e, i=i, par=par: e.transpose(out=ps3b[:, i * 128:(i + 1) * 128], in_=bc[par][:, i, :], identity=ident_bf[:, :]),
                           [t_bc[par], t_ident], [pstok[3]])
                    act(lambda e, par=par: e.activation(out=flat(bcT[par][:, :, :]), in_=ps3b[:, 0:512], func=AF.Copy), [pstok[3]], [t_bcT[par]])
                    yield

                def back(ci):
                    r0 = ci * 128
                    par = ci % 2
                    first = (ci % NCH == 0)
                    last = (ci % NCH == NCH - 1)
                    dt_ = dF[par][:, 0, :]
                    a_ = dF[par][:, 1, :]
                    pe(lambda e, a_=a_: e.matmul(out=ps[:, 4, 16:32], lhsT=cf("triu"), rhs=a_, start=True, stop=True), [t_cst, t_dF[par]], [pstok[4]])
                    dve(lambda e: e.tensor_copy(out=dtv[:, 2, :], in_=ps[:, 4, 16:32]), [pstok[4]], [t_dtv])
                    for g in range(2):
                        pe(lambda e, g=g, par=par: e.matmul(out=ps[:, 4, 256 + g * 128:256 + (g + 1) * 128], lhsT=bcT[par][:, g, :], rhs=bcT[par][:, 2 + g, :],
                                                            start=True, stop=True), [t_bcT[par]], [pstok[4]])
                    dve(lambda e: e.tensor_tensor(out=cbm[:, :, :], in0=ps[:, 4, 256:512].rearrange("p (g t) -> p g t", g=2),
                                                  in1=cf("triu").unsqueeze(1).to_broadcast([128, 2, 128]), op=ALU.mult),
                        [pstok[4], t_cst], [t_cbm])
                    dve(lambda e, a_=a_: e.tensor_tensor(out=R[:, :, :], in0=cf("triu").unsqueeze(1).to_broadcast([128, 16, 128]),
                                                         in1=a_.unsqueeze(2).to_broadcast([128, 16, 128]), op=ALU.mult),
                        [t_cst, t_dF[par]], [t_R])
                    yield
                    ab = [4, 5, 6, 7]
                    for q in range(4):
                        pe(lambda e, q=q: e.matmul(out=ps[:, ab[q], :], lhsT=cf("gt"), rhs=R[:, 4 * q:4 * q + 4, :].rearrange("p a b -> p (a b)"),
                                                   start=True, stop=True), [t_cst, t_R], [pstok[ab[q]]])
                    yield
                    for q in range(4):
                        dve(lambda e, q=q: e.tensor_copy(out=dtv[:, 3, 4 * q:4 * q + 4],
                                                         in_=ps[:, ab[q], :].rearrange("p (h t) -> p h t", h=4)[:, :, 127]),
                            [pstok[ab[q]]], [t_dtv])
                    yield
                    act(lambda e: e.activation(out=flat(LT[:, :, :]), in_=ps[:, 4:8, :].rearrange("p a b -> p (a b)"), func=AF.Exp),
                        [pstok[4], pstok[5], pstok[6], pstok[7]], [t_LT])
                    dve(lambda e: e.tensor_tensor(out=dtv[:, 7, :], in0=dtv[:, 3, :], in1=dtv[:, 2, :], op=ALU.add), [t_dtv], [t_dtv])
                    act(lambda e: e.activation(out=dtv[:, 4, :], in_=dtv[:, 3, :], func=AF.Exp), [t_dtv], [t_dtv])
                    act(lambda e: e.activation(out=dtv[:, 5, :], in_=dtv[:, 7, :], func=AF.Exp), [t_dtv], [t_dtv])
                    act(lambda e: e.activation(out=dtv[:, 6, :], in_=dtv[:, 2, :], func=AF.Exp), [t_dtv], [t_dtv])
                    dve(lambda e, dt_=dt_: e.tensor_tensor(out=dtv[:, 4, :], in0=dtv[:, 4, :], in1=dt_, op=ALU.mult), [t_dtv, t_dF[par]], [t_dtv])
                    yield
                    for g in range(2):
                        dve(lambda e, g=g: e.tensor_tensor(out=MT[:, 8 * g:8 * g + 8, :], in0=LT[:, 8 * g:8 * g + 8, :],
                                                           in1=cbm[:, g, :].unsqueeze(1).to_broadcast([128, 8, 128]), op=ALU.mult),
                            [t_LT, t_cbm], [t_MT])
                    dve(lambda e, par=par, dt_=dt_: e.tensor_tensor(out=xdt[:, :, :], in0=xs[par][:, :, :], in1=dt_.unsqueeze(2).to_broadcast([128, 16, 64]), op=ALU.mult),
                        [t_xs[par], t_dF[par]], [t_xdt])
                    yield
                    dve(lambda e, par=par: e.tensor_tensor(out=xdtd[:, :, :], in0=xs[par][:, :, :], in1=dtv[:, 4, :].unsqueeze(2).to_broadcast([128, 16, 64]), op=ALU.mult),
                        [t_xs[par], t_dtv], [t_xdtd])
                    for h in range(16):
                        pe(lambda e, h=h: e.matmul(out=ps[:, 4 + h // 8, (h % 8) * 64:(h % 8 + 1) * 64], lhsT=MT[:, h, :], rhs=xdt[:, h, :],
                                                   start=True, stop=True), [t_MT, t_xdt], [pstok[4 + h // 8]])
                    if not first:
                        for g in range(2):
                            pe(lambda e, g=g, par=par: e.matmul(out=ps[:, 6 + g, :], lhsT=bcT[par][:, 2 + g, :], rhs=flat(stTb[:, 8 * g:8 * g + 8, :]),
                                                                start=True, stop=True), [t_bcT[par], t_stTb], [pstok[6 + g]])
                        yield
                        dve(lambda e: e.tensor_tensor(out=yy[:, :, :], in0=ps[:, 6:8, :].rearrange("p a (h d) -> p (a h) d", d=64),
                                                      in1=dtv[:, 6, :].unsqueeze(2).to_broadcast([128, 16, 64]), op=ALU.mult),
                            [pstok[6], pstok[7], t_dtv], [t_yy])
                        dve(lambda e: e.tensor_tensor(out=flat(yy[:, :, :]), in0=flat(yy[:, :, :]),
                                                      in1=ps[:, 4:6, :].rearrange("p a b -> p (a b)"), op=ALU.add),
                            [pstok[4], pstok[5], t_yy], [t_yy])
                    else:
                        yield
                        dve(lambda e: e.tensor_copy(out=flat(yy[:, :, :]), in_=ps[:, 4:6, :].rearrange("p a b -> p (a b)")),
                            [pstok[4], pstok[5]], [t_yy])
                    yield
                    if not last:
                        for g in range(2):
                            pe(lambda e, g=g, par=par: e.matmul(out=ps[:, 4 + g, :], lhsT=bc[par][:, g, :], rhs=flat(xdtd[:, 8 * g:8 * g + 8, :]),
                                                                start=True, stop=True), [t_bc[par], t_xdtd], [pstok[4 + g]])
                        if first:
                            dve(lambda e: e.tensor_copy(out=flat(stT[:, :, :]), in_=ps[:, 4:6, :].rearrange("p a b -> p (a b)")),
                                [pstok[4], pstok[5]], [t_stT])
                        else:
                            dve(lambda e: e.tensor_tensor(out=stT[:, :, :], in0=stT[:, :, :], in1=dtv[:, 5, :].unsqueeze(2).to_broadcast([128, 16, 64]), op=ALU.mult),
                                [t_stT, t_dtv], [t_stT])
                            dve(lambda e: e.tensor_tensor(out=flat(stT[:, :, :]), in0=flat(stT[:, :, :]),
                                                          in1=ps[:, 4:6, :].rearrange("p a b -> p (a b)"), op=ALU.add),
                                [pstok[4], pstok[5], t_stT], [t_stT])
                        pool(lambda e: e.tensor_copy(out=stTb[:, :, :], in_=stT[:, :, :]), [t_stT], [t_stTb])
                    yield
                    dve(lambda e, par=par: e.tensor_tensor(out=xs[par][:, :, :], in0=xs[par][:, :, :], in1=sm[:, 2, :].unsqueeze(2).to_broadcast([128, 16, 64]), op=ALU.mult),
                        [t_xs[par], t_sm], [t_xs[par]])
                    dve(lambda e, par=par: e.tensor_tensor(out=yy[:, :, :], in0=yy[:, :, :], in1=xs[par][:, :, :], op=ALU.add), [t_yy, t_xs[par]], [t_yy])
                    yield
                    dve(lambda e, par=par: e.tensor_tensor(out=flat(yy[:, :, :]), in0=flat(yy[:, :, :]), in1=zs[par][:, :], op=ALU.mult), [t_yy, t_zs[par]], [t_yy])
                    for g in range(2):
                        act(lambda e, g=g: e.activation(out=junkB[:, 0:512], in_=flat(yy[:, 8 * g:8 * g + 8, :]),
                                                        func=AF.Square, accum_out=stB[:, 4 + g:5 + g]), [t_yy], [t_junkB, t_stB])
                    dve(lambda e: e.tensor_scalar(out=stB[:, 6:8], in0=stB[:, 4:6], scalar1=1.0 / 512, scalar2=EPS, op0=ALU.mult, op1=ALU.add),
                        [t_stB], [t_stB])
                    rsqrt(stB[:, 8:10], stB[:, 6:8], [t_stB], [t_stB])
                    yield
                    dve(lambda e, par=par: e.tensor_tensor(out=so[par][:, :].rearrange("p (g d) -> p g d", g=2),
                                                           in0=yy[:, :, :].rearrange("p (g a) b -> p g (a b)", g=2),
                                                           in1=stB[:, 8:10].unsqueeze(2).to_broadcast([128, 2, 512]), op=ALU.mult),
                        [t_yy, t_stB], [t_so[par]])
                    finals.append(P.op("pool", lambda e, par=par, r0=r0: e.dma_start(out=cat_d[r0:r0 + 128, 1024:2048], in_=so[par][:, :]),
                                       reads=[t_so[par]], writes=[], dsem=ds_so[par]))
                    yield

                def interleave(g1, g2):
                    gens = [g for g in (g1, g2) if g is not None]
                    while gens:
                        for g in list(gens):
                            try:
                                next(g)
                            except StopIteration:
                                gens.remove(g)

                interleave(front(0), None)
                for ci in range(NT):
                    interleave(back(ci), front(ci + 1) if ci + 1 < NT else None)

        def _sweep3():
            P.barrier()
            with contextlib.ExitStack() as s3:
                def sb3(name, shape, dt=F32):
                    return s3.enter_context(nc.sbuf_tensor(name, list(shape), dt))

                stg = sb3("stg_3", [128, 2576]); t_stg = Tok()

                dve = lambda fn, r=(), w=(): P.op("dve", fn, r, w)
                act = lambda fn, r=(), w=(): P.op("act", fn, r, w)
                pe = lambda fn, r=(), w=(): P.op("pe", fn, r, w)
                pool = lambda fn, r=(), w=(): P.op("pool", fn, r, w)
                ds_small3 = P.dsem(group=True)
                wo = sb3("wo", [128, 16, 1024], BF16); t_wo = Tok()
                gcat = sb3("gcat_s", [128, 16]); t_gcat = Tok()
                P.op("sp", lambda e: e.dma_start(out=gcat[:, :], in_=gcat_d[:, :]), writes=[t_gcat], dsem=ds_small3)
                for k in range(16):
                    P.op("sp", lambda e, k=k: e.dma_start(out=stg[:, 0:1024], in_=wout_d[k * 128:(k + 1) * 128, :]), writes=[t_stg], dsem=ds_stg)
                    dve(lambda e, k=k: e.tensor_scalar(out=wo[:, k, :], in0=stg[:, 0:1024], scalar1=gcat[:, k:k + 1], scalar2=None, op0=ALU.mult),
                        [t_stg, t_gcat], [t_wo])
                Wc = sb3("Wc", [128, 8, 2048], BF16); t_Wc = Tok()
                g2c = sb3("g2c", [128, 8]); t_g2c = Tok()
                P.op("sp", lambda e: e.dma_start(out=g2c[:, :], in_=g2c_d[:, :]), writes=[t_g2c], dsem=ds_small3)
                kT = sb3("kT", [128, 2, 128]); t_kT = Tok()
                P.op("sp", lambda e: e.dma_start(out=kT[:, :, :], in_=kT_d[:, :, :]), writes=[t_kT], dsem=ds_small3)
                stgq = [sb3("stgq%d" % i, [128, 1024]) for i in range(2)]; t_stgq = [Tok(), Tok()]; ds_q = [P.dsem(), P.dsem()]
                for j in range(16):
                    qb = j % 2
                    P.op("sp", lambda e, j=j, qb=qb: e.dma_start(out=stgq[qb][:, :], in_=wqT_d[j * 128:(j + 1) * 128, :]),
                         writes=[t_stgq[qb]], dsem=ds_q[qb])
                    b0 = qb * 2
                    for k in range(8):
                        pe(lambda e, j=j, k=k, qb=qb, b0=b0: e.matmul(out=ps[:, b0 + k // 4, (k % 4) * 128:(k % 4 + 1) * 128],
                                                                      lhsT=stgq[qb][:, k * 128:(k + 1) * 128], rhs=kT[:, j % 2, :], start=True, stop=True),
                           [t_stgq[qb], t_kT], [pstok[b0 + k // 4]])
                    dve(lambda e, j=j, b0=b0: e.tensor_tensor(out=Wc[:, :, j * 128:(j + 1) * 128],
                                                              in0=ps[:, b0:b0 + 2, :].rearrange("p a (k n) -> p (a k) n", n=128),
                                                              in1=g2c[:, :].unsqueeze(2).to_broadcast([128, 8, 128]), op=ALU.mult),
                        [pstok[b0], pstok[b0 + 1], t_g2c], [t_Wc])

                catb = sb3("catb", [128, 2048], BF16); t_catb = Tok(); ds_catb = P.dsem()
                xt = sb3("xt_3", [128, 1024]); t_xt = Tok(); ds_xt = P.dsem()
                catT = sb3("catT", [128, 16, 128], BF16); t_catT = Tok()
                x1 = [sb3("x1_%d" % i, [128, 1024]) for i in range(2)]; t_x1 = [Tok(), Tok()]; ds_x1 = [P.dsem(), P.dsem()]
                junk = sb3("junk_3", [128, 1024], BF16); t_junk = Tok()
                st = sb3("st_3", [128, 16]); t_st = Tok()
                h2b = sb3("h2b", [128, 1024], BF16); t_h2b = Tok()
                h2T = [sb3("h2T_%d" % i, [128, 8, 128], BF16) for i in range(2)]; t_h2T = [Tok(), Tok()]; ds_h2T = [P.dsem(), P.dsem()]
                Swk = sb3("Swk", [128, 16, 128]); t_Swk = Tok()
                vv = sb3("vv", [128, 16, 16]); t_vv = Tok()
                tg_vv = [Tok() for _ in range(16)]; tg_vv2 = [Tok() for _ in range(16)]; tg_Swk = [Tok() for _ in range(16)]
                tg_idx = [Tok() for _ in range(16)]; tg_idx2 = [Tok() for _ in range(16)]
                th_c16 = [Tok() for _ in range(8)]; th_c16b = [Tok() for _ in range(8)]; th_candw = [Tok() for _ in range(8)]
                th_cpos = [Tok() for _ in range(8)]; th_cposb = [Tok() for _ in range(8)]
                idx = sb3("idx", [128, 16, 16], U32); t_idx = Tok()
                idxf = sb3("idxf", [128, 16, 16]); t_idxf = Tok()
                cand = sb3("cand", [128, 8, 16, 16]); t_cand = Tok()
                candw = sb3("candw", [128, 8, 256]); t_candw = Tok()
                c16 = sb3("c16", [128, 8, 16]); t_c16 = Tok()
                cpos = sb3("cpos", [128, 8, 16], U32); t_cpos = Tok()
                cpi = sb3("cpi", [128, 2, 8, 16], U32); t_cpi = Tok()
                cpf = sb3("cpf", [128, 2, 8, 16]); t_cpf = Tok()
                ohs = [sb3("oh%d" % i, [128, 8, 16, 16]) for i in range(2)]; t_ohs = [Tok(), Tok()]
                gt = sb3("gt", [128, 8, 16]); t_gt = Tok()
                z8 = sb3("z8", [128, 8]); t_z8 = Tok()
                e12 = sb3("e12", [128, 2, 8, 16]); t_e12 = Tok()
                rtm = sb3("rtm", [128, 3, 128], BF16); t_rtm = Tok()
                rt = [sb3("rt_%d" % i, [128, 3, 128], BF16) for i in range(2)]; t_rt = [Tok(), Tok()]; ds_rt = [P.dsem(), P.dsem()]
                psT = ps[:, 7, :].bitcast(BF16)
                ps6b = ps[:, 6, :].bitcast(BF16)
                iota16 = cf("iota")[:, 0:16]

                ps4b = ps[:, 4, :].bitcast(BF16)
                Ssb2 = [sb3("Ssb2_%d" % i, [128, 16, 128]) for i in range(2)]; t_Ssb2 = [Tok(), Tok()]

                def front(ci):
                    r0 = ci * 128
                    par = ci % 2
                    P.op("sp", lambda e, r0=r0: e.dma_start(out=catb[:, :], in_=cat_d[r0:r0 + 128, :]), writes=[t_catb], dsem=ds_catb)
                    P.op("sp", lambda e, r0=r0: e.dma_start(out=xt[:, :], in_=x_d[r0:r0 + 128, :]), writes=[t_xt], dsem=ds_xt)
                    for k in range(16):
                        pb = psT if k < 8 else ps6b
                        pe(lambda e, k=k, pb=pb: e.transpose(out=pb[:, (k % 8) * 128:(k % 8 + 1) * 128], in_=catb[:, k * 128:(k + 1) * 128], identity=ident_bf[:, :]),
                           [t_catb, t_ident], [pstok[7 if k < 8 else 6]])
                    act(lambda e: e.activation(out=catT[:, 0:8, :].rearrange("p k t -> p (k t)"), in_=psT[:, :], func=AF.Copy), [pstok[7]], [t_catT])
                    act(lambda e: e.activation(out=catT[:, 8:16, :].rearrange("p k t -> p (k t)"), in_=ps6b[:, :], func=AF.Copy), [pstok[6]], [t_catT])
                    yield
                    for j in range(2):
                        for k in range(16):
                            pe(lambda e, j=j, k=k: e.matmul(out=ps[:, j, :], lhsT=catT[:, k, :], rhs=wo[:, k, j * 512:(j + 1) * 512],
                                                            start=(k == 0), stop=(k == 15)), [t_catT, t_wo], [pstok[j]])
                    yield
                    dve(lambda e, par=par: e.tensor_tensor(out=x1[par][:, :], in0=ps[:, 0:2, :].rearrange("p a b -> p (a b)"), in1=xt[:, :], op=ALU.add),
                        [pstok[0], pstok[1], t_xt], [t_x1[par]])
                    P.op("pool", lambda e, par=par, r0=r0: e.dma_start(out=x1_d[r0:r0 + 128, :], in_=x1[par][:, :]),
                         reads=[t_x1[par]], writes=[], dsem=ds_x1[par])
                    yield
                    act(lambda e, par=par: e.activation(out=junk[:, :], in_=x1[par][:, :], func=AF.Square, accum_out=st[:, 0:1]), [t_x1[par]], [t_junk, t_st])
                    dve(lambda e: e.tensor_scalar(out=st[:, 1:2], in0=st[:, 0:1], scalar1=1.0 / D, scalar2=EPS, op0=ALU.mult, op1=ALU.add), [t_st], [t_st])
                    rsqrt(st[:, 2:3], st[:, 1:2], [t_st], [t_st])
                    act(lambda e, par=par: e.activation(out=h2b[:, :], in_=x1[par][:, :], func=AF.Copy, scale=st[:, 2:3]), [t_x1[par], t_st], [t_h2b])
                    yield
                    for k in range(8):
                        pe(lambda e, k=k: e.transpose(out=psT[:, k * 128:(k + 1) * 128], in_=h2b[:, k * 128:(k + 1) * 128], identity=ident_bf[:, :]),
                           [t_h2b, t_ident], [pstok[7]])
                    act(lambda e, par=par: e.activation(out=h2T[par][:, :, :].rearrange("p k t -> p (k t)"), in_=psT[:, :], func=AF.Copy), [pstok[7]], [t_h2T[par]])
                    P.op("pool", lambda e, par=par, ci=ci: e.dma_start(out=h2T_d[ci, :, :], in_=h2T[par][:, :, :].rearrange("p k t -> p (k t)")),
                         reads=[t_h2T[par]], writes=[], dsem=ds_h2T[par])
                    yield
                    for j in range(4):
                        for k in range(8):
                            pe(lambda e, j=j, k=k, par=par: e.matmul(out=ps[:, j, :], lhsT=h2T[par][:, k, :], rhs=Wc[:, k, j * 512:(j + 1) * 512],
                                                                     start=(k == 0), stop=(k == 7)), [t_h2T[par], t_Wc], [pstok[j]])
                    yield
                    act(lambda e, par=par: e.activation(out=Ssb2[par][:, :, :].rearrange("p a b -> p (a b)"), in_=ps[:, 0:4, :].rearrange("p a b -> p (a b)"), func=AF.Copy),
                        [pstok[0], pstok[1], pstok[2], pstok[3]], [t_Ssb2[par]])
                    yield

                def back(ci):
                    r0 = ci * 128
                    par = ci % 2
                    for j in range(16):
                        dve(lambda e, j=j, par=par: e.max(out=vv[:, j, 0:8], in_=Ssb2[par][:, j, :]), [t_Ssb2[par]], [tg_vv[j]])
                    yield
                    for j in range(16):
                        dve(lambda e, j=j, par=par: e.match_replace(out=Swk[:, j, :], in_to_replace=vv[:, j, 0:8], in_values=Ssb2[par][:, j, :], imm_value=-1e30),
                            [t_Ssb2[par], tg_vv[j]], [tg_Swk[j]])
                    yield
                    for j in range(16):
                        dve(lambda e, j=j, par=par: e.max_index(out=idx[:, j, 0:8], in_max=vv[:, j, 0:8], in_values=Ssb2[par][:, j, :]), [t_Ssb2[par], tg_vv[j]], [tg_idx[j]])
                    yield
                    for j in range(16):
                        dve(lambda e, j=j, par=par: e.max(out=vv[:, j, 8:16], in_=Swk[:, j, :]), [tg_Swk[j]], [tg_vv2[j]])
                    yield
                    for j in range(16):
                        dve(lambda e, j=j, par=par: e.max_index(out=idx[:, j, 8:16], in_max=vv[:, j, 8:16], in_values=Swk[:, j, :]), [tg_Swk[j], tg_vv2[j]], [tg_idx2[j]])
                    yield
                    dve(lambda e: e.tensor_copy(out=idxf[:, :, :], in_=idx[:, :, :]), tg_idx + tg_idx2, [t_idxf])
                    v4 = vv[:, :, :].rearrange("p (h two) k -> p h two k", two=2)
                    dve(lambda e, v4=v4: e.tensor_tensor(out=cand[:, :, :, :], in0=v4[:, :, 0, :].unsqueeze(3).to_broadcast([128, 8, 16, 16]),
                                                         in1=v4[:, :, 1, :].unsqueeze(2).to_broadcast([128, 8, 16, 16]), op=ALU.add), tg_vv + tg_vv2, [t_cand])
                    cfls = [cand[:, h, :, :].rearrange("p a b -> p (a b)") for h in range(8)]
                    for h in range(8):
                        dve(lambda e, h=h: e.max(out=c16[:, h, 0:8], in_=cfls[h]), [t_cand], [th_c16[h]])
                    yield
                    for h in range(8):
                        dve(lambda e, h=h: e.match_replace(out=candw[:, h, :], in_to_replace=c16[:, h, 0:8], in_values=cfls[h], imm_value=-1e30),
                            [t_cand, th_c16[h]], [th_candw[h]])
                    yield
                    for h in range(8):
                        dve(lambda e, h=h: e.max_index(out=cpos[:, h, 0:8], in_max=c16[:, h, 0:8], in_values=cfls[h]), [t_cand, th_c16[h]], [th_cpos[h]])
                    yield
                    for h in range(8):
                        dve(lambda e, h=h: e.max(out=c16[:, h, 8:16], in_=candw[:, h, :]), [th_candw[h]], [th_c16b[h]])
                    yield
                    for h in range(8):
                        dve(lambda e, h=h: e.max_index(out=cpos[:, h, 8:16], in_max=c16[:, h, 8:16], in_values=candw[:, h, :]), [th_candw[h], th_c16b[h]], [th_cposb[h]])
                    yield
                    dve(lambda e: e.tensor_tensor(out=gt[:, :, :], in0=c16[:, :, :], in1=c16[:, :, 0:1].to_broadcast([128, 8, 16]), op=ALU.subtract),
                        th_c16 + th_c16b, [t_gt])
                    act(lambda e: e.activation(out=gt[:, :, :].rearrange("p a b -> p (a b)"), in_=gt[:, :, :].rearrange("p a b -> p (a b)"), func=AF.Exp),
                        [t_gt], [t_gt])
                    dve(lambda e: e.reduce_sum(out=z8[:, :], in_=gt[:, :, :], axis=AX.X), [t_gt], [t_z8])
                    dve(lambda e: e.reciprocal(out=z8[:, :], in_=z8[:, :]), [t_z8], [t_z8])
                    dve(lambda e: e.tensor_tensor(out=gt[:, :, :], in0=gt[:, :, :], in1=z8[:, :].unsqueeze(2).to_broadcast([128, 8, 16]), op=ALU.mult),
                        [t_gt, t_z8], [t_gt])
                    yield
                    dve(lambda e: e.tensor_single_scalar(out=cpi[:, 0, :, :], in_=cpos[:, :, :], scalar=4, op=ALU.logical_shift_right), th_cpos + th_cposb, [t_cpi])
                    dve(lambda e: e.tensor_single_scalar(out=cpi[:, 1, :, :], in_=cpos[:, :, :], scalar=15, op=ALU.bitwise_and), th_cpos + th_cposb, [t_cpi])
                    dve(lambda e: e.tensor_copy(out=cpf[:, :, :, :], in_=cpi[:, :, :, :]), [t_cpi], [t_cpf])
                    i4 = idxf[:, :, :].rearrange("p (h two) k -> p h two k", two=2)
                    yield
                    for w in range(2):
                        dve(lambda e, w=w: e.tensor_tensor(out=ohs[w][:, :, :, :], in0=cpf[:, w, :, :].unsqueeze(3).to_broadcast([128, 8, 16, 16]),
                                                           in1=iota16.unsqueeze(1).unsqueeze(1).to_broadcast([128, 8, 16, 16]), op=ALU.is_equal),
                            [t_cpf, t_cst], [t_ohs[w]])
                        dve(lambda e, w=w, i4=i4: e.tensor_tensor(out=ohs[w][:, :, :, :], in0=ohs[w][:, :, :, :],
                                                                  in1=i4[:, :, w, :].unsqueeze(2).to_broadcast([128, 8, 16, 16]), op=ALU.mult),
                            [t_ohs[w], t_idxf], [t_ohs[w]])
                        dve(lambda e, w=w: e.reduce_sum(out=e12[:, w, :, :], in_=ohs[w][:, :, :, :], axis=AX.X), [t_ohs[w]], [t_e12])
                    yield
                    dve(lambda e: e.tensor_copy(out=rtm[:, 0:2, :], in_=e12[:, :, :, :].rearrange("p w h k -> p w (h k)")), [t_e12], [t_rtm])
                    dve(lambda e: e.tensor_copy(out=rtm[:, 2, :], in_=gt[:, :, :].rearrange("p h k -> p (h k)")), [t_gt], [t_rtm])
                    for w in range(3):
                        pe(lambda e, w=w: e.transpose(out=ps4b[:, w * 128:(w + 1) * 128], in_=rtm[:, w, :], identity=ident_bf[:, :]),
                           [t_rtm, t_ident], [pstok[4]])
                    act(lambda e, par=par: e.activation(out=rt[par][:, :, :].rearrange("p a b -> p (a b)"), in_=ps4b[:, 0:384], func=AF.Copy),
                        [pstok[4]], [t_rt[par]])
                    P.op("pool", lambda e, par=par, ci=ci: e.dma_start(out=rt_d[ci, :, :], in_=rt[par][:, :, :].rearrange("p a b -> p (a b)")),
                         reads=[t_rt[par]], writes=[], dsem=ds_rt[par])


                    yield

                def interleave(g1, g2):
                    gens = [g for g in (g1, g2) if g is not None]
                    while gens:
                        for g in list(gens):
                            try:
                                next(g)
                            except StopIteration:
                                gens.remove(g)

                interleave(front(0), None)
                for ci in range(NT):
                    interleave(back(ci), front(ci + 1) if ci + 1 < NT else None)

        def _sweep4():
            P.barrier()
            dve = lambda fn, r=(), w=(): P.op("dve", fn, r, w)
            act = lambda fn, r=(), w=(): P.op("act", fn, r, w)
            pe = lambda fn, r=(), w=(): P.op("pe", fn, r, w)
            pool = lambda fn, r=(), w=(): P.op("pool", fn, r, w)
            with contextlib.ExitStack() as s4:
                def sb4(name, shape, dt=F32):
                    return s4.enter_context(nc.sbuf_tensor(name, list(shape), dt))
                NS = 256
                fgb = sb4("fgb", [128, 1024]); t_fgb = Tok(); ds_s4b = P.dsem(group=True)
                P.op("sp", lambda e: e.dma_start(out=fgb[:, :], in_=fg_d.partition_broadcast(128)), writes=[t_fgb], dsem=ds_s4b)
                iob = sb4("iob", [128, 128], BF16); t_iob = Tok()
                dve(lambda e: e.tensor_copy(out=iob[:, :], in_=cf("iota")), [t_cst], [t_iob])
                GT = [sb4("GT%d" % i, [128, 128, NS], BF16) for i in range(2)]; t_GT = [Tok(), Tok()]
                h2s = [sb4("h2s%d" % i, [128, 8, NS], BF16) for i in range(2)]; t_h2s = [Tok(), Tok()]; ds_h2s = [P.dsem(), P.dsem()]
                rts = [sb4("rts_sb%d" % i, [128, 2, 3, 128], BF16) for i in range(2)]; t_rts = [Tok(), Tok()]; ds_rts = [P.dsem(), P.dsem()]
                NR = 8
                XA = sb4("XA", [128, NR, 128], BF16); XB = sb4("XB", [128, NR, 128], BF16)
                t_XA = [Tok() for _ in range(NR)]; t_XB = [Tok() for _ in range(NR)]
                Ug = [sb4("Ug%d" % i, [128, 8, 512], BF16) for i in range(2)]; t_Ug = [Tok(), Tok()]; ds_Ug = [P.dsem(), P.dsem()]
                Vg = [sb4("Vg%d" % i, [128, 4, 1024], BF16) for i in range(2)]; t_Vg = [Tok(), Tok()]; ds_Vg = [P.dsem(), P.dsem()]
                aa = [sb4("aa%d" % i, [128, NS], BF16) for i in range(2)]; t_aa = [Tok(), Tok()]
                a2 = [sb4("a2%d" % i, [128, NS], BF16) for i in range(2)]; t_a2 = [Tok(), Tok()]
                x1t = [sb4("x1t%d" % i, [128, 1024]) for i in range(2)]; t_x1t = [Tok(), Tok()]; ds_x1t = [P.dsem(), P.dsem()]
                st = sb4("st_4", [128, 16]); t_st = Tok()
                ot = [sb4("ot%d" % i, [128, 1024]) for i in range(2)]; t_ot = [Tok(), Tok()]; ds_ot = [P.dsem(), P.dsem()]
                nslab = NT // 2
                grp_ctr = [0]

                pending = []

                def gbuild(sl, ta, tb, defer=False):
                    q = sl % 2
                    if ta == 0:
                        for c in range(2):
                            ci = sl * 2 + c
                            P.op("sp", lambda e, ci=ci, c=c, q=q: e.dma_start(out=h2s[q][:, :, c * 128:(c + 1) * 128],
                                                                              in_=h2T_d[ci, :, :].rearrange("p (k t) -> p k t", k=8)),
                                 writes=[t_h2s[q]], dsem=ds_h2s[q])
                            P.op("sp", lambda e, ci=ci, c=c, q=q: e.dma_start(out=rts[q][:, c, :, :], in_=rt_d[ci, :, :].rearrange("p (w t) -> p w t", w=3)),
                                 writes=[t_rts[q]], dsem=ds_rts[q])
                    for t in range(ta, tb):
                        c, tt = t // 128, t % 128
                        slot = t % NR
                        gb = 6 + (t // 4) % 2
                        dve(lambda e, c=c, tt=tt, slot=slot, q=q: e.tensor_scalar(out=XA[:, slot, :], in0=iob[:, :], scalar1=rts[q][:, c, 0, tt:tt + 1],
                                                                                  scalar2=rts[q][:, c, 2, tt:tt + 1], op0=ALU.is_equal, op1=ALU.mult),
                            [t_iob, t_rts[q]], [t_XA[slot]])
                        dve(lambda e, c=c, tt=tt, slot=slot, q=q: e.tensor_scalar(out=XB[:, slot, :], in0=iob[:, :], scalar1=rts[q][:, c, 1, tt:tt + 1],
                                                                                  scalar2=None, op0=ALU.is_equal),
                            [t_iob, t_rts[q]], [t_XB[slot]])
                        pe(lambda e, slot=slot, gb=gb, t=t: e.matmul(out=ps[:, gb, (t % 4) * 128:(t % 4 + 1) * 128], lhsT=XB[:, slot, :], rhs=XA[:, slot, :],
                                                                     start=True, stop=True),
                           [t_XA[slot], t_XB[slot]], [pstok[gb]])
                        if t % 4 == 3:
                            t0 = t - 3

                            def evac(gb=gb, t0=t0, q=q):
                                act(lambda e: e.activation(out=GT[q][:, :, t0:t0 + 4], in_=ps[:, gb, :].rearrange("p (t e) -> p e t", t=4), func=AF.Copy),
                                    [pstok[gb]], [t_GT[q]])
                            if defer:
                                pending.append(evac)
                            else:
                                evac()

                def load_grp(grp):
                    ub = grp_ctr[0] % 2
                    grp_ctr[0] += 1
                    P.op("sp", lambda e, grp=grp, ub=ub: e.dma_start(out=Ug[ub][:, :, :], in_=UTb_d[grp, :, :, :]), writes=[t_Ug[ub]], dsem=ds_Ug[ub])
                    P.op("sp", lambda e, grp=grp, ub=ub: e.dma_start(out=Vg[ub][:, :, :], in_=Vb_d[grp, :, :, :]), writes=[t_Vg[ub]], dsem=ds_Vg[ub])
                    return ub

                gbuild(0, 0, NS)
                for sl in range(nslab):
                    q = sl % 2
                    ubs = {}

                    def scores(eb, q=q, ubs=ubs):
                        grp, b = eb // 4, eb % 4
                        if b == 0:
                            ubs[grp] = load_grp(grp)
                        ub = ubs[grp]
                        sb_ = 4 + eb % 2
                        for k in range(8):
                            pe(lambda e, k=k, b=b, ub=ub, sb_=sb_, q=q: e.matmul(out=ps[:, sb_, 0:NS], lhsT=Ug[ub][:, k, b * 128:(b + 1) * 128], rhs=h2s[q][:, k, :],
                                                                                 start=(k == 0), stop=(k == 7)),
                               [t_Ug[ub], t_h2s[q]], [pstok[sb_]])

                    scores(0)
                    for eb in range(128):
                        grp, b = eb // 4, eb % 4
                        ub = ubs[grp]
                        sb_ = 4 + eb % 2
                        ab = eb % 2
                        if eb + 1 < 128:
                            scores(eb + 1)
                        act(lambda e, sb_=sb_, ab=ab: e.activation(out=aa[ab][:, :], in_=ps[:, sb_, 0:NS], func=AF.Gelu), [pstok[sb_]], [t_aa[ab]])
                        while pending:
                            pending.pop(0)()
                        dve(lambda e, ab=ab, eb=eb, q=q: e.tensor_tensor(out=a2[ab][:, :], in0=aa[ab][:, :], in1=GT[q][:, eb, :], op=ALU.mult),
                            [t_aa[ab], t_GT[q]], [t_a2[ab]])
                        for i in range(2):
                            for j in range(2):
                                pe(lambda e, i=i, j=j, ab=ab, b=b, ub=ub, eb=eb: e.matmul(out=ps[:, i * 2 + j, :], lhsT=a2[ab][:, i * 128:(i + 1) * 128],
                                                                                         rhs=Vg[ub][:, b, j * 512:(j + 1) * 512],
                                                                                         start=(eb == 0), stop=(eb == 127)),
                                   [t_a2[ab], t_Vg[ub]], [pstok[i * 2 + j]])
                        if sl + 1 < nslab:
                            gbuild(sl + 1, 2 * eb, 2 * eb + 2, defer=True)
                    while pending:
                        pending.pop(0)()
                    for i in range(2):
                        ci = sl * 2 + i
                        r0 = ci * 128
                        P.op("sp", lambda e, i=i, r0=r0: e.dma_start(out=x1t[i][:, :], in_=x1_d[r0:r0 + 128, :]), writes=[t_x1t[i]], dsem=ds_x1t[i])
                        dve(lambda e, i=i: e.tensor_tensor(out=x1t[i][:, :], in0=ps[:, 2 * i:2 * i + 2, :].rearrange("p a b -> p (a b)"), in1=x1t[i][:, :], op=ALU.add),
                            [pstok[2 * i], pstok[2 * i + 1], t_x1t[i]], [t_x1t[i]])
                        act(lambda e, i=i: e.activation(out=ot[i][:, :], in_=x1t[i][:, :], func=AF.Square, accum_out=st[:, 0:1]), [t_x1t[i]], [t_ot[i], t_st])
                        dve(lambda e: e.tensor_scalar(out=st[:, 1:2], in0=st[:, 0:1], scalar1=1.0 / D, scalar2=EPS, op0=ALU.mult, op1=ALU.add), [t_st], [t_st])
                        rsqrt(st[:, 2:3], st[:, 1:2], [t_st], [t_st])
                        dve(lambda e, i=i: e.scalar_tensor_tensor(out=ot[i][:, :], in0=x1t[i][:, :], scalar=st[:, 2:3], in1=fgb[:, :], op0=ALU.mult, op1=ALU.mult),
                            [t_x1t[i], t_st, t_fgb], [t_ot[i]])
                        finals.append(P.op("pool", lambda e, i=i, r0=r0: e.dma_start(out=out_d[r0:r0 + 128, :], in_=ot[i][:, :]),
                                           reads=[t_ot[i]], writes=[], dsem=ds_ot[i]))

        for _k, _f in ((1, _sweep1), (2, _sweep2), (3, _sweep3), (4, _sweep4)):
            if _k in sweeps:
                _f()
        P.emit(final_waits=finals)
    nc.input_names = in_names
    return nc


def prep_shared(inp):
    hc = host_consts()
    f = lambda a: np.ascontiguousarray(np.asarray(a, dtype=np.float32))
    sh = {
        "w_in": f(inp["w_in"][0]),
        "norm1_gc": f(inp["norm1_g"][0].reshape(8, 128).T),
        "gmlp_ln_g": f(inp["gmlp_ln_g"][0]),
        "gmlp_ln_b": f(inp["gmlp_ln_b"][0]),
        "gmlp_wsT": f(inp["gmlp_ws"][0].transpose(2, 0, 1)),
        "gmlp_bsT": f(inp["gmlp_bs"][0].T),
        "consts": f(np.concatenate([hc[n] for n in CONST_NAMES], axis=1)),
        "conv_w": f(inp["conv_w"][0].reshape(-1)),
        "conv_b": f(inp["conv_b"][0].reshape(1, -1)),
        "dt_bias": f(inp["dt_bias"][0]),
        "a_log": f(inp["a_log"][0]),
        "d_skip": f(inp["d_skip"][0]),
        "w_out": f(inp["w_out"][0]),
        "gcat": f(np.concatenate([inp["gmlp_out_g"][0], inp["ssd_norm_g"][0]]).reshape(16, 128).T),
        "w_queryT": f(inp["w_query"][0].T),
        "sub_keysT": f(np.stack([inp["sub_keys1"][0].T, inp["sub_keys2"][0].T], axis=1)),
        "norm2_gc": f(inp["norm2_g"][0].reshape(8, 128).T),
    }
    return sh


def prep_experts(inp):
    f = lambda a: np.ascontiguousarray(np.asarray(a, dtype=np.float32))
    return {
        "expert_uT": f(np.asarray(inp["expert_u"][0]).T),
        "expert_v": f(inp["expert_v"][0]),
        "final_g": f(inp["final_g"]),
    }


_NC_CACHE = {}


def kernel(**inputs):
    x = np.asarray(inputs["x"], dtype=np.float32)
    B, L, _ = x.shape
    nseq = B // NCORES
    shared = prep_shared(inputs)
    shared.update(prep_experts(inputs))
    key = (nseq, L)
    if key not in _NC_CACHE:
        _NC_CACHE[key] = build(NSEQ=nseq, L=L)
    nc = _NC_CACHE[key]
    in_maps = []
    for c in range(NCORES):
        m = dict(shared)
        m["x"] = np.ascontiguousarray(x[c * nseq:(c + 1) * nseq].reshape(nseq * L, D))
        in_maps.append(m)
    res = run_bass_kernel_spmd(nc, in_maps, core_ids=list(range(NCORES)))
    outs = [np.asarray(res.results[c]["out"], dtype=np.float32).reshape(nseq, L, D) for c in range(NCORES)]
    return np.concatenate(outs, axis=0)
```

```python
import contextlib
import numpy as np
import ml_dtypes
import concourse.bass as bass
import concourse.mybir as mybir
from concourse.bass_utils import run_bass_kernel_spmd

F32 = mybir.dt.float32
BF16 = mybir.dt.bfloat16
U32 = mybir.dt.uint32
ALU = mybir.AluOpType
AF = mybir.ActivationFunctionType
AX = mybir.AxisListType

D = 1024
GMW = 1024
SSDW = 1024
NH = 16
HP = 64
NST = 128
CONVD = 1536
DIN = 4624
NEXP = 16384
EPS = 1e-6
NCORES = 8


class Tok:
    __slots__ = ("last_write", "reads", "name")

    def __init__(self, name=""):
        self.last_write = None
        self.reads = []
        self.name = name


class Op:
    __slots__ = ("eng", "fn", "deps", "signal", "seq", "dsem", "val")

    def __init__(self, eng, fn):
        self.eng = eng
        self.fn = fn
        self.deps = []
        self.signal = False
        self.seq = 0
        self.dsem = None
        self.val = 0


class DSem:
    def __init__(self, handle, group=False):
        self.handle = handle
        self.count = 0
        self.group = group


class Prog:
    ENGS = ("pe", "act", "dve", "pool", "sp")

    def __init__(self, nc, stack):
        self.nc = nc
        self.stack = stack
        self.ops = {e: [] for e in self.ENGS}
        self.esem = {e: stack.enter_context(nc.semaphore("es_" + e)) for e in self.ENGS}
        self.nsem = 0
        self.dmas = []

    def dsem(self, name=None, group=False):
        self.nsem += 1
        return DSem(self.stack.enter_context(self.nc.semaphore(name or ("ds%d" % self.nsem))), group)

    def op(self, eng, fn, reads=(), writes=(), dsem=None):
        if dsem is not None and dsem.group:
            dsem = self.dsem()
        o = Op(eng, fn)
        deps = []
        for t in reads:
            if t.last_write is not None:
                deps.append(t.last_write)
        for t in writes:
            if t.last_write is not None:
                deps.append(t.last_write)
            deps.extend(t.reads)
        o.deps = deps
        for d in deps:
            d.signal = True
        for t in reads:
            t.reads = [r for r in t.reads if not (r.eng == eng and r.dsem is None and dsem is None)]
            t.reads.append(o)
        for t in writes:
            t.last_write = o
            t.reads = []
        if dsem is not None:
            o.dsem = dsem
            dsem.count += 16
            o.val = dsem.count
        self.ops[eng].append(o)
        if dsem is not None:
            self.dmas.append(o)
        return o

    def barrier(self):
        lasts = [self.ops[e][-1] for e in self.ENGS if self.ops[e] and self.ops[e][-1].dsem is None]
        for e in self.ENGS:
            prev = [o for o in reversed(self.ops[e]) if o.dsem is None][:1]
            lasts_e = prev
        lasts = []
        for e in self.ENGS:
            for o in reversed(self.ops[e]):
                if o.dsem is None:
                    lasts.append(o)
                    break
        deps = lasts + list(self.dmas)
        self.dmas = []
        for e in self.ENGS:
            o = Op(e, lambda eng: eng.nop())
            o.deps = list(deps)
            for d in deps:
                d.signal = True
            self.ops[e].append(o)

    def emit(self, final_waits=()):
        nc = self.nc
        for e in self.ENGS:
            n = 0
            for o in self.ops[e]:
                if o.dsem is None and o.signal:
                    n += 1
                    o.seq = n
        esem = self.esem

        def run(ename, eng):
            waited = {}
            for o in self.ops[ename]:
                need = {}
                for d in o.deps:
                    if d.dsem is not None:
                        key = ("d", id(d.dsem))
                        h, v = d.dsem.handle, d.val
                    else:
                        if d.eng == ename and ename == "pe":
                            continue
                        key = ("e", d.eng)
                        h, v = esem[d.eng], d.seq
                    if waited.get(key, 0) >= v:
                        continue
                    if key not in need or need[key][1] < v:
                        need[key] = (h, v)
                for key, (h, v) in need.items():
                    eng.wait_ge(h, v)
                    waited[key] = v
                ins = o.fn(eng)
                if o.dsem is not None:
                    ins.then_inc(o.dsem.handle, 16)
                elif o.signal:
                    ins.then_inc(esem[ename], 1)
            if ename == "sp":
                for o in final_waits:
                    if o.dsem is not None:
                        eng.wait_ge(o.dsem.handle, o.val)
                    else:
                        eng.wait_ge(esem[o.eng], o.seq)

        for o in final_waits:
            o.signal = True
        for e in self.ENGS:
            n = 0
            for o in self.ops[e]:
                if o.dsem is None and o.signal:
                    n += 1
                    o.seq = n
        with nc.Block() as block:
            @block.sync
            def _(eng):
                run("sp", eng)

            @block.scalar
            def _(eng):
                run("act", eng)

            @block.vector
            def _(eng):
                run("dve", eng)

            @block.gpsimd
            def _(eng):
                run("pool", eng)

            @block.tensor
            def _(eng):
                run("pe", eng)


def host_consts():
    c = {}
    c["ident"] = np.eye(128, dtype=np.float32)
    s = np.arange(128)[:, None]
    t = np.arange(128)[None, :]
    c["triu"] = (s <= t).astype(np.float32)
    for k in range(3):
        sh = 3 - k
        c["shift%d" % k] = (s == t - sh).astype(np.float32)
        c["shiftp%d" % k] = (s == t - sh + 128).astype(np.float32)
    c["ones"] = np.ones((128, 128), dtype=np.float32)
    c["gt"] = (s > t).astype(np.float32)
    c["iota"] = np.broadcast_to(np.arange(128, dtype=np.float32)[None, :], (128, 128)).copy()
    return c


CONST_NAMES = ["ident", "triu", "shift0", "shift1", "shift2", "shiftp0", "shiftp1", "shiftp2", "iota", "ones", "gt"]


def build(NSEQ=4, L=2048, sweeps=(1, 2, 3, 4), debug=False):
    NCH = L // 128
    T = NSEQ * L
    NT = T // 128
    nc = bass.Bass("TRN2", target_bir_lowering=False)
    scratch_kind = "ExternalOutput" if debug else "Internal"

    in_names = []

    def din(name, shape, dt=F32):
        in_names.append(name)
        return nc.dram_tensor(name, list(shape), dt, kind="ExternalInput").ap()

    x_d = din("x", [T, D])
    w_in_d = din("w_in", [D, DIN])
    g1_d = din("norm1_gc", [128, 8])
    lng_d = din("gmlp_ln_g", [GMW])
    lnb_d = din("gmlp_ln_b", [GMW])
    wsT_d = din("gmlp_wsT", [128, 8, 128])
    bsT_d = din("gmlp_bsT", [128, 8])
    consts_d = din("consts", [128, len(CONST_NAMES) * 128])
    convw_d = din("conv_w", [4 * CONVD])
    convb_d = din("conv_b", [1, CONVD])
    dtb_d = din("dt_bias", [NH])
    alog_d = din("a_log", [NH])
    dskip_d = din("d_skip", [NH])
    if 3 in sweeps or 4 in sweeps:
        wout_d = din("w_out", [2048, D])
        gcat_d = din("gcat", [128, 16])
        wqT_d = din("w_queryT", [2048, D])
        kT_d = din("sub_keysT", [128, 2, 128])
        g2c_d = din("norm2_gc", [128, 8])
        if 4 in sweeps:
            uT_d = din("expert_uT", [D, NEXP])
            ev_d = din("expert_v", [NEXP, D])
            fg_d = din("final_g", [D])
            UTb_d = nc.dram_tensor("UTb", [32, 128, 8, 512], BF16, kind="Internal").ap()
            Vb_d = nc.dram_tensor("Vb", [32, 128, 4, 1024], BF16, kind="Internal").ap()
        x1_d = nc.dram_tensor("x1s", [T, D], F32, kind=scratch_kind).ap()
        h2T_d = nc.dram_tensor("h2Ts", [NT, 128, 1024], BF16, kind=scratch_kind).ap()
        rt_d = nc.dram_tensor("rts", [NT, 128, 384], BF16, kind=scratch_kind).ap()
    cat_d = nc.dram_tensor("cat", [T, 2048], BF16, kind=scratch_kind).ap()
    out_d = nc.dram_tensor("out", [T, D], F32, kind="ExternalOutput").ap()

    with contextlib.ExitStack() as stack:
        P = Prog(nc, stack)

        def sb(name, shape, dt=F32):
            return stack.enter_context(nc.sbuf_tensor(name, list(shape), dt))

        ps = stack.enter_context(nc.psum_tensor("ps", [128, 8, 512], F32))
        pstok = [Tok("ps%d" % i) for i in range(8)]

        finals = []
        cst = sb("cst", [128, len(CONST_NAMES) * 128])
        t_cst = Tok()
        ds_c = P.dsem()
        P.op("sp", lambda e: e.dma_start(out=cst[:, :], in_=consts_d[:, :]), writes=[t_cst], dsem=ds_c)

        def cf(name):
            i = CONST_NAMES.index(name)
            return cst[:, i * 128:(i + 1) * 128]

        ident_bf = sb("ident_bf", [128, 128], BF16)
        t_ident = Tok()
        P.op("dve", lambda e: e.tensor_copy(out=ident_bf[:, :], in_=cf("ident")), reads=[t_cst], writes=[t_ident])

        nhalf = sb("nhalf", [128, 16])
        t_nhalf = Tok()
        P.op("pool", lambda e: e.memset(nhalf[:, :], -0.5), writes=[t_nhalf])

        def rsqrt(out_ap, in_ap, toks_r, toks_w):
            n = in_ap.shape[-1]
            return P.op("pool", lambda e: e.tensor_tensor(out=out_ap, in0=in_ap, in1=nhalf[:, 0:n], op=ALU.pow),
                        reads=list(toks_r) + [t_nhalf], writes=list(toks_w))

        ds_stg = P.dsem()

        def prepass_gen(sbx):
            dve = lambda fn, r=(), w=(): P.op("dve", fn, r, w)
            act = lambda fn, r=(), w=(): P.op("act", fn, r, w)
            g2c4 = sbx("g2c4", [128, 8]); t_g2c4 = Tok(); ds_s4 = P.dsem(group=True)
            P.op("sp", lambda e: e.dma_start(out=g2c4[:, :], in_=g2c_d[:, :]), writes=[t_g2c4], dsem=ds_s4)
            cin = [sbx("cin%d" % i, [128, 4096]) for i in range(2)]; t_cin = [Tok(), Tok()]; ds_cin = [P.dsem(), P.dsem()]
            cout = [sbx("cout%d" % i, [128, 4096], BF16) for i in range(2)]; t_cout = [Tok(), Tok()]; ds_cout = [P.dsem(), P.dsem()]
            uT_v = uT_d.rearrange("(k p) e -> p k e", p=128)
            ev_v = ev_d.rearrange("(g b p) d -> g p b d", b=4, p=128)
            for it in range(64):
                bb = it % 2
                g = it // 2
                if it % 2 == 0:
                    P.op("sp", lambda e, g=g, bb=bb: e.dma_start(out=cin[bb][:, :].rearrange("p (k e) -> p k e", k=8),
                                                                 in_=uT_v[:, :, g * 512:(g + 1) * 512]),
                         writes=[t_cin[bb]], dsem=ds_cin[bb])
                    dve(lambda e, bb=bb: e.tensor_tensor(out=cout[bb][:, :].rearrange("p (k e) -> p k e", k=8),
                                                         in0=cin[bb][:, :].rearrange("p (k e) -> p k e", k=8),
                                                         in1=g2c4[:, :].unsqueeze(2).to_broadcast([128, 8, 512]), op=ALU.mult),
                        [t_cin[bb], t_g2c4], [t_cout[bb]])
                    P.op("pool", lambda e, g=g, bb=bb: e.dma_start(out=UTb_d[g, :, :, :].rearrange("p k e -> p (k e)"), in_=cout[bb][:, :]),
                         reads=[t_cout[bb]], writes=[], dsem=ds_cout[bb])
                else:
                    P.op("sp", lambda e, g=g, bb=bb: e.dma_start(out=cin[bb][:, :].rearrange("p (b d) -> p b d", b=4), in_=ev_v[g, :, :, :]),
                         writes=[t_cin[bb]], dsem=ds_cin[bb])
                    act(lambda e, bb=bb: e.activation(out=cout[bb][:, :], in_=cin[bb][:, :], func=AF.Copy), [t_cin[bb]], [t_cout[bb]])
                    P.op("pool", lambda e, g=g, bb=bb: e.dma_start(out=Vb_d[g, :, :, :].rearrange("p b d -> p (b d)"), in_=cout[bb][:, :]),
                         reads=[t_cout[bb]], writes=[], dsem=ds_cout[bb])
                yield

        def _sweep1():
            with contextlib.ExitStack() as s1:
                def sb1(name, shape, dt=F32):
                    return s1.enter_context(nc.sbuf_tensor(name, list(shape), dt))

                stg = sb1("stg_1", [128, 2576]); t_stg = Tok()

                w1 = sb1("w1", [128, 8, 2048], BF16)
                t_w1 = Tok()
                g1c = sb1("g1c", [128, 8])
                t_g1c = Tok()
                ds_small = P.dsem(group=True)
                P.op("sp", lambda e: e.dma_start(out=g1c[:, :], in_=g1_d[:, :]),
                     writes=[t_g1c], dsem=ds_small)
                for k in range(8):
                    P.op("sp", lambda e, k=k: e.dma_start(out=stg[:, 0:2048], in_=w_in_d[k * 128:(k + 1) * 128, 0:2048]),
                         writes=[t_stg], dsem=ds_stg)
                    P.op("dve", lambda e, k=k: e.tensor_scalar(out=w1[:, k, :], in0=stg[:, 0:2048], scalar1=g1c[:, k:k + 1],
                                                               scalar2=None, op0=ALU.mult),
                         reads=[t_stg, t_g1c], writes=[t_w1])
                WT = sb1("WT", [128, 8, 128], BF16)
                t_WT = Tok()
                P.op("sp", lambda e: e.dma_start(out=stg[:, 0:1024], in_=wsT_d.rearrange("s h t -> s (h t)")),
                     writes=[t_stg], dsem=ds_stg)
                P.op("dve", lambda e: e.tensor_tensor(out=WT[:, :, :], in0=stg[:, 0:1024].rearrange("s (h t) -> s h t", h=8),
                                                      in1=cf("triu").unsqueeze(1).to_broadcast([128, 8, 128]), op=ALU.mult),
                     reads=[t_stg, t_cst], writes=[t_WT])
                bsT = sb1("bsT", [128, 8])
                t_bsT = Tok()
                P.op("sp", lambda e: e.dma_start(out=bsT[:, :], in_=bsT_d[:, :]), writes=[t_bsT], dsem=ds_small)
                lngb = sb1("lngb", [128, 1024])
                lnbb = sb1("lnbb", [128, 1024])
                t_ln = Tok()
                P.op("sp", lambda e: e.dma_start(out=lngb[:, :], in_=lng_d.partition_broadcast(128)), writes=[t_ln], dsem=ds_small)
                P.op("sp", lambda e: e.dma_start(out=lnbb[:, :], in_=lnb_d.partition_broadcast(128)), writes=[t_ln], dsem=ds_small)

                NB = 2
                xt = [sb1("xt%d" % i, [128, 1024]) for i in range(NB)]
                t_xt = [Tok() for _ in range(NB)]
                ds_xt = [P.dsem() for _ in range(NB)]
                junk = sb1("junk", [128, 1024], BF16)
                t_junk = Tok()
                st = sb1("st", [128, 16])
                t_st = Tok()
                hb = sb1("hb", [128, 1024], BF16)
                t_hb = Tok()
                hT = sb1("hT", [128, 8, 128], BF16)
                t_hT = Tok()
                vtmp = sb1("vtmp", [128, 1024])
                t_vtmp = Tok()
                vb = sb1("vb", [128, 1024], BF16)
                t_vb = Tok()
                gm = sb1("gm", [128, 1024])
                t_gm = Tok()
                gmn = [sb1("gmn%d" % i, [128, 1024], BF16) for i in range(NB)]
                t_gmn = [Tok() for _ in range(NB)]
                ds_gmn = [P.dsem() for _ in range(NB)]

                ug2 = [sb1("ug2_%d" % i, [128, 1024]) for i in range(2)]; t_ug2 = [Tok(), Tok()]
                vg2 = [sb1("vg2_%d" % i, [128, 1024]) for i in range(2)]; t_vg2 = [Tok(), Tok()]
                stb = [sb1("stb_%d" % i, [128, 16]) for i in range(2)]; t_stb = [Tok(), Tok()]

                def front(ci):
                    r0 = ci * 128
                    b = ci % NB
                    P.op("sp", lambda e, b=b, r0=r0: e.dma_start(out=xt[b][:, :], in_=x_d[r0:r0 + 128, :]),
                         writes=[t_xt[b]], dsem=ds_xt[b])
                    P.op("act", lambda e, b=b: e.activation(out=junk[:, :], in_=xt[b][:, :], func=AF.Square, accum_out=st[:, 0:1]),
                         reads=[t_xt[b]], writes=[t_junk, t_st])
                    P.op("dve", lambda e: e.tensor_scalar(out=st[:, 1:2], in0=st[:, 0:1], scalar1=1.0 / D, scalar2=EPS,
                                                          op0=ALU.mult, op1=ALU.add), reads=[t_st], writes=[t_st])
                    rsqrt(st[:, 2:3], st[:, 1:2], [t_st], [t_st])
                    P.op("act", lambda e, b=b: e.activation(out=hb[:, :], in_=xt[b][:, :], func=AF.Copy, scale=st[:, 2:3]),
                         reads=[t_xt[b], t_st], writes=[t_hb])
                    yield
                    psT = ps[:, 7, :].bitcast(BF16)
                    for k in range(8):
                        P.op("pe", lambda e, k=k, psT=psT: e.transpose(out=psT[:, k * 128:(k + 1) * 128], in_=hb[:, k * 128:(k + 1) * 128],
                                                                       identity=ident_bf[:, :]),
                             reads=[t_hb, t_ident], writes=[pstok[7]])
                    P.op("dve", lambda e, psT=psT: e.tensor_copy(out=hT[:, :, :].rearrange("p k t -> p (k t)"), in_=psT[:, :]),
                         reads=[pstok[7]], writes=[t_hT])
                    yield
                    for j in range(4):
                        for k in range(8):
                            P.op("pe", lambda e, j=j, k=k: e.matmul(out=ps[:, j, :], lhsT=hT[:, k, :], rhs=w1[:, k, j * 512:(j + 1) * 512],
                                                                    start=(k == 0), stop=(k == 7)),
                                 reads=[t_hT, t_w1], writes=[pstok[j]])
                    yield
                    P.op("act", lambda e, b=b: e.activation(out=ug2[b][:, :], in_=ps[:, 0:2, :].rearrange("p a b -> p (a b)"), func=AF.Gelu),
                         reads=[pstok[0], pstok[1]], writes=[t_ug2[b]])
                    P.op("act", lambda e, b=b: e.activation(out=vg2[b][:, :], in_=ps[:, 2:4, :].rearrange("p a b -> p (a b)"), func=AF.Gelu,
                                                       accum_out=stb[b][:, 3:4]),
                         reads=[pstok[2], pstok[3]], writes=[t_vg2[b], t_stb[b]])
                    yield

                def back(ci):
                    r0 = ci * 128
                    b = ci % NB
                    P.op("act", lambda e, b=b: e.activation(out=junk[:, :], in_=vg2[b][:, :], func=AF.Square, accum_out=stb[b][:, 4:5]),
                         reads=[t_vg2[b]], writes=[t_junk, t_stb[b]])
                    P.op("dve", lambda e, b=b: e.tensor_scalar(out=stb[b][:, 5:6], in0=stb[b][:, 3:4], scalar1=1.0 / GMW, scalar2=None, op0=ALU.mult),
                         reads=[t_stb[b]], writes=[t_stb[b]])
                    P.op("dve", lambda e, b=b: e.tensor_tensor(out=stb[b][:, 6:7], in0=stb[b][:, 5:6], in1=stb[b][:, 5:6], op=ALU.mult),
                         reads=[t_stb[b]], writes=[t_stb[b]])
                    P.op("dve", lambda e, b=b: e.scalar_tensor_tensor(out=stb[b][:, 7:8], in0=stb[b][:, 4:5], scalar=1.0 / GMW, in1=stb[b][:, 6:7],
                                                                 op0=ALU.mult, op1=ALU.subtract),
                         reads=[t_stb[b]], writes=[t_stb[b]])
                    P.op("dve", lambda e, b=b: e.tensor_scalar(out=stb[b][:, 12:13], in0=stb[b][:, 7:8], scalar1=EPS, scalar2=None,
                                                          op0=ALU.add), reads=[t_stb[b]], writes=[t_stb[b]])
                    rsqrt(stb[b][:, 8:9], stb[b][:, 12:13], [t_stb[b]], [t_stb[b]])
                    P.op("dve", lambda e, b=b: e.scalar_tensor_tensor(out=vtmp[:, :], in0=vg2[b][:, :], scalar=stb[b][:, 5:6], in1=lngb[:, :],
                                                                 op0=ALU.subtract, op1=ALU.mult),
                         reads=[t_vg2[b], t_stb[b], t_ln], writes=[t_vtmp])
                    P.op("dve", lambda e, b=b: e.scalar_tensor_tensor(out=vb[:, :], in0=vtmp[:, :], scalar=stb[b][:, 8:9], in1=lnbb[:, :],
                                                                 op0=ALU.mult, op1=ALU.add),
                         reads=[t_vtmp, t_stb[b], t_ln], writes=[t_vb])
                    yield
                    for h in range(8):
                        P.op("pe", lambda e, h=h: e.matmul(out=ps[:, 4 + h // 4, (h % 4) * 128:(h % 4 + 1) * 128], lhsT=WT[:, h, :],
                                                           rhs=vb[:, h * 128:(h + 1) * 128], start=True, stop=True),
                             reads=[t_WT, t_vb], writes=[pstok[4 + h // 4]])
                    P.op("dve", lambda e, b=b: e.tensor_tensor(out=gm[:, :].rearrange("p (h d) -> p h d", h=8),
                                                          in0=ps[:, 4:6, :].rearrange("p a (h d) -> p (a h) d", d=128),
                                                          in1=bsT[:, :].unsqueeze(2).to_broadcast([128, 8, 128]), op=ALU.add),
                         reads=[pstok[4], pstok[5], t_bsT], writes=[t_gm])
                    yield
                    P.op("dve", lambda e, b=b: e.tensor_tensor(out=gm[:, :], in0=gm[:, :], in1=ug2[b][:, :], op=ALU.mult),
                         reads=[t_gm, t_ug2[b]], writes=[t_gm])
                    P.op("act", lambda e, b=b: e.activation(out=junk[:, :], in_=gm[:, :], func=AF.Square, accum_out=stb[b][:, 9:10]),
                         reads=[t_gm], writes=[t_junk, t_stb[b]])
                    P.op("dve", lambda e, b=b: e.tensor_scalar(out=stb[b][:, 10:11], in0=stb[b][:, 9:10], scalar1=1.0 / GMW, scalar2=EPS,
                                                          op0=ALU.mult, op1=ALU.add), reads=[t_stb[b]], writes=[t_stb[b]])
                    rsqrt(stb[b][:, 11:12], stb[b][:, 10:11], [t_stb[b]], [t_stb[b]])
                    yield
                    P.op("act", lambda e, b=b: e.activation(out=gmn[b][:, :], in_=gm[:, :], func=AF.Copy, scale=stb[b][:, 11:12]),
                         reads=[t_gm, t_stb[b]], writes=[t_gmn[b]])
                    finals.append(P.op("pool", lambda e, b=b, r0=r0: e.dma_start(out=cat_d[r0:r0 + 128, 0:1024], in_=gmn[b][:, :]),
                                       reads=[t_gmn[b]], writes=[], dsem=ds_gmn[b]))


                    yield

                def interleave(g1, g2):
                    gens = [g for g in (g1, g2) if g is not None]
                    while gens:
                        for g in list(gens):
                            try:
                                next(g)
                            except StopIteration:
                                gens.remove(g)

                pp = prepass_gen(sb1) if 4 in sweeps else None
                interleave(front(0), None)
                for ci in range(NT):
                    interleave(back(ci), front(ci + 1) if ci + 1 < NT else None)
                    if pp is not None:
                        for _ in range(-(-64 // NT)):
                            next(pp, None)
                if pp is not None:
                    for _ in pp:
                        pass

        def _sweep2():
            P.barrier()
            with contextlib.ExitStack() as s2:
                def sb2(name, shape, dt=F32):
                    return s2.enter_context(nc.sbuf_tensor(name, list(shape), dt))

                stg = sb2("stg_2", [128, 2576]); t_stg = Tok()

                dve = lambda fn, r=(), w=(): P.op("dve", fn, r, w)
                act = lambda fn, r=(), w=(): P.op("act", fn, r, w)
                pe = lambda fn, r=(), w=(): P.op("pe", fn, r, w)
                pool = lambda fn, r=(), w=(): P.op("pool", fn, r, w)

                ds_small2 = P.dsem(group=True)
                w2 = sb2("w2", [128, 8, 2576], BF16)
                t_w2 = Tok()
                g1c2 = sb2("g1c2", [128, 8])
                t_g1c2 = Tok()
                P.op("sp", lambda e: e.dma_start(out=g1c2[:, :], in_=g1_d[:, :]), writes=[t_g1c2], dsem=ds_small2)
                for k in range(8):
                    P.op("sp", lambda e, k=k: e.dma_start(out=stg[:, 0:2576], in_=w_in_d[k * 128:(k + 1) * 128, 2048:4624]),
                         writes=[t_stg], dsem=ds_stg)
                    dve(lambda e, k=k: e.tensor_scalar(out=w2[:, k, :], in0=stg[:, 0:2576], scalar1=g1c2[:, k:k + 1],
                                                       scalar2=None, op0=ALU.mult), [t_stg, t_g1c2], [t_w2])
                shb = sb2("shb", [128, 6, 128], BF16)
                t_shb = Tok()
                i0 = CONST_NAMES.index("shift0")
                dve(lambda e: e.tensor_copy(out=shb[:, :, :].rearrange("p a b -> p (a b)"), in_=cst[:, i0 * 128:(i0 + 6) * 128]),
                    [t_cst], [t_shb])
                onesb = sb2("onesb", [128, 128], BF16)
                t_onesb = Tok()
                dve(lambda e: e.tensor_copy(out=onesb[:, :], in_=cf("ones")), [t_cst], [t_onesb])
                convb = sb2("convb", [1, CONVD], BF16)
                t_convb = Tok()
                P.op("sp", lambda e: e.dma_start(out=stg[0:1, 0:CONVD], in_=convb_d[:, :]), writes=[t_stg], dsem=ds_stg)
                dve(lambda e: e.tensor_copy(out=convb[:, :], in_=stg[0:1, 0:CONVD]), [t_stg], [t_convb])
                convw = sb2("convw", [128, 4, CONVD])
                t_convw = Tok()
                P.op("sp", lambda e: e.dma_start(out=convw[:, :, :].rearrange("p k c -> p (k c)"), in_=convw_d.partition_broadcast(128)),
                     writes=[t_convw], dsem=ds_small2)
                sm = sb2("sm", [128, 4, 16])
                t_sm = Tok()
                P.op("sp", lambda e: e.dma_start(out=sm[:, 0, :], in_=dtb_d.partition_broadcast(128)), writes=[t_sm], dsem=ds_small2)
                P.op("sp", lambda e: e.dma_start(out=sm[:, 1, :], in_=alog_d.partition_broadcast(128)), writes=[t_sm], dsem=ds_small2)
                P.op("sp", lambda e: e.dma_start(out=sm[:, 2, :], in_=dskip_d.partition_broadcast(128)), writes=[t_sm], dsem=ds_small2)
                act(lambda e: e.activation(out=sm[:, 1, :], in_=sm[:, 1, :], func=AF.Exp), [t_sm], [t_sm])
                dve(lambda e: e.tensor_scalar(out=sm[:, 1, :], in0=sm[:, 1, :], scalar1=-1.0, scalar2=None, op0=ALU.mult), [t_sm], [t_sm])

                xt = sb2("xt_2", [128, 1024]); t_xt = Tok(); ds_xt = P.dsem()
                junkF = sb2("junkF_2", [128, 1024], BF16); t_junkF = Tok()
                junkB = sb2("junkB_2", [128, 512], BF16); t_junkB = Tok()
                stF = sb2("stF_2", [128, 4]); t_stF = Tok()
                stB = sb2("stB_2", [128, 16]); t_stB = Tok()
                hb = sb2("hb_2", [128, 1024], BF16); t_hb = Tok()
                hT = sb2("hT_2", [128, 8, 128], BF16); t_hT = Tok()
                zs = [sb2("zs%d" % i, [128, 1024]) for i in range(2)]; t_zs = [Tok(), Tok()]
                xw = [sb2("xw%d" % i, [128, 4, CONVD], BF16) for i in range(2)]; t_xw = [Tok(), Tok()]
                xs = [sb2("xs%d" % i, [128, 16, 64]) for i in range(2)]; t_xs = [Tok(), Tok()]
                bc = [sb2("bc%d" % i, [128, 4, 128], BF16) for i in range(2)]; t_bc = [Tok(), Tok()]
                bcT = [sb2("bcT%d" % i, [128, 4, 128], BF16) for i in range(2)]; t_bcT = [Tok(), Tok()]
                dF = [sb2("dF%d" % i, [128, 4, 16]) for i in range(2)]; t_dF = [Tok(), Tok()]
                dtv = sb2("dtv", [128, 8, 16]); t_dtv = Tok()
                R = sb2("R", [128, 16, 128]); t_R = Tok()
                LT = sb2("LT", [128, 16, 128], BF16); t_LT = Tok()
                cbm = sb2("cbm", [128, 2, 128], BF16); t_cbm = Tok()
                MT = sb2("MT", [128, 16, 128], BF16); t_MT = Tok()
                xdt = sb2("xdt", [128, 16, 64], BF16); t_xdt = Tok()
                xdtd = sb2("xdtd", [128, 16, 64], BF16); t_xdtd = Tok()
                stT = sb2("stT", [128, 16, 64]); t_stT = Tok()
                stTb = sb2("stTb", [128, 16, 64], BF16); t_stTb = Tok()
                yy = sb2("yy", [128, 16, 64]); t_yy = Tok()
                so = [sb2("so%d" % i, [128, 1024], BF16) for i in range(2)]; t_so = [Tok(), Tok()]
                ds_so = [P.dsem(), P.dsem()]
                ps3b = ps[:, 3, :].bitcast(BF16)
                flat = lambda ap: ap.rearrange("p a b -> p (a b)")

                def front(ci):
                    r0 = ci * 128
                    par = ci % 2
                    first = (ci % NCH == 0)
                    P.op("sp", lambda e, r0=r0: e.dma_start(out=xt[:, :], in_=x_d[r0:r0 + 128, :]), writes=[t_xt], dsem=ds_xt)
                    act(lambda e: e.activation(out=junkF[:, :], in_=xt[:, :], func=AF.Square, accum_out=stF[:, 0:1]), [t_xt], [t_junkF, t_stF])
                    dve(lambda e: e.tensor_scalar(out=stF[:, 1:2], in0=stF[:, 0:1], scalar1=1.0 / D, scalar2=EPS, op0=ALU.mult, op1=ALU.add),
                        [t_stF], [t_stF])
                    rsqrt(stF[:, 2:3], stF[:, 1:2], [t_stF], [t_stF])
                    act(lambda e: e.activation(out=hb[:, :], in_=xt[:, :], func=AF.Copy, scale=stF[:, 2:3]), [t_xt, t_stF], [t_hb])
                    yield
                    for k in range(8):
                        pe(lambda e, k=k: e.transpose(out=ps3b[:, k * 128:(k + 1) * 128], in_=hb[:, k * 128:(k + 1) * 128], identity=ident_bf[:, :]),
                           [t_hb, t_ident], [pstok[3]])
                    act(lambda e: e.activation(out=hT[:, :, :].rearrange("p k t -> p (k t)"), in_=ps3b[:, :], func=AF.Copy), [pstok[3]], [t_hT])
                    yield
                    for j in range(2):
                        for k in range(8):
                            pe(lambda e, j=j, k=k: e.matmul(out=ps[:, j, :], lhsT=hT[:, k, :], rhs=w2[:, k, j * 512:(j + 1) * 512],
                                                            start=(k == 0), stop=(k == 7)), [t_hT, t_w2], [pstok[j]])
                    for k in range(8):
                        pe(lambda e, k=k: e.matmul(out=ps[:, 2, 0:16], lhsT=hT[:, k, :], rhs=w2[:, k, 2560:2576],
                                                   start=(k == 0), stop=(k == 7)), [t_hT, t_w2], [pstok[2]])
                    yield
                    act(lambda e, par=par: e.activation(out=zs[par][:, :], in_=ps[:, 0:2, :].rearrange("p a b -> p (a b)"), func=AF.Silu),
                        [pstok[0], pstok[1]], [t_zs[par]])
                    dve(lambda e, par=par: e.tensor_tensor(out=dF[par][:, 2, :], in0=ps[:, 2, 0:16], in1=sm[:, 0, :], op=ALU.add), [pstok[2], t_sm], [t_dF[par]])
                    yield
                    for j in range(3):
                        for k in range(8):
                            pe(lambda e, j=j, k=k: e.matmul(out=ps[:, j, :], lhsT=hT[:, k, :], rhs=w2[:, k, 1024 + j * 512:1024 + (j + 1) * 512],
                                                            start=(k == 0), stop=(k == 7)), [t_hT, t_w2], [pstok[j]])
                    yield
                    dve(lambda e, par=par: e.tensor_scalar(out=dF[par][:, 3, :], in0=dF[par][:, 2, :], scalar1=-1.0, scalar2=None, op0=ALU.mult), [t_dF[par]], [t_dF[par]])
                    dve(lambda e, par=par: e.tensor_tensor(out=dF[par][:, 3, :], in0=dF[par][:, 3, :], in1=dF[par][:, 2, :], op=ALU.max), [t_dF[par]], [t_dF[par]])
                    act(lambda e, par=par: e.activation(out=dF[par][:, 3, :], in_=dF[par][:, 3, :], func=AF.Exp, scale=-1.0), [t_dF[par]], [t_dF[par]])
                    act(lambda e, par=par: e.activation(out=dF[par][:, 3, :], in_=dF[par][:, 3, :], func=AF.Ln, bias=1.0), [t_dF[par]], [t_dF[par]])
                    dve(lambda e, par=par: e.scalar_tensor_tensor(out=dF[par][:, 0, :], in0=dF[par][:, 2, :], scalar=0.0, in1=dF[par][:, 3, :],
                                                                  op0=ALU.max, op1=ALU.add), [t_dF[par]], [t_dF[par]])
                    dve(lambda e, par=par: e.tensor_tensor(out=dF[par][:, 1, :], in0=dF[par][:, 0, :], in1=sm[:, 1, :], op=ALU.mult), [t_dF[par], t_sm], [t_dF[par]])
                    yield
                    for k in range(4):
                        dve(lambda e, k=k, par=par: e.tensor_tensor(out=xw[par][:, k, :], in0=ps[:, 0:3, :].rearrange("p a b -> p (a b)"),
                                                                    in1=convw[:, k, :], op=ALU.mult),
                            [pstok[0], pstok[1], pstok[2], t_convw], [t_xw[par]])
                        if k == 1:
                            yield
                    yield
                    for j in range(3):
                        sl = slice(j * 512, (j + 1) * 512)
                        pe(lambda e, j=j, sl=sl, par=par: e.matmul(out=ps[:, j, :], lhsT=ident_bf[:, :], rhs=xw[par][:, 3, sl], start=True, stop=False),
                           [t_ident, t_xw[par]], [pstok[j]])
                        for k in range(3):
                            pe(lambda e, j=j, sl=sl, k=k, par=par: e.matmul(out=ps[:, j, :], lhsT=shb[:, k, :], rhs=xw[par][:, k, sl], start=False, stop=False),
                               [t_shb, t_xw[par]], [pstok[j]])
                        if not first:
                            for k in range(3):
                                pe(lambda e, j=j, sl=sl, k=k, par=par: e.matmul(out=ps[:, j, :], lhsT=shb[:, 3 + k, :], rhs=xw[1 - par][:, k, sl],
                                                                                start=False, stop=False),
                                   [t_shb, t_xw[1 - par]], [pstok[j]])
                        pe(lambda e, j=j, sl=sl: e.matmul(out=ps[:, j, :], lhsT=onesb[0:1, :], rhs=convb[0:1, sl], start=False, stop=True),
                           [t_onesb, t_convb], [pstok[j]])
                    yield
                    act(lambda e, par=par: e.activation(out=flat(xs[par][:, :, :]), in_=ps[:, 0:2, :].rearrange("p a b -> p (a b)"), func=AF.Silu),
                        [pstok[0], pstok[1]], [t_xs[par]])
                    act(lambda e, par=par: e.activation(out=flat(bc[par][:, :, :]), in_=ps[:, 2, :], func=AF.Silu), [pstok[2]], [t_bc[par]])
                    yield
                    for i in range(4):
                        pe(lambda e, i=i, par=par: e.transpose(out=ps3b[:, i * 128:(i + 1) * 128], in_=bc[par][:, i, :], identity=ident_bf[:, :]),
                           [t_bc[par], t_ident], [pstok[3]])
                    act(lambda e, par=par: e.activation(out=flat(bcT[par][:, :, :]), in_=ps3b[:, 0:512], func=AF.Copy), [pstok[3]], [t_bcT[par]])
                    yield

                def back(ci):
                    r0 = ci * 128
                    par = ci % 2
                    first = (ci % NCH == 0)
                    last = (ci % NCH == NCH - 1)
                    dt_ = dF[par][:, 0, :]
                    a_ = dF[par][:, 1, :]
                    pe(lambda e, a_=a_: e.matmul(out=ps[:, 4, 16:32], lhsT=cf("triu"), rhs=a_, start=True, stop=True), [t_cst, t_dF[par]], [pstok[4]])
                    dve(lambda e: e.tensor_copy(out=dtv[:, 2, :], in_=ps[:, 4, 16:32]), [pstok[4]], [t_dtv])
                    for g in range(2):
                        pe(lambda e, g=g, par=par: e.matmul(out=ps[:, 4, 256 + g * 128:256 + (g + 1) * 128], lhsT=bcT[par][:, g, :], rhs=bcT[par][:, 2 + g, :],
                                                            start=True, stop=True), [t_bcT[par]], [pstok[4]])
                    dve(lambda e: e.tensor_tensor(out=cbm[:, :, :], in0=ps[:, 4, 256:512].rearrange("p (g t) -> p g t", g=2),
                                                  in1=cf("triu").unsqueeze(1).to_broadcast([128, 2, 128]), op=ALU.mult),
                        [pstok[4], t_cst], [t_cbm])
                    dve(lambda e, a_=a_: e.tensor_tensor(out=R[:, :, :], in0=cf("triu").unsqueeze(1).to_broadcast([128, 16, 128]),
                                                         in1=a_.unsqueeze(2).to_broadcast([128, 16, 128]), op=ALU.mult),
                        [t_cst, t_dF[par]], [t_R])
                    yield
                    ab = [4, 5, 6, 7]
                    for q in range(4):
                        pe(lambda e, q=q: e.matmul(out=ps[:, ab[q], :], lhsT=cf("gt"), rhs=R[:, 4 * q:4 * q + 4, :].rearrange("p a b -> p (a b)"),
                                                   start=True, stop=True), [t_cst, t_R], [pstok[ab[q]]])
                    yield
                    for q in range(4):
                        dve(lambda e, q=q: e.tensor_copy(out=dtv[:, 3, 4 * q:4 * q + 4],
                                                         in_=ps[:, ab[q], :].rearrange("p (h t) -> p h t", h=4)[:, :, 127]),
                            [pstok[ab[q]]], [t_dtv])
                    yield
                    act(lambda e: e.activation(out=flat(LT[:, :, :]), in_=ps[:, 4:8, :].rearrange("p a b -> p (a b)"), func=AF.Exp),
                        [pstok[4], pstok[5], pstok[6], pstok[7]], [t_LT])
                    dve(lambda e: e.tensor_tensor(out=dtv[:, 7, :], in0=dtv[:, 3, :], in1=dtv[:, 2, :], op=ALU.add), [t_dtv], [t_dtv])
                    act(lambda e: e.activation(out=dtv[:, 4, :], in_=dtv[:, 3, :], func=AF.Exp), [t_dtv], [t_dtv])
                    act(lambda e: e.activation(out=dtv[:, 5, :], in_=dtv[:, 7, :], func=AF.Exp), [t_dtv], [t_dtv])
                    act(lambda e: e.activation(out=dtv[:, 6, :], in_=dtv[:, 2, :], func=AF.Exp), [t_dtv], [t_dtv])
                    dve(lambda e, dt_=dt_: e.tensor_tensor(out=dtv[:, 4, :], in0=dtv[:, 4, :], in1=dt_, op=ALU.mult), [t_dtv, t_dF[par]], [t_dtv])
                    yield
                    for g in range(2):
                        dve(lambda e, g=g: e.tensor_tensor(out=MT[:, 8 * g:8 * g + 8, :], in0=LT[:, 8 * g:8 * g + 8, :],
                                                           in1=cbm[:, g, :].unsqueeze(1).to_broadcast([128, 8, 128]), op=ALU.mult),
                            [t_LT, t_cbm], [t_MT])
                    dve(lambda e, par=par, dt_=dt_: e.tensor_tensor(out=xdt[:, :, :], in0=xs[par][:, :, :], in1=dt_.unsqueeze(2).to_broadcast([128, 16, 64]), op=ALU.mult),
                        [t_xs[par], t_dF[par]], [t_xdt])
                    yield
                    dve(lambda e, par=par: e.tensor_tensor(out=xdtd[:, :, :], in0=xs[par][:, :, :], in1=dtv[:, 4, :].unsqueeze(2).to_broadcast([128, 16, 64]), op=ALU.mult),
                        [t_xs[par], t_dtv], [t_xdtd])
                    for h in range(16):
                        pe(lambda e, h=h: e.matmul(out=ps[:, 4 + h // 8, (h % 8) * 64:(h % 8 + 1) * 64], lhsT=MT[:, h, :], rhs=xdt[:, h, :],
                                                   start=True, stop=True), [t_MT, t_xdt], [pstok[4 + h // 8]])
                    if not first:
                        for g in range(2):
                            pe(lambda e, g=g, par=par: e.matmul(out=ps[:, 6 + g, :], lhsT=bcT[par][:, 2 + g, :], rhs=flat(stTb[:, 8 * g:8 * g + 8, :]),
                                                                start=True, stop=True), [t_bcT[par], t_stTb], [pstok[6 + g]])
                        yield
                        dve(lambda e: e.tensor_tensor(out=yy[:, :, :], in0=ps[:, 6:8, :].rearrange("p a (h d) -> p (a h) d", d=64),
                                                      in1=dtv[:, 6, :].unsqueeze(2).to_broadcast([128, 16, 64]), op=ALU.mult),
                            [pstok[6], pstok[7], t_dtv], [t_yy])
                        dve(lambda e: e.tensor_tensor(out=flat(yy[:, :, :]), in0=flat(yy[:, :, :]),
                                                      in1=ps[:, 4:6, :].rearrange("p a b -> p (a b)"), op=ALU.add),
                            [pstok[4], pstok[5], t_yy], [t_yy])
                    else:
                        yield
                        dve(lambda e: e.tensor_copy(out=flat(yy[:, :, :]), in_=ps[:, 4:6, :].rearrange("p a b -> p (a b)")),
                            [pstok[4], pstok[5]], [t_yy])
                    yield
                    if not last:
                        for g in range(2):
                            pe(lambda e, g=g, par=par: e.matmul(out=ps[:, 4 + g, :], lhsT=bc[par][:, g, :], rhs=flat(xdtd[:, 8 * g:8 * g + 8, :]),
                                                                start=True, stop=True), [t_bc[par], t_xdtd], [pstok[4 + g]])
                        if first:
                            dve(lambda e: e.tensor_copy(out=flat(stT[:, :, :]), in_=ps[:, 4:6, :].rearrange("p a b -> p (a b)")),
                                [pstok[4], pstok[5]], [t_stT])
                        else:
                            dve(lambda e: e.tensor_tensor(out=stT[:, :, :], in0=stT[:, :, :], in1=dtv[:, 5, :].unsqueeze(2).to_broadcast([128, 16, 64]), op=ALU.mult),
                                [t_stT, t_dtv], [t_stT])
                            dve(lambda e: e.tensor_tensor(out=flat(stT[:, :, :]), in0=flat(stT[:, :, :]),
                                                          in1=ps[:, 4:6, :].rearrange("p a b -> p (a b)"), op=ALU.add),
                                [pstok[4], pstok[5], t_stT], [t_stT])
                        pool(lambda e: e.tensor_copy(out=stTb[:, :, :], in_=stT[:, :, :]), [t_stT], [t_stTb])
                    yield
                    dve(lambda e, par=par: e.tensor_tensor(out=xs[par][:, :, :], in0=xs[par][:, :, :], in1=sm[:, 2, :].unsqueeze(2).to_broadcast([128, 16, 64]), op=ALU.mult),
                        [t_xs[par], t_sm], [t_xs[par]])
                    dve(lambda e, par=par: e.tensor_tensor(out=yy[:, :, :], in0=yy[:, :, :], in1=xs[par][:, :, :], op=ALU.add), [t_yy, t_xs[par]], [t_yy])
                    yield
                    dve(lambda e, par=par: e.tensor_tensor(out=flat(yy[:, :, :]), in0=flat(yy[:, :, :]), in1=zs[par][:, :], op=ALU.mult), [t_yy, t_zs[par]], [t_yy])
                    for g in range(2):
                        act(lambda e, g=g: e.activation(out=junkB[:, 0:512], in_=flat(yy[:, 8 * g:8 * g + 8, :]),
                                                        func=AF.Square, accum_out=stB[:, 4 + g:5 + g]), [t_yy], [t_junkB, t_stB])
                    dve(lambda e: e.tensor_scalar(out=stB[:, 6:8], in0=stB[:, 4:6], scalar1=1.0 / 512, scalar2=EPS, op0=ALU.mult, op1=ALU.add),
                        [t_stB], [t_stB])
                    rsqrt(stB[:, 8:10], stB[:, 6:8], [t_stB], [t_stB])
                    yield
                    dve(lambda e, par=par: e.tensor_tensor(out=so[par][:, :].rearrange("p (g d) -> p g d", g=2),
                                                           in0=yy[:, :, :].rearrange("p (g a) b -> p g (a b)", g=2),
                                                           in1=stB[:, 8:10].unsqueeze(2).to_broadcast([128, 2, 512]), op=ALU.mult),
                        [t_yy, t_stB], [t_so[par]])
                    finals.append(P.op("pool", lambda e, par=par, r0=r0: e.dma_start(out=cat_d[r0:r0 + 128, 1024:2048], in_=so[par][:, :]),
                                       reads=[t_so[par]], writes=[], dsem=ds_so[par]))
                    yield

                def interleave(g1, g2):
                    gens = [g for g in (g1, g2) if g is not None]
                    while gens:
                        for g in list(gens):
                            try:
                                next(g)
                            except StopIteration:
                                gens.remove(g)

                interleave(front(0), None)
                for ci in range(NT):
                    interleave(back(ci), front(ci + 1) if ci + 1 < NT else None)

        def _sweep3():
            P.barrier()
            with contextlib.ExitStack() as s3:
                def sb3(name, shape, dt=F32):
                    return s3.enter_context(nc.sbuf_tensor(name, list(shape), dt))

                stg = sb3("stg_3", [128, 2576]); t_stg = Tok()

                dve = lambda fn, r=(), w=(): P.op("dve", fn, r, w)
                act = lambda fn, r=(), w=(): P.op("act", fn, r, w)
                pe = lambda fn, r=(), w=(): P.op("pe", fn, r, w)
                pool = lambda fn, r=(), w=(): P.op("pool", fn, r, w)
                ds_small3 = P.dsem(group=True)
                wo = sb3("wo", [128, 16, 1024], BF16); t_wo = Tok()
                gcat = sb3("gcat_s", [128, 16]); t_gcat = Tok()
                P.op("sp", lambda e: e.dma_start(out=gcat[:, :], in_=gcat_d[:, :]), writes=[t_gcat], dsem=ds_small3)
                for k in range(16):
                    P.op("sp", lambda e, k=k: e.dma_start(out=stg[:, 0:1024], in_=wout_d[k * 128:(k + 1) * 128, :]), writes=[t_stg], dsem=ds_stg)
                    dve(lambda e, k=k: e.tensor_scalar(out=wo[:, k, :], in0=stg[:, 0:1024], scalar1=gcat[:, k:k + 1], scalar2=None, op0=ALU.mult),
                        [t_stg, t_gcat], [t_wo])
                Wc = sb3("Wc", [128, 8, 2048], BF16); t_Wc = Tok()
                g2c = sb3("g2c", [128, 8]); t_g2c = Tok()
                P.op("sp", lambda e: e.dma_start(out=g2c[:, :], in_=g2c_d[:, :]), writes=[t_g2c], dsem=ds_small3)
                kT = sb3("kT", [128, 2, 128]); t_kT = Tok()
                P.op("sp", lambda e: e.dma_start(out=kT[:, :, :], in_=kT_d[:, :, :]), writes=[t_kT], dsem=ds_small3)
                stgq = [sb3("stgq%d" % i, [128, 1024]) for i in range(2)]; t_stgq = [Tok(), Tok()]; ds_q = [P.dsem(), P.dsem()]
                for j in range(16):
                    qb = j % 2
                    P.op("sp", lambda e, j=j, qb=qb: e.dma_start(out=stgq[qb][:, :], in_=wqT_d[j * 128:(j + 1) * 128, :]),
                         writes=[t_stgq[qb]], dsem=ds_q[qb])
                    b0 = qb * 2
                    for k in range(8):
                        pe(lambda e, j=j, k=k, qb=qb, b0=b0: e.matmul(out=ps[:, b0 + k // 4, (k % 4) * 128:(k % 4 + 1) * 128],
                                                                      lhsT=stgq[qb][:, k * 128:(k + 1) * 128], rhs=kT[:, j % 2, :], start=True, stop=True),
                           [t_stgq[qb], t_kT], [pstok[b0 + k // 4]])
                    dve(lambda e, j=j, b0=b0: e.tensor_tensor(out=Wc[:, :, j * 128:(j + 1) * 128],
                                                              in0=ps[:, b0:b0 + 2, :].rearrange("p a (k n) -> p (a k) n", n=128),
                                                              in1=g2c[:, :].unsqueeze(2).to_broadcast([128, 8, 128]), op=ALU.mult),
                        [pstok[b0], pstok[b0 + 1], t_g2c], [t_Wc])

                catb = sb3("catb", [128, 2048], BF16); t_catb = Tok(); ds_catb = P.dsem()
                xt = sb3("xt_3", [128, 1024]); t_xt = Tok(); ds_xt = P.dsem()
                catT = sb3("catT", [128, 16, 128], BF16); t_catT = Tok()
                x1 = [sb3("x1_%d" % i, [128, 1024]) for i in range(2)]; t_x1 = [Tok(), Tok()]; ds_x1 = [P.dsem(), P.dsem()]
                junk = sb3("junk_3", [128, 1024], BF16); t_junk = Tok()
                st = sb3("st_3", [128, 16]); t_st = Tok()
                h2b = sb3("h2b", [128, 1024], BF16); t_h2b = Tok()
                h2T = [sb3("h2T_%d" % i, [128, 8, 128], BF16) for i in range(2)]; t_h2T = [Tok(), Tok()]; ds_h2T = [P.dsem(), P.dsem()]
                Swk = sb3("Swk", [128, 16, 128]); t_Swk = Tok()
                vv = sb3("vv", [128, 16, 16]); t_vv = Tok()
                tg_vv = [Tok() for _ in range(16)]; tg_vv2 = [Tok() for _ in range(16)]; tg_Swk = [Tok() for _ in range(16)]
                tg_idx = [Tok() for _ in range(16)]; tg_idx2 = [Tok() for _ in range(16)]
                th_c16 = [Tok() for _ in range(8)]; th_c16b = [Tok() for _ in range(8)]; th_candw = [Tok() for _ in range(8)]
                th_cpos = [Tok() for _ in range(8)]; th_cposb = [Tok() for _ in range(8)]
                idx = sb3("idx", [128, 16, 16], U32); t_idx = Tok()
                idxf = sb3("idxf", [128, 16, 16], BF16); t_idxf = Tok()
                cand = sb3("cand", [128, 8, 16, 16]); t_cand = Tok()
                candw = sb3("candw", [128, 8, 256]); t_candw = Tok()
                c16 = sb3("c16", [128, 8, 16]); t_c16 = Tok()
                cpos = sb3("cpos", [128, 8, 16], U32); t_cpos = Tok()
                cpi = sb3("cpi", [128, 2, 8, 16], U32); t_cpi = Tok()
                cpf = sb3("cpf", [128, 2, 8, 16]); t_cpf = Tok()
                ohs = [sb3("oh%d" % i, [128, 8, 16, 16], BF16) for i in range(2)]; t_ohs = [Tok(), Tok()]
                gt = sb3("gt", [128, 8, 16]); t_gt = Tok()
                z8 = sb3("z8", [128, 8]); t_z8 = Tok()
                e12 = sb3("e12", [128, 2, 8, 16]); t_e12 = Tok()
                rtm = sb3("rtm", [128, 3, 128], BF16); t_rtm = Tok()
                rt = [sb3("rt_%d" % i, [128, 3, 128], BF16) for i in range(2)]; t_rt = [Tok(), Tok()]; ds_rt = [P.dsem(), P.dsem()]
                psT = ps[:, 7, :].bitcast(BF16)
                ps6b = ps[:, 6, :].bitcast(BF16)
                iota16 = cf("iota")[:, 0:16]

                ps4b = ps[:, 4, :].bitcast(BF16)
                Ssb2 = [sb3("Ssb2_%d" % i, [128, 16, 128]) for i in range(2)]; t_Ssb2 = [Tok(), Tok()]

                def front(ci):
                    r0 = ci * 128
                    par = ci % 2
                    P.op("sp", lambda e, r0=r0: e.dma_start(out=catb[:, :], in_=cat_d[r0:r0 + 128, :]), writes=[t_catb], dsem=ds_catb)
                    P.op("sp", lambda e, r0=r0: e.dma_start(out=xt[:, :], in_=x_d[r0:r0 + 128, :]), writes=[t_xt], dsem=ds_xt)
                    for k in range(16):
                        pb = psT if k < 8 else ps6b
                        pe(lambda e, k=k, pb=pb: e.transpose(out=pb[:, (k % 8) * 128:(k % 8 + 1) * 128], in_=catb[:, k * 128:(k + 1) * 128], identity=ident_bf[:, :]),
                           [t_catb, t_ident], [pstok[7 if k < 8 else 6]])
                    act(lambda e: e.activation(out=catT[:, 0:8, :].rearrange("p k t -> p (k t)"), in_=psT[:, :], func=AF.Copy), [pstok[7]], [t_catT])
                    act(lambda e: e.activation(out=catT[:, 8:16, :].rearrange("p k t -> p (k t)"), in_=ps6b[:, :], func=AF.Copy), [pstok[6]], [t_catT])
                    yield
                    for j in range(2):
                        for k in range(16):
                            pe(lambda e, j=j, k=k: e.matmul(out=ps[:, j, :], lhsT=catT[:, k, :], rhs=wo[:, k, j * 512:(j + 1) * 512],
                                                            start=(k == 0), stop=(k == 15)), [t_catT, t_wo], [pstok[j]])
                    yield
                    dve(lambda e, par=par: e.tensor_tensor(out=x1[par][:, :], in0=ps[:, 0:2, :].rearrange("p a b -> p (a b)"), in1=xt[:, :], op=ALU.add),
                        [pstok[0], pstok[1], t_xt], [t_x1[par]])
                    P.op("pool", lambda e, par=par, r0=r0: e.dma_start(out=x1_d[r0:r0 + 128, :], in_=x1[par][:, :]),
                         reads=[t_x1[par]], writes=[], dsem=ds_x1[par])
                    yield
                    act(lambda e, par=par: e.activation(out=junk[:, :], in_=x1[par][:, :], func=AF.Square, accum_out=st[:, 0:1]), [t_x1[par]], [t_junk, t_st])
                    dve(lambda e: e.tensor_scalar(out=st[:, 1:2], in0=st[:, 0:1], scalar1=1.0 / D, scalar2=EPS, op0=ALU.mult, op1=ALU.add), [t_st], [t_st])
                    rsqrt(st[:, 2:3], st[:, 1:2], [t_st], [t_st])
                    act(lambda e, par=par: e.activation(out=h2b[:, :], in_=x1[par][:, :], func=AF.Copy, scale=st[:, 2:3]), [t_x1[par], t_st], [t_h2b])
                    yield
                    for k in range(8):
                        pe(lambda e, k=k: e.transpose(out=psT[:, k * 128:(k + 1) * 128], in_=h2b[:, k * 128:(k + 1) * 128], identity=ident_bf[:, :]),
                           [t_h2b, t_ident], [pstok[7]])
                    act(lambda e, par=par: e.activation(out=h2T[par][:, :, :].rearrange("p k t -> p (k t)"), in_=psT[:, :], func=AF.Copy), [pstok[7]], [t_h2T[par]])
                    P.op("pool", lambda e, par=par, ci=ci: e.dma_start(out=h2T_d[ci, :, :], in_=h2T[par][:, :, :].rearrange("p k t -> p (k t)")),
                         reads=[t_h2T[par]], writes=[], dsem=ds_h2T[par])
                    yield
                    for j in range(4):
                        for k in range(8):
                            pe(lambda e, j=j, k=k, par=par: e.matmul(out=ps[:, j, :], lhsT=h2T[par][:, k, :], rhs=Wc[:, k, j * 512:(j + 1) * 512],
                                                                     start=(k == 0), stop=(k == 7)), [t_h2T[par], t_Wc], [pstok[j]])
                    yield
                    act(lambda e, par=par: e.activation(out=Ssb2[par][:, :, :].rearrange("p a b -> p (a b)"), in_=ps[:, 0:4, :].rearrange("p a b -> p (a b)"), func=AF.Copy),
                        [pstok[0], pstok[1], pstok[2], pstok[3]], [t_Ssb2[par]])
                    yield

                def back(ci):
                    r0 = ci * 128
                    par = ci % 2
                    for j in range(16):
                        dve(lambda e, j=j, par=par: e.max(out=vv[:, j, 0:8], in_=Ssb2[par][:, j, :]), [t_Ssb2[par]], [tg_vv[j]])
                    yield
                    for j in range(16):
                        dve(lambda e, j=j, par=par: e.match_replace(out=Swk[:, j, :], in_to_replace=vv[:, j, 0:8], in_values=Ssb2[par][:, j, :], imm_value=-1e30),
                            [t_Ssb2[par], tg_vv[j]], [tg_Swk[j]])
                    yield
                    for j in range(16):
                        dve(lambda e, j=j, par=par: e.max_index(out=idx[:, j, 0:8], in_max=vv[:, j, 0:8], in_values=Ssb2[par][:, j, :]), [t_Ssb2[par], tg_vv[j]], [tg_idx[j]])
                    yield
                    for j in range(16):
                        dve(lambda e, j=j, par=par: e.max(out=vv[:, j, 8:16], in_=Swk[:, j, :]), [tg_Swk[j]], [tg_vv2[j]])
                    yield
                    for j in range(16):
                        dve(lambda e, j=j, par=par: e.max_index(out=idx[:, j, 8:16], in_max=vv[:, j, 8:16], in_values=Swk[:, j, :]), [tg_Swk[j], tg_vv2[j]], [tg_idx2[j]])
                    yield
                    dve(lambda e: e.tensor_copy(out=idxf[:, :, :], in_=idx[:, :, :]), tg_idx + tg_idx2, [t_idxf])
                    v4 = vv[:, :, :].rearrange("p (h two) k -> p h two k", two=2)
                    dve(lambda e, v4=v4: e.tensor_tensor(out=cand[:, :, :, :], in0=v4[:, :, 0, :].unsqueeze(3).to_broadcast([128, 8, 16, 16]),
                                                         in1=v4[:, :, 1, :].unsqueeze(2).to_broadcast([128, 8, 16, 16]), op=ALU.add), tg_vv + tg_vv2, [t_cand])
                    cfls = [cand[:, h, :, :].rearrange("p a b -> p (a b)") for h in range(8)]
                    for h in range(8):
                        dve(lambda e, h=h: e.max(out=c16[:, h, 0:8], in_=cfls[h]), [t_cand], [th_c16[h]])
                    yield
                    for h in range(8):
                        dve(lambda e, h=h: e.match_replace(out=candw[:, h, :], in_to_replace=c16[:, h, 0:8], in_values=cfls[h], imm_value=-1e30),
                            [t_cand, th_c16[h]], [th_candw[h]])
                    yield
                    for h in range(8):
                        dve(lambda e, h=h: e.max_index(out=cpos[:, h, 0:8], in_max=c16[:, h, 0:8], in_values=cfls[h]), [t_cand, th_c16[h]], [th_cpos[h]])
                    yield
                    for h in range(8):
                        dve(lambda e, h=h: e.max(out=c16[:, h, 8:16], in_=candw[:, h, :]), [th_candw[h]], [th_c16b[h]])
                    yield
                    for h in range(8):
                        dve(lambda e, h=h: e.max_index(out=cpos[:, h, 8:16], in_max=c16[:, h, 8:16], in_values=candw[:, h, :]), [th_candw[h], th_c16b[h]], [th_cposb[h]])
                    yield
                    dve(lambda e: e.tensor_tensor(out=gt[:, :, :], in0=c16[:, :, :], in1=c16[:, :, 0:1].to_broadcast([128, 8, 16]), op=ALU.subtract),
                        th_c16 + th_c16b, [t_gt])
                    act(lambda e: e.activation(out=gt[:, :, :].rearrange("p a b -> p (a b)"), in_=gt[:, :, :].rearrange("p a b -> p (a b)"), func=AF.Exp),
                        [t_gt], [t_gt])
                    dve(lambda e: e.reduce_sum(out=z8[:, :], in_=gt[:, :, :], axis=AX.X), [t_gt], [t_z8])
                    dve(lambda e: e.reciprocal(out=z8[:, :], in_=z8[:, :]), [t_z8], [t_z8])
                    dve(lambda e: e.tensor_tensor(out=gt[:, :, :], in0=gt[:, :, :], in1=z8[:, :].unsqueeze(2).to_broadcast([128, 8, 16]), op=ALU.mult),
                        [t_gt, t_z8], [t_gt])
                    yield
                    dve(lambda e: e.tensor_single_scalar(out=cpi[:, 0, :, :], in_=cpos[:, :, :], scalar=4, op=ALU.logical_shift_right), th_cpos + th_cposb, [t_cpi])
                    dve(lambda e: e.tensor_single_scalar(out=cpi[:, 1, :, :], in_=cpos[:, :, :], scalar=15, op=ALU.bitwise_and), th_cpos + th_cposb, [t_cpi])
                    dve(lambda e: e.tensor_copy(out=cpf[:, :, :, :], in_=cpi[:, :, :, :]), [t_cpi], [t_cpf])
                    i4 = idxf[:, :, :].rearrange("p (h two) k -> p h two k", two=2)
                    yield
                    for w in range(2):
                        dve(lambda e, w=w: e.tensor_tensor(out=ohs[w][:, :, :, :], in0=cpf[:, w, :, :].unsqueeze(3).to_broadcast([128, 8, 16, 16]),
                                                           in1=iota16.unsqueeze(1).unsqueeze(1).to_broadcast([128, 8, 16, 16]), op=ALU.is_equal),
                            [t_cpf, t_cst], [t_ohs[w]])
                        dve(lambda e, w=w, i4=i4: e.tensor_tensor(out=ohs[w][:, :, :, :], in0=ohs[w][:, :, :, :],
                                                                  in1=i4[:, :, w, :].unsqueeze(2).to_broadcast([128, 8, 16, 16]), op=ALU.mult),
                            [t_ohs[w], t_idxf], [t_ohs[w]])
                        dve(lambda e, w=w: e.reduce_sum(out=e12[:, w, :, :], in_=ohs[w][:, :, :, :], axis=AX.X), [t_ohs[w]], [t_e12])
                    yield
                    dve(lambda e: e.tensor_copy(out=rtm[:, 0:2, :], in_=e12[:, :, :, :].rearrange("p w h k -> p w (h k)")), [t_e12], [t_rtm])
                    dve(lambda e: e.tensor_copy(out=rtm[:, 2, :], in_=gt[:, :, :].rearrange("p h k -> p (h k)")), [t_gt], [t_rtm])
                    for w in range(3):
                        pe(lambda e, w=w: e.transpose(out=ps4b[:, w * 128:(w + 1) * 128], in_=rtm[:, w, :], identity=ident_bf[:, :]),
                           [t_rtm, t_ident], [pstok[4]])
                    act(lambda e, par=par: e.activation(out=rt[par][:, :, :].rearrange("p a b -> p (a b)"), in_=ps4b[:, 0:384], func=AF.Copy),
                        [pstok[4]], [t_rt[par]])
                    P.op("pool", lambda e, par=par, ci=ci: e.dma_start(out=rt_d[ci, :, :], in_=rt[par][:, :, :].rearrange("p a b -> p (a b)")),
                         reads=[t_rt[par]], writes=[], dsem=ds_rt[par])


                    yield

                def interleave(g1, g2):
                    gens = [g for g in (g1, g2) if g is not None]
                    while gens:
                        for g in list(gens):
                            try:
                                next(g)
                            except StopIteration:
                                gens.remove(g)

                interleave(front(0), None)
                for ci in range(NT):
                    interleave(back(ci), front(ci + 1) if ci + 1 < NT else None)

        def _sweep4():
            P.barrier()
            dve = lambda fn, r=(), w=(): P.op("dve", fn, r, w)
            act = lambda fn, r=(), w=(): P.op("act", fn, r, w)
            pe = lambda fn, r=(), w=(): P.op("pe", fn, r, w)
            pool = lambda fn, r=(), w=(): P.op("pool", fn, r, w)
            with contextlib.ExitStack() as s4:
                def sb4(name, shape, dt=F32):
                    return s4.enter_context(nc.sbuf_tensor(name, list(shape), dt))
                NS = 256
                fgb = sb4("fgb", [128, 1024]); t_fgb = Tok(); ds_s4b = P.dsem(group=True)
                P.op("sp", lambda e: e.dma_start(out=fgb[:, :], in_=fg_d.partition_broadcast(128)), writes=[t_fgb], dsem=ds_s4b)
                iob = sb4("iob", [128, 128], BF16); t_iob = Tok()
                dve(lambda e: e.tensor_copy(out=iob[:, :], in_=cf("iota")), [t_cst], [t_iob])
                GT = [sb4("GT%d" % i, [128, 128, NS], BF16) for i in range(2)]; t_GT = [Tok(), Tok()]
                h2s = [sb4("h2s%d" % i, [128, 8, NS], BF16) for i in range(2)]; t_h2s = [Tok(), Tok()]; ds_h2s = [P.dsem(), P.dsem()]
                rts = [sb4("rts_sb%d" % i, [128, 2, 3, 128], BF16) for i in range(2)]; t_rts = [Tok(), Tok()]; ds_rts = [P.dsem(), P.dsem()]
                NR = 8
                XA = sb4("XA", [128, NR, 128], BF16); XB = sb4("XB", [128, NR, 128], BF16)
                t_XA = [Tok() for _ in range(NR)]; t_XB = [Tok() for _ in range(NR)]
                Ug = [sb4("Ug%d" % i, [128, 8, 512], BF16) for i in range(2)]; t_Ug = [Tok(), Tok()]; ds_Ug = [P.dsem(), P.dsem()]
                Vg = [sb4("Vg%d" % i, [128, 4, 1024], BF16) for i in range(2)]; t_Vg = [Tok(), Tok()]; ds_Vg = [P.dsem(), P.dsem()]
                aa = [sb4("aa%d" % i, [128, NS], BF16) for i in range(2)]; t_aa = [Tok(), Tok()]
                a2 = [sb4("a2%d" % i, [128, NS], BF16) for i in range(2)]; t_a2 = [Tok(), Tok()]
                x1t = [sb4("x1t%d" % i, [128, 1024]) for i in range(2)]; t_x1t = [Tok(), Tok()]; ds_x1t = [P.dsem(), P.dsem()]
                st = sb4("st_4", [128, 16]); t_st = Tok()
                ot = [sb4("ot%d" % i, [128, 1024]) for i in range(2)]; t_ot = [Tok(), Tok()]; ds_ot = [P.dsem(), P.dsem()]
                nslab = NT // 2
                grp_ctr = [0]

                pending = []

                def gbuild(sl, ta, tb, defer=False):
                    q = sl % 2
                    if ta == 0:
                        for c in range(2):
                            ci = sl * 2 + c
                            P.op("sp", lambda e, ci=ci, c=c, q=q: e.dma_start(out=h2s[q][:, :, c * 128:(c + 1) * 128],
                                                                              in_=h2T_d[ci, :, :].rearrange("p (k t) -> p k t", k=8)),
                                 writes=[t_h2s[q]], dsem=ds_h2s[q])
                            P.op("sp", lambda e, ci=ci, c=c, q=q: e.dma_start(out=rts[q][:, c, :, :], in_=rt_d[ci, :, :].rearrange("p (w t) -> p w t", w=3)),
                                 writes=[t_rts[q]], dsem=ds_rts[q])
                    for t in range(ta, tb):
                        c, tt = t // 128, t % 128
                        slot = t % NR
                        gb = 6 + (t // 4) % 2
                        dve(lambda e, c=c, tt=tt, slot=slot, q=q: e.tensor_scalar(out=XA[:, slot, :], in0=iob[:, :], scalar1=rts[q][:, c, 0, tt:tt + 1],
                                                                                  scalar2=rts[q][:, c, 2, tt:tt + 1], op0=ALU.is_equal, op1=ALU.mult),
                            [t_iob, t_rts[q]], [t_XA[slot]])
                        dve(lambda e, c=c, tt=tt, slot=slot, q=q: e.tensor_scalar(out=XB[:, slot, :], in0=iob[:, :], scalar1=rts[q][:, c, 1, tt:tt + 1],
                                                                                  scalar2=None, op0=ALU.is_equal),
                            [t_iob, t_rts[q]], [t_XB[slot]])
                        pe(lambda e, slot=slot, gb=gb, t=t: e.matmul(out=ps[:, gb, (t % 4) * 128:(t % 4 + 1) * 128], lhsT=XB[:, slot, :], rhs=XA[:, slot, :],
                                                                     start=True, stop=True),
                           [t_XA[slot], t_XB[slot]], [pstok[gb]])
                        if t % 4 == 3:
                            t0 = t - 3

                            def evac(gb=gb, t0=t0, q=q):
                                act(lambda e: e.activation(out=GT[q][:, :, t0:t0 + 4], in_=ps[:, gb, :].rearrange("p (t e) -> p e t", t=4), func=AF.Copy),
                                    [pstok[gb]], [t_GT[q]])
                            if defer:
                                pending.append(evac)
                            else:
                                evac()

                def load_grp(grp):
                    ub = grp_ctr[0] % 2
                    grp_ctr[0] += 1
                    P.op("sp", lambda e, grp=grp, ub=ub: e.dma_start(out=Ug[ub][:, :, :], in_=UTb_d[grp, :, :, :]), writes=[t_Ug[ub]], dsem=ds_Ug[ub])
                    P.op("sp", lambda e, grp=grp, ub=ub: e.dma_start(out=Vg[ub][:, :, :], in_=Vb_d[grp, :, :, :]), writes=[t_Vg[ub]], dsem=ds_Vg[ub])
                    return ub

                gbuild(0, 0, NS)
                for sl in range(nslab):
                    q = sl % 2
                    ubs = {}

                    def scores(eb, q=q, ubs=ubs):
                        grp, b = eb // 4, eb % 4
                        if b == 0:
                            ubs[grp] = load_grp(grp)
                        ub = ubs[grp]
                        sb_ = 4 + eb % 2
                        for k in range(8):
                            pe(lambda e, k=k, b=b, ub=ub, sb_=sb_, q=q: e.matmul(out=ps[:, sb_, 0:NS], lhsT=Ug[ub][:, k, b * 128:(b + 1) * 128], rhs=h2s[q][:, k, :],
                                                                                 start=(k == 0), stop=(k == 7)),
                               [t_Ug[ub], t_h2s[q]], [pstok[sb_]])

                    scores(0)
                    for eb in range(128):
                        grp, b = eb // 4, eb % 4
                        ub = ubs[grp]
                        sb_ = 4 + eb % 2
                        ab = eb % 2
                        if eb + 1 < 128:
                            scores(eb + 1)
                        act(lambda e, sb_=sb_, ab=ab: e.activation(out=aa[ab][:, :], in_=ps[:, sb_, 0:NS], func=AF.Gelu), [pstok[sb_]], [t_aa[ab]])
                        while pending:
                            pending.pop(0)()
                        dve(lambda e, ab=ab, eb=eb, q=q: e.tensor_tensor(out=a2[ab][:, :], in0=aa[ab][:, :], in1=GT[q][:, eb, :], op=ALU.mult),
                            [t_aa[ab], t_GT[q]], [t_a2[ab]])
                        for i in range(2):
                            for j in range(2):
                                pe(lambda e, i=i, j=j, ab=ab, b=b, ub=ub, eb=eb: e.matmul(out=ps[:, i * 2 + j, :], lhsT=a2[ab][:, i * 128:(i + 1) * 128],
                                                                                         rhs=Vg[ub][:, b, j * 512:(j + 1) * 512],
                                                                                         start=(eb == 0), stop=(eb == 127)),
                                   [t_a2[ab], t_Vg[ub]], [pstok[i * 2 + j]])
                        if sl + 1 < nslab:
                            gbuild(sl + 1, 2 * eb, 2 * eb + 2, defer=True)
                    while pending:
                        pending.pop(0)()
                    for i in range(2):
                        ci = sl * 2 + i
                        r0 = ci * 128
                        P.op("sp", lambda e, i=i, r0=r0: e.dma_start(out=x1t[i][:, :], in_=x1_d[r0:r0 + 128, :]), writes=[t_x1t[i]], dsem=ds_x1t[i])
                        dve(lambda e, i=i: e.tensor_tensor(out=x1t[i][:, :], in0=ps[:, 2 * i:2 * i + 2, :].rearrange("p a b -> p (a b)"), in1=x1t[i][:, :], op=ALU.add),
                            [pstok[2 * i], pstok[2 * i + 1], t_x1t[i]], [t_x1t[i]])
                        act(lambda e, i=i: e.activation(out=ot[i][:, :], in_=x1t[i][:, :], func=AF.Square, accum_out=st[:, 0:1]), [t_x1t[i]], [t_ot[i], t_st])
                        dve(lambda e: e.tensor_scalar(out=st[:, 1:2], in0=st[:, 0:1], scalar1=1.0 / D, scalar2=EPS, op0=ALU.mult, op1=ALU.add), [t_st], [t_st])
                        rsqrt(st[:, 2:3], st[:, 1:2], [t_st], [t_st])
                        dve(lambda e, i=i: e.scalar_tensor_tensor(out=ot[i][:, :], in0=x1t[i][:, :], scalar=st[:, 2:3], in1=fgb[:, :], op0=ALU.mult, op1=ALU.mult),
                            [t_x1t[i], t_st, t_fgb], [t_ot[i]])
                        finals.append(P.op("pool", lambda e, i=i, r0=r0: e.dma_start(out=out_d[r0:r0 + 128, :], in_=ot[i][:, :]),
                                           reads=[t_ot[i]], writes=[], dsem=ds_ot[i]))

        for _k, _f in ((1, _sweep1), (2, _sweep2), (3, _sweep3), (4, _sweep4)):
            if _k in sweeps:
                _f()
        P.emit(final_waits=finals)
    nc.input_names = in_names
    return nc


def prep_shared(inp):
    hc = host_consts()
    f = lambda a: np.ascontiguousarray(np.asarray(a, dtype=np.float32))
    sh = {
        "w_in": f(inp["w_in"][0]),
        "norm1_gc": f(inp["norm1_g"][0].reshape(8, 128).T),
        "gmlp_ln_g": f(inp["gmlp_ln_g"][0]),
        "gmlp_ln_b": f(inp["gmlp_ln_b"][0]),
        "gmlp_wsT": f(inp["gmlp_ws"][0].transpose(2, 0, 1)),
        "gmlp_bsT": f(inp["gmlp_bs"][0].T),
        "consts": f(np.concatenate([hc[n] for n in CONST_NAMES], axis=1)),
        "conv_w": f(inp["conv_w"][0].reshape(-1)),
        "conv_b": f(inp["conv_b"][0].reshape(1, -1)),
        "dt_bias": f(inp["dt_bias"][0]),
        "a_log": f(inp["a_log"][0]),
        "d_skip": f(inp["d_skip"][0]),
        "w_out": f(inp["w_out"][0]),
        "gcat": f(np.concatenate([inp["gmlp_out_g"][0], inp["ssd_norm_g"][0]]).reshape(16, 128).T),
        "w_queryT": f(inp["w_query"][0].T),
        "sub_keysT": f(np.stack([inp["sub_keys1"][0].T, inp["sub_keys2"][0].T], axis=1)),
        "norm2_gc": f(inp["norm2_g"][0].reshape(8, 128).T),
    }
    return sh


def prep_experts(inp):
    f = lambda a: np.ascontiguousarray(np.asarray(a, dtype=np.float32))
    return {
        "expert_uT": f(np.asarray(inp["expert_u"][0]).T),
        "expert_v": f(inp["expert_v"][0]),
        "final_g": f(inp["final_g"]),
    }


_NC_CACHE = {}


def kernel(**inputs):
    x = np.asarray(inputs["x"], dtype=np.float32)
    B, L, _ = x.shape
    nseq = B // NCORES
    shared = prep_shared(inputs)
    shared.update(prep_experts(inputs))
    key = (nseq, L)
    if key not in _NC_CACHE:
        _NC_CACHE[key] = build(NSEQ=nseq, L=L)
    nc = _NC_CACHE[key]
    in_maps = []
    for c in range(NCORES):
        m = dict(shared)
        m["x"] = np.ascontiguousarray(x[c * nseq:(c + 1) * nseq].reshape(nseq * L, D))
        in_maps.append(m)
    res = run_bass_kernel_spmd(nc, in_maps, core_ids=list(range(NCORES)))
    outs = [np.asarray(res.results[c]["out"], dtype=np.float32).reshape(nseq, L, D) for c in range(NCORES)]
    return np.concatenate(outs, axis=0)
```

```python
import contextlib
import numpy as np
import ml_dtypes
import concourse.bass as bass
import concourse.mybir as mybir
from concourse.bass_utils import run_bass_kernel_spmd

F32 = mybir.dt.float32
BF16 = mybir.dt.bfloat16
U32 = mybir.dt.uint32
ALU = mybir.AluOpType
AF = mybir.ActivationFunctionType
AX = mybir.AxisListType

D = 1024
GMW = 1024
SSDW = 1024
NH = 16
HP = 64
NST = 128
CONVD = 1536
DIN = 4624
NEXP = 16384
EPS = 1e-6
NCORES = 8


class Tok:
    __slots__ = ("last_write", "reads", "name")

    def __init__(self, name=""):
        self.last_write = None
        self.reads = []
        self.name = name


class Op:
    __slots__ = ("eng", "fn", "deps", "signal", "seq", "dsem", "val")

    def __init__(self, eng, fn):
        self.eng = eng
        self.fn = fn
        self.deps = []
        self.signal = False
        self.seq = 0
        self.dsem = None
        self.val = 0


class DSem:
    def __init__(self, handle, group=False):
        self.handle = handle
        self.count = 0
        self.group = group


class Prog:
    ENGS = ("pe", "act", "dve", "pool", "sp")

    def __init__(self, nc, stack):
        self.nc = nc
        self.stack = stack
        self.ops = {e: [] for e in self.ENGS}
        self.esem = {e: stack.enter_context(nc.semaphore("es_" + e)) for e in self.ENGS}
        self.nsem = 0
        self.dmas = []

    def dsem(self, name=None, group=False):
        self.nsem += 1
        return DSem(self.stack.enter_context(self.nc.semaphore(name or ("ds%d" % self.nsem))), group)

    def op(self, eng, fn, reads=(), writes=(), dsem=None):
        if dsem is not None and dsem.group:
            dsem = self.dsem()
        o = Op(eng, fn)
        deps = []
        for t in reads:
            if t.last_write is not None:
                deps.append(t.last_write)
        for t in writes:
            if t.last_write is not None:
                deps.append(t.last_write)
            deps.extend(t.reads)
        o.deps = deps
        for d in deps:
            d.signal = True
        for t in reads:
            t.reads = [r for r in t.reads if not (r.eng == eng and r.dsem is None and dsem is None)]
            t.reads.append(o)
        for t in writes:
            t.last_write = o
            t.reads = []
        if dsem is not None:
            o.dsem = dsem
            dsem.count += 16
            o.val = dsem.count
        self.ops[eng].append(o)
        if dsem is not None:
            self.dmas.append(o)
        return o

    def barrier(self):
        lasts = [self.ops[e][-1] for e in self.ENGS if self.ops[e] and self.ops[e][-1].dsem is None]
        for e in self.ENGS:
            prev = [o for o in reversed(self.ops[e]) if o.dsem is None][:1]
            lasts_e = prev
        lasts = []
        for e in self.ENGS:
            for o in reversed(self.ops[e]):
                if o.dsem is None:
                    lasts.append(o)
                    break
        deps = lasts + list(self.dmas)
        self.dmas = []
        for e in self.ENGS:
            o = Op(e, lambda eng: eng.nop())
            o.deps = list(deps)
            for d in deps:
                d.signal = True
            self.ops[e].append(o)

    def emit(self, final_waits=()):
        nc = self.nc
        for e in self.ENGS:
            n = 0
            for o in self.ops[e]:
                if o.dsem is None and o.signal:
                    n += 1
                    o.seq = n
        esem = self.esem

        def run(ename, eng):
            waited = {}
            for o in self.ops[ename]:
                need = {}
                for d in o.deps:
                    if d.dsem is not None:
                        key = ("d", id(d.dsem))
                        h, v = d.dsem.handle, d.val
                    else:
                        if d.eng == ename and ename == "pe":
                            continue
                        key = ("e", d.eng)
                        h, v = esem[d.eng], d.seq
                    if waited.get(key, 0) >= v:
                        continue
                    if key not in need or need[key][1] < v:
                        need[key] = (h, v)
                for key, (h, v) in need.items():
                    eng.wait_ge(h, v)
                    waited[key] = v
                ins = o.fn(eng)
                if o.dsem is not None:
                    ins.then_inc(o.dsem.handle, 16)
                elif o.signal:
                    ins.then_inc(esem[ename], 1)
            if ename == "sp":
                for o in final_waits:
                    if o.dsem is not None:
                        eng.wait_ge(o.dsem.handle, o.val)
                    else:
                        eng.wait_ge(esem[o.eng], o.seq)

        for o in final_waits:
            o.signal = True
        for e in self.ENGS:
            n = 0
            for o in self.ops[e]:
                if o.dsem is None and o.signal:
                    n += 1
                    o.seq = n
        with nc.Block() as block:
            @block.sync
            def _(eng):
                run("sp", eng)

            @block.scalar
            def _(eng):
                run("act", eng)

            @block.vector
            def _(eng):
                run("dve", eng)

            @block.gpsimd
            def _(eng):
                run("pool", eng)

            @block.tensor
            def _(eng):
                run("pe", eng)


def host_consts():
    c = {}
    c["ident"] = np.eye(128, dtype=np.float32)
    s = np.arange(128)[:, None]
    t = np.arange(128)[None, :]
    c["triu"] = (s <= t).astype(np.float32)
    for k in range(3):
        sh = 3 - k
        c["shift%d" % k] = (s == t - sh).astype(np.float32)
        c["shiftp%d" % k] = (s == t - sh + 128).astype(np.float32)
    c["ones"] = np.ones((128, 128), dtype=np.float32)
    c["gt"] = (s > t).astype(np.float32)
    c["iota"] = np.broadcast_to(np.arange(128, dtype=np.float32)[None, :], (128, 128)).copy()
    return c


CONST_NAMES = ["ident", "triu", "shift0", "shift1", "shift2", "shiftp0", "shiftp1", "shiftp2", "iota", "ones", "gt"]


def build(NSEQ=4, L=2048, sweeps=(1, 2, 3, 4), debug=False):
    NCH = L // 128
    T = NSEQ * L
    NT = T // 128
    nc = bass.Bass("TRN2", target_bir_lowering=False)
    scratch_kind = "ExternalOutput" if debug else "Internal"

    in_names = []

    def din(name, shape, dt=F32):
        in_names.append(name)
        return nc.dram_tensor(name, list(shape), dt, kind="ExternalInput").ap()

    x_d = din("x", [T, D])
    w_in_d = din("w_in", [D, DIN])
    g1_d = din("norm1_gc", [128, 8])
    lng_d = din("gmlp_ln_g", [GMW])
    lnb_d = din("gmlp_ln_b", [GMW])
    wsT_d = din("gmlp_wsT", [128, 8, 128])
    bsT_d = din("gmlp_bsT", [128, 8])
    consts_d = din("consts", [128, len(CONST_NAMES) * 128])
    convw_d = din("conv_w", [4 * CONVD])
    convb_d = din("conv_b", [1, CONVD])
    dtb_d = din("dt_bias", [NH])
    alog_d = din("a_log", [NH])
    dskip_d = din("d_skip", [NH])
    if 3 in sweeps or 4 in sweeps:
        wout_d = din("w_out", [2048, D])
        gcat_d = din("gcat", [128, 16])
        wqT_d = din("w_queryT", [2048, D])
        kT_d = din("sub_keysT", [128, 2, 128])
        g2c_d = din("norm2_gc", [128, 8])
        if 4 in sweeps:
            uT_d = din("expert_uT", [D, NEXP])
            ev_d = din("expert_v", [NEXP, D])
            fg_d = din("final_g", [D])
            UTb_d = nc.dram_tensor("UTb", [32, 128, 8, 512], BF16, kind="Internal").ap()
            Vb_d = nc.dram_tensor("Vb", [32, 128, 4, 1024], BF16, kind="Internal").ap()
        x1_d = nc.dram_tensor("x1s", [T, D], F32, kind=scratch_kind).ap()
        h2T_d = nc.dram_tensor("h2Ts", [NT, 128, 1024], BF16, kind=scratch_kind).ap()
        rt_d = nc.dram_tensor("rts", [NT, 128, 384], BF16, kind=scratch_kind).ap()
    cat_d = nc.dram_tensor("cat", [T, 2048], BF16, kind=scratch_kind).ap()
    out_d = nc.dram_tensor("out", [T, D], F32, kind="ExternalOutput").ap()

    with contextlib.ExitStack() as stack:
        P = Prog(nc, stack)

        def sb(name, shape, dt=F32):
            return stack.enter_context(nc.sbuf_tensor(name, list(shape), dt))

        ps = stack.enter_context(nc.psum_tensor("ps", [128, 8, 512], F32))
        pstok = [Tok("ps%d" % i) for i in range(8)]

        finals = []
        cst = sb("cst", [128, len(CONST_NAMES) * 128])
        t_cst = Tok()
        ds_c = P.dsem()
        P.op("sp", lambda e: e.dma_start(out=cst[:, :], in_=consts_d[:, :]), writes=[t_cst], dsem=ds_c)

        def cf(name):
            i = CONST_NAMES.index(name)
            return cst[:, i * 128:(i + 1) * 128]

        ident_bf = sb("ident_bf", [128, 128], BF16)
        t_ident = Tok()
        P.op("dve", lambda e: e.tensor_copy(out=ident_bf[:, :], in_=cf("ident")), reads=[t_cst], writes=[t_ident])

        nhalf = sb("nhalf", [128, 16])
        t_nhalf = Tok()
        P.op("pool", lambda e: e.memset(nhalf[:, :], -0.5), writes=[t_nhalf])

        def rsqrt(out_ap, in_ap, toks_r, toks_w):
            n = in_ap.shape[-1]
            return P.op("pool", lambda e: e.tensor_tensor(out=out_ap, in0=in_ap, in1=nhalf[:, 0:n], op=ALU.pow),
                        reads=list(toks_r) + [t_nhalf], writes=list(toks_w))

        ds_stg = P.dsem()

        def prepass_gen(sbx):
            dve = lambda fn, r=(), w=(): P.op("dve", fn, r, w)
            act = lambda fn, r=(), w=(): P.op("act", fn, r, w)
            g2c4 = sbx("g2c4", [128, 8]); t_g2c4 = Tok(); ds_s4 = P.dsem(group=True)
            P.op("sp", lambda e: e.dma_start(out=g2c4[:, :], in_=g2c_d[:, :]), writes=[t_g2c4], dsem=ds_s4)
            cin = [sbx("cin%d" % i, [128, 4096]) for i in range(2)]; t_cin = [Tok(), Tok()]; ds_cin = [P.dsem(), P.dsem()]
            cout = [sbx("cout%d" % i, [128, 4096], BF16) for i in range(2)]; t_cout = [Tok(), Tok()]; ds_cout = [P.dsem(), P.dsem()]
            uT_v = uT_d.rearrange("(k p) e -> p k e", p=128)
            ev_v = ev_d.rearrange("(g b p) d -> g p b d", b=4, p=128)
            for it in range(64):
                bb = it % 2
                g = it // 2
                if it % 2 == 0:
                    P.op("sp", lambda e, g=g, bb=bb: e.dma_start(out=cin[bb][:, :].rearrange("p (k e) -> p k e", k=8),
                                                                 in_=uT_v[:, :, g * 512:(g + 1) * 512]),
                         writes=[t_cin[bb]], dsem=ds_cin[bb])
                    dve(lambda e, bb=bb: e.tensor_tensor(out=cout[bb][:, :].rearrange("p (k e) -> p k e", k=8),
                                                         in0=cin[bb][:, :].rearrange("p (k e) -> p k e", k=8),
                                                         in1=g2c4[:, :].unsqueeze(2).to_broadcast([128, 8, 512]), op=ALU.mult),
                        [t_cin[bb], t_g2c4], [t_cout[bb]])
                    P.op("pool", lambda e, g=g, bb=bb: e.dma_start(out=UTb_d[g, :, :, :].rearrange("p k e -> p (k e)"), in_=cout[bb][:, :]),
                         reads=[t_cout[bb]], writes=[], dsem=ds_cout[bb])
                else:
                    P.op("sp", lambda e, g=g, bb=bb: e.dma_start(out=cin[bb][:, :].rearrange("p (b d) -> p b d", b=4), in_=ev_v[g, :, :, :]),
                         writes=[t_cin[bb]], dsem=ds_cin[bb])
                    act(lambda e, bb=bb: e.activation(out=cout[bb][:, :], in_=cin[bb][:, :], func=AF.Copy), [t_cin[bb]], [t_cout[bb]])
                    P.op("pool", lambda e, g=g, bb=bb: e.dma_start(out=Vb_d[g, :, :, :].rearrange("p b d -> p (b d)"), in_=cout[bb][:, :]),
                         reads=[t_cout[bb]], writes=[], dsem=ds_cout[bb])
                yield

        def _sweep1():
            with contextlib.ExitStack() as s1:
                def sb1(name, shape, dt=F32):
                    return s1.enter_context(nc.sbuf_tensor(name, list(shape), dt))

                stg = sb1("stg_1", [128, 2576]); t_stg = Tok()

                w1 = sb1("w1", [128, 8, 2048], BF16)
                t_w1 = Tok()
                g1c = sb1("g1c", [128, 8])
                t_g1c = Tok()
                ds_small = P.dsem(group=True)
                P.op("sp", lambda e: e.dma_start(out=g1c[:, :], in_=g1_d[:, :]),
                     writes=[t_g1c], dsem=ds_small)
                for k in range(8):
                    P.op("sp", lambda e, k=k: e.dma_start(out=stg[:, 0:2048], in_=w_in_d[k * 128:(k + 1) * 128, 0:2048]),
                         writes=[t_stg], dsem=ds_stg)
                    P.op("dve", lambda e, k=k: e.tensor_scalar(out=w1[:, k, :], in0=stg[:, 0:2048], scalar1=g1c[:, k:k + 1],
                                                               scalar2=None, op0=ALU.mult),
                         reads=[t_stg, t_g1c], writes=[t_w1])
                WT = sb1("WT", [128, 8, 128], BF16)
                t_WT = Tok()
                P.op("sp", lambda e: e.dma_start(out=stg[:, 0:1024], in_=wsT_d.rearrange("s h t -> s (h t)")),
                     writes=[t_stg], dsem=ds_stg)
                P.op("dve", lambda e: e.tensor_tensor(out=WT[:, :, :], in0=stg[:, 0:1024].rearrange("s (h t) -> s h t", h=8),
                                                      in1=cf("triu").unsqueeze(1).to_broadcast([128, 8, 128]), op=ALU.mult),
                     reads=[t_stg, t_cst], writes=[t_WT])
                bsT = sb1("bsT", [128, 8])
                t_bsT = Tok()
                P.op("sp", lambda e: e.dma_start(out=bsT[:, :], in_=bsT_d[:, :]), writes=[t_bsT], dsem=ds_small)
                lngb = sb1("lngb", [128, 1024])
                lnbb = sb1("lnbb", [128, 1024])
                t_ln = Tok()
                P.op("sp", lambda e: e.dma_start(out=lngb[:, :], in_=lng_d.partition_broadcast(128)), writes=[t_ln], dsem=ds_small)
                P.op("sp", lambda e: e.dma_start(out=lnbb[:, :], in_=lnb_d.partition_broadcast(128)), writes=[t_ln], dsem=ds_small)

                NB = 2
                xt = [sb1("xt%d" % i, [128, 1024]) for i in range(NB)]
                t_xt = [Tok() for _ in range(NB)]
                ds_xt = [P.dsem() for _ in range(NB)]
                junk = sb1("junk", [128, 1024], BF16)
                t_junk = Tok()
                st = sb1("st", [128, 16])
                t_st = Tok()
                hb = sb1("hb", [128, 1024], BF16)
                t_hb = Tok()
                hT = sb1("hT", [128, 8, 128], BF16)
                t_hT = Tok()
                vtmp = sb1("vtmp", [128, 1024])
                t_vtmp = Tok()
                vb = sb1("vb", [128, 1024], BF16)
                t_vb = Tok()
                gm = sb1("gm", [128, 1024])
                t_gm = Tok()
                gmn = [sb1("gmn%d" % i, [128, 1024], BF16) for i in range(NB)]
                t_gmn = [Tok() for _ in range(NB)]
                ds_gmn = [P.dsem() for _ in range(NB)]

                ug2 = [sb1("ug2_%d" % i, [128, 1024]) for i in range(2)]; t_ug2 = [Tok(), Tok()]
                vg2 = [sb1("vg2_%d" % i, [128, 1024]) for i in range(2)]; t_vg2 = [Tok(), Tok()]
                stb = [sb1("stb_%d" % i, [128, 16]) for i in range(2)]; t_stb = [Tok(), Tok()]

                def front(ci):
                    r0 = ci * 128
                    b = ci % NB
                    P.op("sp", lambda e, b=b, r0=r0: e.dma_start(out=xt[b][:, :], in_=x_d[r0:r0 + 128, :]),
                         writes=[t_xt[b]], dsem=ds_xt[b])
                    P.op("act", lambda e, b=b: e.activation(out=junk[:, :], in_=xt[b][:, :], func=AF.Square, accum_out=st[:, 0:1]),
                         reads=[t_xt[b]], writes=[t_junk, t_st])
                    P.op("dve", lambda e: e.tensor_scalar(out=st[:, 1:2], in0=st[:, 0:1], scalar1=1.0 / D, scalar2=EPS,
                                                          op0=ALU.mult, op1=ALU.add), reads=[t_st], writes=[t_st])
                    rsqrt(st[:, 2:3], st[:, 1:2], [t_st], [t_st])
                    P.op("act", lambda e, b=b: e.activation(out=hb[:, :], in_=xt[b][:, :], func=AF.Copy, scale=st[:, 2:3]),
                         reads=[t_xt[b], t_st], writes=[t_hb])
                    yield
                    psT = ps[:, 7, :].bitcast(BF16)
                    for k in range(8):
                        P.op("pe", lambda e, k=k, psT=psT: e.transpose(out=psT[:, k * 128:(k + 1) * 128], in_=hb[:, k * 128:(k + 1) * 128],
                                                                       identity=ident_bf[:, :]),
                             reads=[t_hb, t_ident], writes=[pstok[7]])
                    P.op("dve", lambda e, psT=psT: e.tensor_copy(out=hT[:, :, :].rearrange("p k t -> p (k t)"), in_=psT[:, :]),
                         reads=[pstok[7]], writes=[t_hT])
                    yield
                    for j in range(4):
                        for k in range(8):
                            P.op("pe", lambda e, j=j, k=k: e.matmul(out=ps[:, j, :], lhsT=hT[:, k, :], rhs=w1[:, k, j * 512:(j + 1) * 512],
                                                                    start=(k == 0), stop=(k == 7)),
                                 reads=[t_hT, t_w1], writes=[pstok[j]])
                    yield
                    P.op("act", lambda e, b=b: e.activation(out=ug2[b][:, :], in_=ps[:, 0:2, :].rearrange("p a b -> p (a b)"), func=AF.Gelu),
                         reads=[pstok[0], pstok[1]], writes=[t_ug2[b]])
                    P.op("act", lambda e, b=b: e.activation(out=vg2[b][:, :], in_=ps[:, 2:4, :].rearrange("p a b -> p (a b)"), func=AF.Gelu,
                                                       accum_out=stb[b][:, 3:4]),
                         reads=[pstok[2], pstok[3]], writes=[t_vg2[b], t_stb[b]])
                    yield

                def back(ci):
                    r0 = ci * 128
                    b = ci % NB
                    P.op("act", lambda e, b=b: e.activation(out=junk[:, :], in_=vg2[b][:, :], func=AF.Square, accum_out=stb[b][:, 4:5]),
                         reads=[t_vg2[b]], writes=[t_junk, t_stb[b]])
                    P.op("dve", lambda e, b=b: e.tensor_scalar(out=stb[b][:, 5:6], in0=stb[b][:, 3:4], scalar1=1.0 / GMW, scalar2=None, op0=ALU.mult),
                         reads=[t_stb[b]], writes=[t_stb[b]])
                    P.op("dve", lambda e, b=b: e.tensor_tensor(out=stb[b][:, 6:7], in0=stb[b][:, 5:6], in1=stb[b][:, 5:6], op=ALU.mult),
                         reads=[t_stb[b]], writes=[t_stb[b]])
                    P.op("dve", lambda e, b=b: e.scalar_tensor_tensor(out=stb[b][:, 7:8], in0=stb[b][:, 4:5], scalar=1.0 / GMW, in1=stb[b][:, 6:7],
                                                                 op0=ALU.mult, op1=ALU.subtract),
                         reads=[t_stb[b]], writes=[t_stb[b]])
                    P.op("dve", lambda e, b=b: e.tensor_scalar(out=stb[b][:, 12:13], in0=stb[b][:, 7:8], scalar1=EPS, scalar2=None,
                                                          op0=ALU.add), reads=[t_stb[b]], writes=[t_stb[b]])
                    rsqrt(stb[b][:, 8:9], stb[b][:, 12:13], [t_stb[b]], [t_stb[b]])
                    P.op("dve", lambda e, b=b: e.scalar_tensor_tensor(out=vtmp[:, :], in0=vg2[b][:, :], scalar=stb[b][:, 5:6], in1=lngb[:, :],
                                                                 op0=ALU.subtract, op1=ALU.mult),
                         reads=[t_vg2[b], t_stb[b], t_ln], writes=[t_vtmp])
                    P.op("dve", lambda e, b=b: e.scalar_tensor_tensor(out=vb[:, :], in0=vtmp[:, :], scalar=stb[b][:, 8:9], in1=lnbb[:, :],
                                                                 op0=ALU.mult, op1=ALU.add),
                         reads=[t_vtmp, t_stb[b], t_ln], writes=[t_vb])
                    yield
                    for h in range(8):
                        P.op("pe", lambda e, h=h: e.matmul(out=ps[:, 4 + h // 4, (h % 4) * 128:(h % 4 + 1) * 128], lhsT=WT[:, h, :],
                                                           rhs=vb[:, h * 128:(h + 1) * 128], start=True, stop=True),
                             reads=[t_WT, t_vb], writes=[pstok[4 + h // 4]])
                    P.op("dve", lambda e, b=b: e.tensor_tensor(out=gm[:, :].rearrange("p (h d) -> p h d", h=8),
                                                          in0=ps[:, 4:6, :].rearrange("p a (h d) -> p (a h) d", d=128),
                                                          in1=bsT[:, :].unsqueeze(2).to_broadcast([128, 8, 128]), op=ALU.add),
                         reads=[pstok[4], pstok[5], t_bsT], writes=[t_gm])
                    yield
                    P.op("dve", lambda e, b=b: e.tensor_tensor(out=gm[:, :], in0=gm[:, :], in1=ug2[b][:, :], op=ALU.mult),
                         reads=[t_gm, t_ug2[b]], writes=[t_gm])
                    P.op("act", lambda e, b=b: e.activation(out=junk[:, :], in_=gm[:, :], func=AF.Square, accum_out=stb[b][:, 9:10]),
                         reads=[t_gm], writes=[t_junk, t_stb[b]])
                    P.op("dve", lambda e, b=b: e.tensor_scalar(out=stb[b][:, 10:11], in0=stb[b][:, 9:10], scalar1=1.0 / GMW, scalar2=EPS,
                                                          op0=ALU.mult, op1=ALU.add), reads=[t_stb[b]], writes=[t_stb[b]])
                    rsqrt(stb[b][:, 11:12], stb[b][:, 10:11], [t_stb[b]], [t_stb[b]])
                    yield
                    P.op("act", lambda e, b=b: e.activation(out=gmn[b][:, :], in_=gm[:, :], func=AF.Copy, scale=stb[b][:, 11:12]),
                         reads=[t_gm, t_stb[b]], writes=[t_gmn[b]])
                    finals.append(P.op("pool", lambda e, b=b, r0=r0: e.dma_start(out=cat_d[r0:r0 + 128, 0:1024], in_=gmn[b][:, :]),
                                       reads=[t_gmn[b]], writes=[], dsem=ds_gmn[b]))


                    yield

                def interleave(g1, g2):
                    gens = [g for g in (g1, g2) if g is not None]
                    while gens:
                        for g in list(gens):
                            try:
                                next(g)
                            except StopIteration:
                                gens.remove(g)

                pp = prepass_gen(sb1) if 4 in sweeps else None
                interleave(front(0), None)
                for ci in range(NT):
                    interleave(back(ci), front(ci + 1) if ci + 1 < NT else None)
                    if pp is not None:
                        for _ in range(-(-64 // NT)):
                            next(pp, None)
                if pp is not None:
                    for _ in pp:
                        pass

        def _sweep2():
            P.barrier()
            with contextlib.ExitStack() as s2:
                def sb2(name, shape, dt=F32):
                    return s2.enter_context(nc.sbuf_tensor(name, list(shape), dt))

                stg = sb2("stg_2", [128, 2576]); t_stg = Tok()

                dve = lambda fn, r=(), w=(): P.op("dve", fn, r, w)
                act = lambda fn, r=(), w=(): P.op("act", fn, r, w)
                pe = lambda fn, r=(), w=(): P.op("pe", fn, r, w)
                pool = lambda fn, r=(), w=(): P.op("pool", fn, r, w)

                ds_small2 = P.dsem(group=True)
                w2 = sb2("w2", [128, 8, 2576], BF16)
                t_w2 = Tok()
                g1c2 = sb2("g1c2", [128, 8])
                t_g1c2 = Tok()
                P.op("sp", lambda e: e.dma_start(out=g1c2[:, :], in_=g1_d[:, :]), writes=[t_g1c2], dsem=ds_small2)
                for k in range(8):
                    P.op("sp", lambda e, k=k: e.dma_start(out=stg[:, 0:2576], in_=w_in_d[k * 128:(k + 1) * 128, 2048:4624]),
                         writes=[t_stg], dsem=ds_stg)
                    dve(lambda e, k=k: e.tensor_scalar(out=w2[:, k, :], in0=stg[:, 0:2576], scalar1=g1c2[:, k:k + 1],
                                                       scalar2=None, op0=ALU.mult), [t_stg, t_g1c2], [t_w2])
                shb = sb2("shb", [128, 6, 128], BF16)
                t_shb = Tok()
                i0 = CONST_NAMES.index("shift0")
                dve(lambda e: e.tensor_copy(out=shb[:, :, :].rearrange("p a b -> p (a b)"), in_=cst[:, i0 * 128:(i0 + 6) * 128]),
                    [t_cst], [t_shb])
                onesb = sb2("onesb", [128, 128], BF16)
                t_onesb = Tok()
                dve(lambda e: e.tensor_copy(out=onesb[:, :], in_=cf("ones")), [t_cst], [t_onesb])
                convb = sb2("convb", [1, CONVD], BF16)
                t_convb = Tok()
                P.op("sp", lambda e: e.dma_start(out=stg[0:1, 0:CONVD], in_=convb_d[:, :]), writes=[t_stg], dsem=ds_stg)
                dve(lambda e: e.tensor_copy(out=convb[:, :], in_=stg[0:1, 0:CONVD]), [t_stg], [t_convb])
                convw = sb2("convw", [128, 4, CONVD])
                t_convw = Tok()
                P.op("sp", lambda e: e.dma_start(out=convw[:, :, :].rearrange("p k c -> p (k c)"), in_=convw_d.partition_broadcast(128)),
                     writes=[t_convw], dsem=ds_small2)
                sm = sb2("sm", [128, 4, 16])
                t_sm = Tok()
                P.op("sp", lambda e: e.dma_start(out=sm[:, 0, :], in_=dtb_d.partition_broadcast(128)), writes=[t_sm], dsem=ds_small2)
                P.op("sp", lambda e: e.dma_start(out=sm[:, 1, :], in_=alog_d.partition_broadcast(128)), writes=[t_sm], dsem=ds_small2)
                P.op("sp", lambda e: e.dma_start(out=sm[:, 2, :], in_=dskip_d.partition_broadcast(128)), writes=[t_sm], dsem=ds_small2)
                act(lambda e: e.activation(out=sm[:, 1, :], in_=sm[:, 1, :], func=AF.Exp), [t_sm], [t_sm])
                dve(lambda e: e.tensor_scalar(out=sm[:, 1, :], in0=sm[:, 1, :], scalar1=-1.0, scalar2=None, op0=ALU.mult), [t_sm], [t_sm])

                xt = sb2("xt_2", [128, 1024]); t_xt = Tok(); ds_xt = P.dsem()
                junkF = sb2("junkF_2", [128, 1024], BF16); t_junkF = Tok()
                junkB = sb2("junkB_2", [128, 512], BF16); t_junkB = Tok()
                stF = sb2("stF_2", [128, 4]); t_stF = Tok()
                stB = sb2("stB_2", [128, 16]); t_stB = Tok()
                hb = sb2("hb_2", [128, 1024], BF16); t_hb = Tok()
                hT = sb2("hT_2", [128, 8, 128], BF16); t_hT = Tok()
                zs = [sb2("zs%d" % i, [128, 1024]) for i in range(2)]; t_zs = [Tok(), Tok()]
                xw = [sb2("xw%d" % i, [128, 4, CONVD], BF16) for i in range(2)]; t_xw = [Tok(), Tok()]
                xs = [sb2("xs%d" % i, [128, 16, 64]) for i in range(2)]; t_xs = [Tok(), Tok()]
                bc = [sb2("bc%d" % i, [128, 4, 128], BF16) for i in range(2)]; t_bc = [Tok(), Tok()]
                bcT = [sb2("bcT%d" % i, [128, 4, 128], BF16) for i in range(2)]; t_bcT = [Tok(), Tok()]
                dF = [sb2("dF%d" % i, [128, 4, 16]) for i in range(2)]; t_dF = [Tok(), Tok()]
                dtv = sb2("dtv", [128, 8, 16]); t_dtv = Tok()
                R = sb2("R", [128, 16, 128]); t_R = Tok()
                LT = sb2("LT", [128, 16, 128], BF16); t_LT = Tok()
                cbm = sb2("cbm", [128, 2, 128], BF16); t_cbm = Tok()
                MT = sb2("MT", [128, 16, 128], BF16); t_MT = Tok()
                xdt = sb2("xdt", [128, 16, 64], BF16); t_xdt = Tok()
                xdtd = sb2("xdtd", [128, 16, 64], BF16); t_xdtd = Tok()
                stT = sb2("stT", [128, 16, 64]); t_stT = Tok()
                stTb = sb2("stTb", [128, 16, 64], BF16); t_stTb = Tok()
                yy = sb2("yy", [128, 16, 64]); t_yy = Tok()
                so = [sb2("so%d" % i, [128, 1024], BF16) for i in range(2)]; t_so = [Tok(), Tok()]
                ds_so = [P.dsem(), P.dsem()]
                ps3b = ps[:, 3, :].bitcast(BF16)
                flat = lambda ap: ap.rearrange("p a b -> p (a b)")

                def front(ci):
                    r0 = ci * 128
                    par = ci % 2
                    first = (ci % NCH == 0)
                    P.op("sp", lambda e, r0=r0: e.dma_start(out=xt[:, :], in_=x_d[r0:r0 + 128, :]), writes=[t_xt], dsem=ds_xt)
                    act(lambda e: e.activation(out=junkF[:, :], in_=xt[:, :], func=AF.Square, accum_out=stF[:, 0:1]), [t_xt], [t_junkF, t_stF])
                    dve(lambda e: e.tensor_scalar(out=stF[:, 1:2], in0=stF[:, 0:1], scalar1=1.0 / D, scalar2=EPS, op0=ALU.mult, op1=ALU.add),
                        [t_stF], [t_stF])
                    rsqrt(stF[:, 2:3], stF[:, 1:2], [t_stF], [t_stF])
                    act(lambda e: e.activation(out=hb[:, :], in_=xt[:, :], func=AF.Copy, scale=stF[:, 2:3]), [t_xt, t_stF], [t_hb])
                    yield
                    for k in range(8):
                        pe(lambda e, k=k: e.transpose(out=ps3b[:, k * 128:(k + 1) * 128], in_=hb[:, k * 128:(k + 1) * 128], identity=ident_bf[:, :]),
                           [t_hb, t_ident], [pstok[3]])
                    act(lambda e: e.activation(out=hT[:, :, :].rearrange("p k t -> p (k t)"), in_=ps3b[:, :], func=AF.Copy), [pstok[3]], [t_hT])
                    yield
                    for j in range(2):
                        for k in range(8):
                            pe(lambda e, j=j, k=k: e.matmul(out=ps[:, j, :], lhsT=hT[:, k, :], rhs=w2[:, k, j * 512:(j + 1) * 512],
                                                            start=(k == 0), stop=(k == 7)), [t_hT, t_w2], [pstok[j]])
                    for k in range(8):
                        pe(lambda e, k=k: e.matmul(out=ps[:, 2, 0:16], lhsT=hT[:, k, :], rhs=w2[:, k, 2560:2576],
                                                   start=(k == 0), stop=(k == 7)), [t_hT, t_w2], [pstok[2]])
                    yield
                    act(lambda e, par=par: e.activation(out=zs[par][:, :], in_=ps[:, 0:2, :].rearrange("p a b -> p (a b)"), func=AF.Silu),
                        [pstok[0], pstok[1]], [t_zs[par]])
                    dve(lambda e, par=par: e.tensor_tensor(out=dF[par][:, 2, :], in0=ps[:, 2, 0:16], in1=sm[:, 0, :], op=ALU.add), [pstok[2], t_sm], [t_dF[par]])
                    yield
                    for j in range(3):
                        for k in range(8):
                            pe(lambda e, j=j, k=k: e.matmul(out=ps[:, j, :], lhsT=hT[:, k, :], rhs=w2[:, k, 1024 + j * 512:1024 + (j + 1) * 512],
                                                            start=(k == 0), stop=(k == 7)), [t_hT, t_w2], [pstok[j]])
                    yield
                    dve(lambda e, par=par: e.tensor_scalar(out=dF[par][:, 3, :], in0=dF[par][:, 2, :], scalar1=-1.0, scalar2=None, op0=ALU.mult), [t_dF[par]], [t_dF[par]])
                    dve(lambda e, par=par: e.tensor_tensor(out=dF[par][:, 3, :], in0=dF[par][:, 3, :], in1=dF[par][:, 2, :], op=ALU.max), [t_dF[par]], [t_dF[par]])
                    act(lambda e, par=par: e.activation(out=dF[par][:, 3, :], in_=dF[par][:, 3, :], func=AF.Exp, scale=-1.0), [t_dF[par]], [t_dF[par]])
                    act(lambda e, par=par: e.activation(out=dF[par][:, 3, :], in_=dF[par][:, 3, :], func=AF.Ln, bias=1.0), [t_dF[par]], [t_dF[par]])
                    dve(lambda e, par=par: e.scalar_tensor_tensor(out=dF[par][:, 0, :], in0=dF[par][:, 2, :], scalar=0.0, in1=dF[par][:, 3, :],
                                                                  op0=ALU.max, op1=ALU.add), [t_dF[par]], [t_dF[par]])
                    dve(lambda e, par=par: e.tensor_tensor(out=dF[par][:, 1, :], in0=dF[par][:, 0, :], in1=sm[:, 1, :], op=ALU.mult), [t_dF[par], t_sm], [t_dF[par]])
                    yield
                    for k in range(4):
                        dve(lambda e, k=k, par=par: e.tensor_tensor(out=xw[par][:, k, :], in0=ps[:, 0:3, :].rearrange("p a b -> p (a b)"),
                                                                    in1=convw[:, k, :], op=ALU.mult),
                            [pstok[0], pstok[1], pstok[2], t_convw], [t_xw[par]])
                        if k == 1:
                            yield
                    yield
                    for j in range(3):
                        sl = slice(j * 512, (j + 1) * 512)
                        pe(lambda e, j=j, sl=sl, par=par: e.matmul(out=ps[:, j, :], lhsT=ident_bf[:, :], rhs=xw[par][:, 3, sl], start=True, stop=False),
                           [t_ident, t_xw[par]], [pstok[j]])
                        for k in range(3):
                            pe(lambda e, j=j, sl=sl, k=k, par=par: e.matmul(out=ps[:, j, :], lhsT=shb[:, k, :], rhs=xw[par][:, k, sl], start=False, stop=False),
                               [t_shb, t_xw[par]], [pstok[j]])
                        if not first:
                            for k in range(3):
                                pe(lambda e, j=j, sl=sl, k=k, par=par: e.matmul(out=ps[:, j, :], lhsT=shb[:, 3 + k, :], rhs=xw[1 - par][:, k, sl],
                                                                                start=False, stop=False),
                                   [t_shb, t_xw[1 - par]], [pstok[j]])
                        pe(lambda e, j=j, sl=sl: e.matmul(out=ps[:, j, :], lhsT=onesb[0:1, :], rhs=convb[0:1, sl], start=False, stop=True),
                           [t_onesb, t_convb], [pstok[j]])
                    yield
                    act(lambda e, par=par: e.activation(out=flat(xs[par][:, :, :]), in_=ps[:, 0:2, :].rearrange("p a b -> p (a b)"), func=AF.Silu),
                        [pstok[0], pstok[1]], [t_xs[par]])
                    act(lambda e, par=par: e.activation(out=flat(bc[par][:, :, :]), in_=ps[:, 2, :], func=AF.Silu), [pstok[2]], [t_bc[par]])
                    yield
                    for i in range(4):
                        pe(lambda e, i=i, par=par: e.transpose(out=ps3b[:, i * 128:(i + 1) * 128], in_=bc[par][:, i, :], identity=ident_bf[:, :]),
                           [t_bc[par], t_ident], [pstok[3]])
                    act(lambda e, par=par: e.activation(out=flat(bcT[par][:, :, :]), in_=ps3b[:, 0:512], func=AF.Copy), [pstok[3]], [t_bcT[par]])
                    yield

                def back(ci):
                    r0 = ci * 128
                    par = ci % 2
                    first = (ci % NCH == 0)
                    last = (ci % NCH == NCH - 1)
                    dt_ = dF[par][:, 0, :]
                    a_ = dF[par][:, 1, :]
                    pe(lambda e, a_=a_: e.matmul(out=ps[:, 4, 16:32], lhsT=cf("triu"), rhs=a_, start=True, stop=True), [t_cst, t_dF[par]], [pstok[4]])
                    dve(lambda e: e.tensor_copy(out=dtv[:, 2, :], in_=ps[:, 4, 16:32]), [pstok[4]], [t_dtv])
                    for g in range(2):
                        pe(lambda e, g=g, par=par: e.matmul(out=ps[:, 4, 256 + g * 128:256 + (g + 1) * 128], lhsT=bcT[par][:, g, :], rhs=bcT[par][:, 2 + g, :],
                                                            start=True, stop=True), [t_bcT[par]], [pstok[4]])
                    dve(lambda e: e.tensor_tensor(out=cbm[:, :, :], in0=ps[:, 4, 256:512].rearrange("p (g t) -> p g t", g=2),
                                                  in1=cf("triu").unsqueeze(1).to_broadcast([128, 2, 128]), op=ALU.mult),
                        [pstok[4], t_cst], [t_cbm])
                    dve(lambda e, a_=a_: e.tensor_tensor(out=R[:, :, :], in0=cf("triu").unsqueeze(1).to_broadcast([128, 16, 128]),
                                                         in1=a_.unsqueeze(2).to_broadcast([128, 16, 128]), op=ALU.mult),
                        [t_cst, t_dF[par]], [t_R])
                    yield
                    ab = [4, 5, 6, 7]
                    for q in range(4):
                        pe(lambda e, q=q: e.matmul(out=ps[:, ab[q], :], lhsT=cf("gt"), rhs=R[:, 4 * q:4 * q + 4, :].rearrange("p a b -> p (a b)"),
                                                   start=True, stop=True), [t_cst, t_R], [pstok[ab[q]]])
                    yield
                    for q in range(4):
                        dve(lambda e, q=q: e.tensor_copy(out=dtv[:, 3, 4 * q:4 * q + 4],
                                                         in_=ps[:, ab[q], :].rearrange("p (h t) -> p h t", h=4)[:, :, 127]),
                            [pstok[ab[q]]], [t_dtv])
                    yield
                    act(lambda e: e.activation(out=flat(LT[:, :, :]), in_=ps[:, 4:8, :].rearrange("p a b -> p (a b)"), func=AF.Exp),
                        [pstok[4], pstok[5], pstok[6], pstok[7]], [t_LT])
                    dve(lambda e: e.tensor_tensor(out=dtv[:, 7, :], in0=dtv[:, 3, :], in1=dtv[:, 2, :], op=ALU.add), [t_dtv], [t_dtv])
                    act(lambda e: e.activation(out=dtv[:, 4, :], in_=dtv[:, 3, :], func=AF.Exp), [t_dtv], [t_dtv])
                    act(lambda e: e.activation(out=dtv[:, 5, :], in_=dtv[:, 7, :], func=AF.Exp), [t_dtv], [t_dtv])
                    act(lambda e: e.activation(out=dtv[:, 6, :], in_=dtv[:, 2, :], func=AF.Exp), [t_dtv], [t_dtv])
                    dve(lambda e, dt_=dt_: e.tensor_tensor(out=dtv[:, 4, :], in0=dtv[:, 4, :], in1=dt_, op=ALU.mult), [t_dtv, t_dF[par]], [t_dtv])
                    yield
                    for g in range(2):
                        dve(lambda e, g=g: e.tensor_tensor(out=MT[:, 8 * g:8 * g + 8, :], in0=LT[:, 8 * g:8 * g + 8, :],
                                                           in1=cbm[:, g, :].unsqueeze(1).to_broadcast([128, 8, 128]), op=ALU.mult),
                            [t_LT, t_cbm], [t_MT])
                    dve(lambda e, par=par, dt_=dt_: e.tensor_tensor(out=xdt[:, :, :], in0=xs[par][:, :, :], in1=dt_.unsqueeze(2).to_broadcast([128, 16, 64]), op=ALU.mult),
                        [t_xs[par], t_dF[par]], [t_xdt])
                    yield
                    dve(lambda e, par=par: e.tensor_tensor(out=xdtd[:, :, :], in0=xs[par][:, :, :], in1=dtv[:, 4, :].unsqueeze(2).to_broadcast([128, 16, 64]), op=ALU.mult),
                        [t_xs[par], t_dtv], [t_xdtd])
                    for h in range(16):
                        pe(lambda e, h=h: e.matmul(out=ps[:, 4 + h // 8, (h % 8) * 64:(h % 8 + 1) * 64], lhsT=MT[:, h, :], rhs=xdt[:, h, :],
                                                   start=True, stop=True), [t_MT, t_xdt], [pstok[4 + h // 8]])
                    if not first:
                        for g in range(2):
                            pe(lambda e, g=g, par=par: e.matmul(out=ps[:, 6 + g, :], lhsT=bcT[par][:, 2 + g, :], rhs=flat(stTb[:, 8 * g:8 * g + 8, :]),
                                                                start=True, stop=True), [t_bcT[par], t_stTb], [pstok[6 + g]])
                        yield
                        dve(lambda e: e.tensor_tensor(out=yy[:, :, :], in0=ps[:, 6:8, :].rearrange("p a (h d) -> p (a h) d", d=64),
                                                      in1=dtv[:, 6, :].unsqueeze(2).to_broadcast([128, 16, 64]), op=ALU.mult),
                            [pstok[6], pstok[7], t_dtv], [t_yy])
                        dve(lambda e: e.tensor_tensor(out=flat(yy[:, :, :]), in0=flat(yy[:, :, :]),
                                                      in1=ps[:, 4:6, :].rearrange("p a b -> p (a b)"), op=ALU.add),
                            [pstok[4], pstok[5], t_yy], [t_yy])
                    else:
                        yield
                        dve(lambda e: e.tensor_copy(out=flat(yy[:, :, :]), in_=ps[:, 4:6, :].rearrange("p a b -> p (a b)")),
                            [pstok[4], pstok[5]], [t_yy])
                    yield
                    if not last:
                        for g in range(2):
                            pe(lambda e, g=g, par=par: e.matmul(out=ps[:, 4 + g, :], lhsT=bc[par][:, g, :], rhs=flat(xdtd[:, 8 * g:8 * g + 8, :]),
                                                                start=True, stop=True), [t_bc[par], t_xdtd], [pstok[4 + g]])
                        if first:
                            dve(lambda e: e.tensor_copy(out=flat(stT[:, :, :]), in_=ps[:, 4:6, :].rearrange("p a b -> p (a b)")),
                                [pstok[4], pstok[5]], [t_stT])
                        else:
                            dve(lambda e: e.tensor_tensor(out=stT[:, :, :], in0=stT[:, :, :], in1=dtv[:, 5, :].unsqueeze(2).to_broadcast([128, 16, 64]), op=ALU.mult),
                                [t_stT, t_dtv], [t_stT])
                            dve(lambda e: e.tensor_tensor(out=flat(stT[:, :, :]), in0=flat(stT[:, :, :]),
                                                          in1=ps[:, 4:6, :].rearrange("p a b -> p (a b)"), op=ALU.add),
                                [pstok[4], pstok[5], t_stT], [t_stT])
                        pool(lambda e: e.tensor_copy(out=stTb[:, :, :], in_=stT[:, :, :]), [t_stT], [t_stTb])
                    yield
                    dve(lambda e, par=par: e.tensor_tensor(out=xs[par][:, :, :], in0=xs[par][:, :, :], in1=sm[:, 2, :].unsqueeze(2).to_broadcast([128, 16, 64]), op=ALU.mult),
                        [t_xs[par], t_sm], [t_xs[par]])
                    dve(lambda e, par=par: e.tensor_tensor(out=yy[:, :, :], in0=yy[:, :, :], in1=xs[par][:, :, :], op=ALU.add), [t_yy, t_xs[par]], [t_yy])
                    yield
                    dve(lambda e, par=par: e.tensor_tensor(out=flat(yy[:, :, :]), in0=flat(yy[:, :, :]), in1=zs[par][:, :], op=ALU.mult), [t_yy, t_zs[par]], [t_yy])
                    for g in range(2):
                        act(lambda e, g=g: e.activation(out=junkB[:, 0:512], in_=flat(yy[:, 8 * g:8 * g + 8, :]),
                                                        func=AF.Square, accum_out=stB[:, 4 + g:5 + g]), [t_yy], [t_junkB, t_stB])
                    dve(lambda e: e.tensor_scalar(out=stB[:, 6:8], in0=stB[:, 4:6], scalar1=1.0 / 512, scalar2=EPS, op0=ALU.mult, op1=ALU.add),
                        [t_stB], [t_stB])
                    rsqrt(stB[:, 8:10], stB[:, 6:8], [t_stB], [t_stB])
                    yield
                    dve(lambda e, par=par: e.tensor_tensor(out=so[par][:, :].rearrange("p (g d) -> p g d", g=2),
                                                           in0=yy[:, :, :].rearrange("p (g a) b -> p g (a b)", g=2),
                                                           in1=stB[:, 8:10].unsqueeze(2).to_broadcast([128, 2, 512]), op=ALU.mult),
                        [t_yy, t_stB], [t_so[par]])
                    finals.append(P.op("pool", lambda e, par=par, r0=r0: e.dma_start(out=cat_d[r0:r0 + 128, 1024:2048], in_=so[par][:, :]),
                                       reads=[t_so[par]], writes=[], dsem=ds_so[par]))
                    yield

                def interleave(g1, g2):
                    gens = [g for g in (g1, g2) if g is not None]
                    while gens:
                        for g in list(gens):
                            try:
                                next(g)
                            except StopIteration:
                                gens.remove(g)

                interleave(front(0), None)
                for ci in range(NT):
                    interleave(back(ci), front(ci + 1) if ci + 1 < NT else None)

        def _sweep3():
            P.barrier()
            with contextlib.ExitStack() as s3:
                def sb3(name, shape, dt=F32):
                    return s3.enter_context(nc.sbuf_tensor(name, list(shape), dt))

                stg = sb3("stg_3", [128, 2576]); t_stg = Tok()

                dve = lambda fn, r=(), w=(): P.op("dve", fn, r, w)
                act = lambda fn, r=(), w=(): P.op("act", fn, r, w)
                pe = lambda fn, r=(), w=(): P.op("pe", fn, r, w)
                pool = lambda fn, r=(), w=(): P.op("pool", fn, r, w)
                ds_small3 = P.dsem(group=True)
                wo = sb3("wo", [128, 16, 1024], BF16); t_wo = Tok()
                gcat = sb3("gcat_s", [128, 16]); t_gcat = Tok()
                P.op("sp", lambda e: e.dma_start(out=gcat[:, :], in_=gcat_d[:, :]), writes=[t_gcat], dsem=ds_small3)
                Wc = sb3("Wc", [128, 8, 2048], BF16); t_Wc = Tok()
                g2c = sb3("g2c", [128, 8]); t_g2c = Tok()
                P.op("sp", lambda e: e.dma_start(out=g2c[:, :], in_=g2c_d[:, :]), writes=[t_g2c], dsem=ds_small3)
                kT = sb3("kT", [128, 2, 128]); t_kT = Tok()
                P.op("sp", lambda e: e.dma_start(out=kT[:, :, :], in_=kT_d[:, :, :]), writes=[t_kT], dsem=ds_small3)
                stgq = [sb3("stgq%d" % i, [128, 1024]) for i in range(2)]; t_stgq = [Tok(), Tok()]; ds_q = [P.dsem(), P.dsem()]
                for j in range(16):
                    qb = j % 2
                    P.op("sp", lambda e, k=j: e.dma_start(out=stg[:, 0:1024], in_=wout_d[k * 128:(k + 1) * 128, :]), writes=[t_stg], dsem=ds_stg)
                    dve(lambda e, k=j: e.tensor_scalar(out=wo[:, k, :], in0=stg[:, 0:1024], scalar1=gcat[:, k:k + 1], scalar2=None, op0=ALU.mult),
                        [t_stg, t_gcat], [t_wo])
                    P.op("sp", lambda e, j=j, qb=qb: e.dma_start(out=stgq[qb][:, :], in_=wqT_d[j * 128:(j + 1) * 128, :]),
                         writes=[t_stgq[qb]], dsem=ds_q[qb])
                    b0 = qb * 2
                    for k in range(8):
                        pe(lambda e, j=j, k=k, qb=qb, b0=b0: e.matmul(out=ps[:, b0 + k // 4, (k % 4) * 128:(k % 4 + 1) * 128],
                                                                      lhsT=stgq[qb][:, k * 128:(k + 1) * 128], rhs=kT[:, j % 2, :], start=True, stop=True),
                           [t_stgq[qb], t_kT], [pstok[b0 + k // 4]])
                    dve(lambda e, j=j, b0=b0: e.tensor_tensor(out=Wc[:, :, j * 128:(j + 1) * 128],
                                                              in0=ps[:, b0:b0 + 2, :].rearrange("p a (k n) -> p (a k) n", n=128),
                                                              in1=g2c[:, :].unsqueeze(2).to_broadcast([128, 8, 128]), op=ALU.mult),
                        [pstok[b0], pstok[b0 + 1], t_g2c], [t_Wc])

                catb = sb3("catb", [128, 2048], BF16); t_catb = Tok(); ds_catb = P.dsem()
                xt = sb3("xt_3", [128, 1024]); t_xt = Tok(); ds_xt = P.dsem()
                catT = sb3("catT", [128, 16, 128], BF16); t_catT = Tok()
                x1 = [sb3("x1_%d" % i, [128, 1024]) for i in range(2)]; t_x1 = [Tok(), Tok()]; ds_x1 = [P.dsem(), P.dsem()]
                junk = sb3("junk_3", [128, 1024], BF16); t_junk = Tok()
                st = sb3("st_3", [128, 16]); t_st = Tok()
                h2b = sb3("h2b", [128, 1024], BF16); t_h2b = Tok()
                h2T = [sb3("h2T_%d" % i, [128, 8, 128], BF16) for i in range(2)]; t_h2T = [Tok(), Tok()]; ds_h2T = [P.dsem(), P.dsem()]
                Swk = sb3("Swk", [128, 16, 128]); t_Swk = Tok()
                vv = sb3("vv", [128, 16, 16]); t_vv = Tok()
                tg_vv = [Tok() for _ in range(16)]; tg_vv2 = [Tok() for _ in range(16)]; tg_Swk = [Tok() for _ in range(16)]
                tg_idx = [Tok() for _ in range(16)]; tg_idx2 = [Tok() for _ in range(16)]
                th_c16 = [Tok() for _ in range(8)]; th_c16b = [Tok() for _ in range(8)]; th_candw = [Tok() for _ in range(8)]
                th_cpos = [Tok() for _ in range(8)]; th_cposb = [Tok() for _ in range(8)]
                idx = sb3("idx", [128, 16, 16], U32); t_idx = Tok()
                idxf = sb3("idxf", [128, 16, 16], BF16); t_idxf = Tok()
                cand = sb3("cand", [128, 8, 112]); t_cand = Tok()
                candw = sb3("candw", [128, 8, 112]); t_candw = Tok()
                c16 = sb3("c16", [128, 8, 16]); t_c16 = Tok()
                cpos = sb3("cpos", [128, 8, 16], U32); t_cpos = Tok()
                cpi = sb3("cpi", [128, 4, 8, 16], U32); t_cpi = Tok()
                cpf4 = sb3("cpf4", [128, 4, 8, 16]); t_cpf4 = Tok()
                cpt = sb3("cpt", [128, 3, 8, 16]); t_cpt = Tok()
                cpf = sb3("cpf", [128, 2, 8, 16]); t_cpf = Tok()
                ohs = [sb3("oh%d" % i, [128, 8, 16, 16], BF16) for i in range(2)]; t_ohs = [Tok(), Tok()]
                gt = sb3("gt", [128, 8, 16]); t_gt = Tok()
                z8 = sb3("z8", [128, 8]); t_z8 = Tok()
                e12 = sb3("e12", [128, 2, 8, 16]); t_e12 = Tok()
                rtm = sb3("rtm", [128, 3, 128], BF16); t_rtm = Tok()
                rt = [sb3("rt_%d" % i, [128, 3, 128], BF16) for i in range(2)]; t_rt = [Tok(), Tok()]; ds_rt = [P.dsem(), P.dsem()]
                psT = ps[:, 7, :].bitcast(BF16)
                ps6b = ps[:, 6, :].bitcast(BF16)
                iota16 = cf("iota")[:, 0:16]

                ps4b = ps[:, 4, :].bitcast(BF16)
                Ssb2 = [sb3("Ssb2_%d" % i, [128, 16, 128]) for i in range(2)]; t_Ssb2 = [Tok(), Tok()]

                def front(ci):
                    r0 = ci * 128
                    par = ci % 2
                    P.op("sp", lambda e, r0=r0: e.dma_start(out=catb[:, :], in_=cat_d[r0:r0 + 128, :]), writes=[t_catb], dsem=ds_catb)
                    P.op("sp", lambda e, r0=r0: e.dma_start(out=xt[:, :], in_=x_d[r0:r0 + 128, :]), writes=[t_xt], dsem=ds_xt)
                    for k in range(16):
                        pb = psT if k < 8 else ps6b
                        pe(lambda e, k=k, pb=pb: e.transpose(out=pb[:, (k % 8) * 128:(k % 8 + 1) * 128], in_=catb[:, k * 128:(k + 1) * 128], identity=ident_bf[:, :]),
                           [t_catb, t_ident], [pstok[7 if k < 8 else 6]])
                    act(lambda e: e.activation(out=catT[:, 0:8, :].rearrange("p k t -> p (k t)"), in_=psT[:, :], func=AF.Copy), [pstok[7]], [t_catT])
                    act(lambda e: e.activation(out=catT[:, 8:16, :].rearrange("p k t -> p (k t)"), in_=ps6b[:, :], func=AF.Copy), [pstok[6]], [t_catT])
                    yield
                    for j in range(2):
                        for k in range(16):
                            pe(lambda e, j=j, k=k: e.matmul(out=ps[:, j, :], lhsT=catT[:, k, :], rhs=wo[:, k, j * 512:(j + 1) * 512],
                                                            start=(k == 0), stop=(k == 15)), [t_catT, t_wo], [pstok[j]])
                    yield
                    dve(lambda e, par=par: e.tensor_tensor(out=x1[par][:, :], in0=ps[:, 0:2, :].rearrange("p a b -> p (a b)"), in1=xt[:, :], op=ALU.add),
                        [pstok[0], pstok[1], t_xt], [t_x1[par]])
                    P.op("pool", lambda e, par=par, r0=r0: e.dma_start(out=x1_d[r0:r0 + 128, :], in_=x1[par][:, :]),
                         reads=[t_x1[par]], writes=[], dsem=ds_x1[par])
                    yield
                    act(lambda e, par=par: e.activation(out=junk[:, :], in_=x1[par][:, :], func=AF.Square, accum_out=st[:, 0:1]), [t_x1[par]], [t_junk, t_st])
                    dve(lambda e: e.tensor_scalar(out=st[:, 1:2], in0=st[:, 0:1], scalar1=1.0 / D, scalar2=EPS, op0=ALU.mult, op1=ALU.add), [t_st], [t_st])
                    rsqrt(st[:, 2:3], st[:, 1:2], [t_st], [t_st])
                    act(lambda e, par=par: e.activation(out=h2b[:, :], in_=x1[par][:, :], func=AF.Copy, scale=st[:, 2:3]), [t_x1[par], t_st], [t_h2b])
                    yield
                    for k in range(8):
                        pe(lambda e, k=k: e.transpose(out=psT[:, k * 128:(k + 1) * 128], in_=h2b[:, k * 128:(k + 1) * 128], identity=ident_bf[:, :]),
                           [t_h2b, t_ident], [pstok[7]])
                    act(lambda e, par=par: e.activation(out=h2T[par][:, :, :].rearrange("p k t -> p (k t)"), in_=psT[:, :], func=AF.Copy), [pstok[7]], [t_h2T[par]])
                    P.op("pool", lambda e, par=par, ci=ci: e.dma_start(out=h2T_d[ci, :, :], in_=h2T[par][:, :, :].rearrange("p k t -> p (k t)")),
                         reads=[t_h2T[par]], writes=[], dsem=ds_h2T[par])
                    yield
                    for j in range(4):
                        for k in range(8):
                            pe(lambda e, j=j, k=k, par=par: e.matmul(out=ps[:, j, :], lhsT=h2T[par][:, k, :], rhs=Wc[:, k, j * 512:(j + 1) * 512],
                                                                     start=(k == 0), stop=(k == 7)), [t_h2T[par], t_Wc], [pstok[j]])
                    yield
                    act(lambda e, par=par: e.activation(out=Ssb2[par][:, :, :].rearrange("p a b -> p (a b)"), in_=ps[:, 0:4, :].rearrange("p a b -> p (a b)"), func=AF.Copy),
                        [pstok[0], pstok[1], pstok[2], pstok[3]], [t_Ssb2[par]])
                    yield

                def back(ci):
                    r0 = ci * 128
                    par = ci % 2
                    for j in range(16):
                        dve(lambda e, j=j, par=par: e.max(out=vv[:, j, 0:8], in_=Ssb2[par][:, j, :]), [t_Ssb2[par]], [tg_vv[j]])
                    yield
                    for j in range(16):
                        dve(lambda e, j=j, par=par: e.match_replace(out=Swk[:, j, :], in_to_replace=vv[:, j, 0:8], in_values=Ssb2[par][:, j, :], imm_value=-1e30),
                            [t_Ssb2[par], tg_vv[j]], [tg_Swk[j]])
                    yield
                    for j in range(16):
                        dve(lambda e, j=j, par=par: e.max_index(out=idx[:, j, 0:8], in_max=vv[:, j, 0:8], in_values=Ssb2[par][:, j, :]), [t_Ssb2[par], tg_vv[j]], [tg_idx[j]])
                    yield
                    for j in range(16):
                        dve(lambda e, j=j, par=par: e.max(out=vv[:, j, 8:16], in_=Swk[:, j, :]), [tg_Swk[j]], [tg_vv2[j]])
                    yield
                    for j in range(16):
                        dve(lambda e, j=j, par=par: e.max_index(out=idx[:, j, 8:16], in_max=vv[:, j, 8:16], in_values=Swk[:, j, :]), [tg_Swk[j], tg_vv2[j]], [tg_idx2[j]])
                    yield
                    dve(lambda e: e.tensor_copy(out=idxf[:, :, :], in_=idx[:, :, :]), tg_idx + tg_idx2, [t_idxf])
                    v4 = vv[:, :, :].rearrange("p (h two) k -> p h two k", two=2)
                    dve(lambda e, v4=v4: e.tensor_tensor(out=cand[:, :, 0:64].rearrange("p h (i j) -> p h i j", i=4),
                                                         in0=v4[:, :, 0, 0:4].unsqueeze(3).to_broadcast([128, 8, 4, 16]),
                                                         in1=v4[:, :, 1, :].unsqueeze(2).to_broadcast([128, 8, 4, 16]), op=ALU.add), tg_vv + tg_vv2, [t_cand])
                    dve(lambda e, v4=v4: e.tensor_tensor(out=cand[:, :, 64:112].rearrange("p h (i j) -> p h i j", i=12),
                                                         in0=v4[:, :, 0, 4:16].unsqueeze(3).to_broadcast([128, 8, 12, 4]),
                                                         in1=v4[:, :, 1, 0:4].unsqueeze(2).to_broadcast([128, 8, 12, 4]), op=ALU.add), tg_vv + tg_vv2, [t_cand])
                    cfls = [cand[:, h, :] for h in range(8)]
                    for h in range(8):
                        dve(lambda e, h=h: e.max(out=c16[:, h, 0:8], in_=cfls[h]), [t_cand], [th_c16[h]])
                    yield
                    for h in range(8):
                        dve(lambda e, h=h: e.match_replace(out=candw[:, h, :], in_to_replace=c16[:, h, 0:8], in_values=cfls[h], imm_value=-1e30),
                            [t_cand, th_c16[h]], [th_candw[h]])
                    yield
                    for h in range(8):
                        dve(lambda e, h=h: e.max_index(out=cpos[:, h, 0:8], in_max=c16[:, h, 0:8], in_values=cfls[h]), [t_cand, th_c16[h]], [th_cpos[h]])
                    yield
                    for h in range(8):
                        dve(lambda e, h=h: e.max(out=c16[:, h, 8:16], in_=candw[:, h, :]), [th_candw[h]], [th_c16b[h]])
                    yield
                    for h in range(8):
                        dve(lambda e, h=h: e.max_index(out=cpos[:, h, 8:16], in_max=c16[:, h, 8:16], in_values=candw[:, h, :]), [th_candw[h], th_c16b[h]], [th_cposb[h]])
                    yield
                    dve(lambda e: e.tensor_tensor(out=gt[:, :, :], in0=c16[:, :, :], in1=c16[:, :, 0:1].to_broadcast([128, 8, 16]), op=ALU.subtract),
                        th_c16 + th_c16b, [t_gt])
                    act(lambda e: e.activation(out=gt[:, :, :].rearrange("p a b -> p (a b)"), in_=gt[:, :, :].rearrange("p a b -> p (a b)"), func=AF.Exp),
                        [t_gt], [t_gt])
                    dve(lambda e: e.reduce_sum(out=z8[:, :], in_=gt[:, :, :], axis=AX.X), [t_gt], [t_z8])
                    dve(lambda e: e.reciprocal(out=z8[:, :], in_=z8[:, :]), [t_z8], [t_z8])
                    dve(lambda e: e.tensor_tensor(out=gt[:, :, :], in0=gt[:, :, :], in1=z8[:, :].unsqueeze(2).to_broadcast([128, 8, 16]), op=ALU.mult),
                        [t_gt, t_z8], [t_gt])
                    yield
                    dve(lambda e: e.tensor_single_scalar(out=cpi[:, 0, :, :], in_=cpos[:, :, :], scalar=4, op=ALU.logical_shift_right), th_cpos + th_cposb, [t_cpi])
                    dve(lambda e: e.tensor_single_scalar(out=cpi[:, 1, :, :], in_=cpos[:, :, :], scalar=15, op=ALU.bitwise_and), th_cpos + th_cposb, [t_cpi])
                    dve(lambda e: e.tensor_single_scalar(out=cpi[:, 2, :, :], in_=cpos[:, :, :], scalar=2, op=ALU.logical_shift_right), th_cpos + th_cposb, [t_cpi])
                    dve(lambda e: e.tensor_single_scalar(out=cpi[:, 3, :, :], in_=cpos[:, :, :], scalar=3, op=ALU.bitwise_and), th_cpos + th_cposb, [t_cpi])
                    dve(lambda e: e.tensor_copy(out=cpf4[:, :, :, :], in_=cpi[:, :, :, :]), [t_cpi], [t_cpf4])
                    dve(lambda e: e.tensor_single_scalar(out=cpt[:, 0, :, :], in_=cpf4[:, 0, :, :], scalar=4.0, op=ALU.is_ge), [t_cpf4], [t_cpt])
                    dve(lambda e: e.scalar_tensor_tensor(out=cpt[:, 1, :, :], in0=cpf4[:, 2, :, :], scalar=-12.0, in1=cpf4[:, 0, :, :],
                                                         op0=ALU.add, op1=ALU.subtract), [t_cpf4], [t_cpt])
                    dve(lambda e: e.tensor_tensor(out=cpt[:, 2, :, :], in0=cpf4[:, 3, :, :], in1=cpf4[:, 1, :, :], op=ALU.subtract), [t_cpf4], [t_cpt])
                    dve(lambda e: e.tensor_tensor(out=cpt[:, 1:3, :, :], in0=cpt[:, 1:3, :, :],
                                                  in1=cpt[:, 0:1, :, :].to_broadcast([128, 2, 8, 16]), op=ALU.mult), [t_cpt], [t_cpt])
                    dve(lambda e: e.tensor_tensor(out=cpf[:, :, :, :], in0=cpt[:, 1:3, :, :], in1=cpf4[:, 0:2, :, :], op=ALU.add), [t_cpt, t_cpf4], [t_cpf])
                    i4 = idxf[:, :, :].rearrange("p (h two) k -> p h two k", two=2)
                    yield
                    for w in range(2):
                        dve(lambda e, w=w: e.tensor_tensor(out=ohs[w][:, :, :, :], in0=cpf[:, w, :, :].unsqueeze(3).to_broadcast([128, 8, 16, 16]),
                                                           in1=iota16.unsqueeze(1).unsqueeze(1).to_broadcast([128, 8, 16, 16]), op=ALU.is_equal),
                            [t_cpf, t_cst], [t_ohs[w]])
                        dve(lambda e, w=w, i4=i4: e.tensor_tensor(out=ohs[w][:, :, :, :], in0=ohs[w][:, :, :, :],
                                                                  in1=i4[:, :, w, :].unsqueeze(2).to_broadcast([128, 8, 16, 16]), op=ALU.mult),
                            [t_ohs[w], t_idxf], [t_ohs[w]])
                        dve(lambda e, w=w: e.reduce_sum(out=e12[:, w, :, :], in_=ohs[w][:, :, :, :], axis=AX.X), [t_ohs[w]], [t_e12])
                    yield
                    dve(lambda e: e.tensor_copy(out=rtm[:, 0:2, :], in_=e12[:, :, :, :].rearrange("p w h k -> p w (h k)")), [t_e12], [t_rtm])
                    dve(lambda e: e.tensor_copy(out=rtm[:, 2, :], in_=gt[:, :, :].rearrange("p h k -> p (h k)")), [t_gt], [t_rtm])
                    for w in range(3):
                        pe(lambda e, w=w: e.transpose(out=ps4b[:, w * 128:(w + 1) * 128], in_=rtm[:, w, :], identity=ident_bf[:, :]),
                           [t_rtm, t_ident], [pstok[4]])
                    act(lambda e, par=par: e.activation(out=rt[par][:, :, :].rearrange("p a b -> p (a b)"), in_=ps4b[:, 0:384], func=AF.Copy),
                        [pstok[4]], [t_rt[par]])
                    P.op("pool", lambda e, par=par, ci=ci: e.dma_start(out=rt_d[ci, :, :], in_=rt[par][:, :, :].rearrange("p a b -> p (a b)")),
                         reads=[t_rt[par]], writes=[], dsem=ds_rt[par])


                    yield

                def interleave(g1, g2):
                    gens = [g for g in (g1, g2) if g is not None]
                    while gens:
                        for g in list(gens):
                            try:
                                next(g)
                            except StopIteration:
                                gens.remove(g)

                interleave(front(0), None)
                for ci in range(NT):
                    interleave(back(ci), front(ci + 1) if ci + 1 < NT else None)

        def _sweep4():
            P.barrier()
            dve = lambda fn, r=(), w=(): P.op("dve", fn, r, w)
            act = lambda fn, r=(), w=(): P.op("act", fn, r, w)
            pe = lambda fn, r=(), w=(): P.op("pe", fn, r, w)
            pool = lambda fn, r=(), w=(): P.op("pool", fn, r, w)
            with contextlib.ExitStack() as s4:
                def sb4(name, shape, dt=F32):
                    return s4.enter_context(nc.sbuf_tensor(name, list(shape), dt))
                NS = 256
                fgb = sb4("fgb", [128, 1024]); t_fgb = Tok(); ds_s4b = P.dsem(group=True)
                P.op("sp", lambda e: e.dma_start(out=fgb[:, :], in_=fg_d.partition_broadcast(128)), writes=[t_fgb], dsem=ds_s4b)
                iob = sb4("iob", [128, 128], BF16); t_iob = Tok()
                dve(lambda e: e.tensor_copy(out=iob[:, :], in_=cf("iota")), [t_cst], [t_iob])
                GT = [sb4("GT%d" % i, [128, 128, NS], BF16) for i in range(2)]; t_GT = [Tok(), Tok()]
                h2s = [sb4("h2s%d" % i, [128, 8, NS], BF16) for i in range(2)]; t_h2s = [Tok(), Tok()]; ds_h2s = [P.dsem(), P.dsem()]
                rts = [sb4("rts_sb%d" % i, [128, 2, 3, 128], BF16) for i in range(2)]; t_rts = [Tok(), Tok()]; ds_rts = [P.dsem(), P.dsem()]
                NR = 8
                XA = sb4("XA", [128, NR, 128], BF16); XB = sb4("XB", [128, NR, 128], BF16)
                t_XA = [Tok() for _ in range(NR)]; t_XB = [Tok() for _ in range(NR)]
                Ug = [sb4("Ug%d" % i, [128, 8, 512], BF16) for i in range(2)]; t_Ug = [Tok(), Tok()]; ds_Ug = [P.dsem(), P.dsem()]
                Vg = [sb4("Vg%d" % i, [128, 4, 1024], BF16) for i in range(2)]; t_Vg = [Tok(), Tok()]; ds_Vg = [P.dsem(), P.dsem()]
                aa = [sb4("aa%d" % i, [128, NS], BF16) for i in range(2)]; t_aa = [Tok(), Tok()]
                a2 = [sb4("a2%d" % i, [128, NS], BF16) for i in range(2)]; t_a2 = [Tok(), Tok()]
                x1t = [sb4("x1t%d" % i, [128, 1024]) for i in range(2)]; t_x1t = [Tok(), Tok()]; ds_x1t = [P.dsem(), P.dsem()]
                st = sb4("st_4", [128, 16]); t_st = Tok()
                ot = [sb4("ot%d" % i, [128, 1024]) for i in range(2)]; t_ot = [Tok(), Tok()]; ds_ot = [P.dsem(), P.dsem()]
                nslab = NT // 2
                grp_ctr = [0]

                pending = []

                def gbuild(sl, ta, tb, defer=False):
                    q = sl % 2
                    if ta == 0:
                        for c in range(2):
                            ci = sl * 2 + c
                            P.op("sp", lambda e, ci=ci, c=c, q=q: e.dma_start(out=h2s[q][:, :, c * 128:(c + 1) * 128],
                                                                              in_=h2T_d[ci, :, :].rearrange("p (k t) -> p k t", k=8)),
                                 writes=[t_h2s[q]], dsem=ds_h2s[q])
                            P.op("sp", lambda e, ci=ci, c=c, q=q: e.dma_start(out=rts[q][:, c, :, :], in_=rt_d[ci, :, :].rearrange("p (w t) -> p w t", w=3)),
                                 writes=[t_rts[q]], dsem=ds_rts[q])
                    for t in range(ta, tb):
                        c, tt = t // 128, t % 128
                        slot = t % NR
                        gb = 6 + (t // 4) % 2
                        dve(lambda e, c=c, tt=tt, slot=slot, q=q: e.tensor_scalar(out=XA[:, slot, :], in0=iob[:, :], scalar1=rts[q][:, c, 0, tt:tt + 1],
                                                                                  scalar2=rts[q][:, c, 2, tt:tt + 1], op0=ALU.is_equal, op1=ALU.mult),
                            [t_iob, t_rts[q]], [t_XA[slot]])
                        dve(lambda e, c=c, tt=tt, slot=slot, q=q: e.tensor_scalar(out=XB[:, slot, :], in0=iob[:, :], scalar1=rts[q][:, c, 1, tt:tt + 1],
                                                                                  scalar2=None, op0=ALU.is_equal),
                            [t_iob, t_rts[q]], [t_XB[slot]])
                        pe(lambda e, slot=slot, gb=gb, t=t: e.matmul(out=ps[:, gb, (t % 4) * 128:(t % 4 + 1) * 128], lhsT=XB[:, slot, :], rhs=XA[:, slot, :],
                                                                     start=True, stop=True),
                           [t_XA[slot], t_XB[slot]], [pstok[gb]])
                        if t % 4 == 3:
                            t0 = t - 3

                            def evac(gb=gb, t0=t0, q=q):
                                act(lambda e: e.activation(out=GT[q][:, :, t0:t0 + 4], in_=ps[:, gb, :].rearrange("p (t e) -> p e t", t=4), func=AF.Copy),
                                    [pstok[gb]], [t_GT[q]])
                            if defer:
                                pending.append(evac)
                            else:
                                evac()

                def load_grp(grp):
                    ub = grp_ctr[0] % 2
                    grp_ctr[0] += 1
                    P.op("sp", lambda e, grp=grp, ub=ub: e.dma_start(out=Ug[ub][:, :, :], in_=UTb_d[grp, :, :, :]), writes=[t_Ug[ub]], dsem=ds_Ug[ub])
                    P.op("sp", lambda e, grp=grp, ub=ub: e.dma_start(out=Vg[ub][:, :, :], in_=Vb_d[grp, :, :, :]), writes=[t_Vg[ub]], dsem=ds_Vg[ub])
                    return ub

                gbuild(0, 0, NS)
                for sl in range(nslab):
                    q = sl % 2
                    ubs = {}

                    def scores(eb, q=q, ubs=ubs):
                        grp, b = eb // 4, eb % 4
                        if b == 0:
                            ubs[grp] = load_grp(grp)
                        ub = ubs[grp]
                        sb_ = 4 + eb % 2
                        for k in range(8):
                            pe(lambda e, k=k, b=b, ub=ub, sb_=sb_, q=q: e.matmul(out=ps[:, sb_, 0:NS], lhsT=Ug[ub][:, k, b * 128:(b + 1) * 128], rhs=h2s[q][:, k, :],
                                                                                 start=(k == 0), stop=(k == 7)),
                               [t_Ug[ub], t_h2s[q]], [pstok[sb_]])

                    scores(0)
                    for eb in range(128):
                        grp, b = eb // 4, eb % 4
                        ub = ubs[grp]
                        sb_ = 4 + eb % 2
                        ab = eb % 2
                        if eb + 1 < 128:
                            scores(eb + 1)
                        act(lambda e, sb_=sb_, ab=ab: e.activation(out=aa[ab][:, :], in_=ps[:, sb_, 0:NS], func=AF.Gelu), [pstok[sb_]], [t_aa[ab]])
                        while pending:
                            pending.pop(0)()
                        dve(lambda e, ab=ab, eb=eb, q=q: e.tensor_tensor(out=a2[ab][:, :], in0=aa[ab][:, :], in1=GT[q][:, eb, :], op=ALU.mult),
                            [t_aa[ab], t_GT[q]], [t_a2[ab]])
                        for i in range(2):
                            for j in range(2):
                                pe(lambda e, i=i, j=j, ab=ab, b=b, ub=ub, eb=eb: e.matmul(out=ps[:, i * 2 + j, :], lhsT=a2[ab][:, i * 128:(i + 1) * 128],
                                                                                         rhs=Vg[ub][:, b, j * 512:(j + 1) * 512],
                                                                                         start=(eb == 0), stop=(eb == 127)),
                                   [t_a2[ab], t_Vg[ub]], [pstok[i * 2 + j]])
                        if sl + 1 < nslab:
                            gbuild(sl + 1, 2 * eb, 2 * eb + 2, defer=True)
                    while pending:
                        pending.pop(0)()
                    for i in range(2):
                        ci = sl * 2 + i
                        r0 = ci * 128
                        P.op("sp", lambda e, i=i, r0=r0: e.dma_start(out=x1t[i][:, :], in_=x1_d[r0:r0 + 128, :]), writes=[t_x1t[i]], dsem=ds_x1t[i])
                        dve(lambda e, i=i: e.tensor_tensor(out=x1t[i][:, :], in0=ps[:, 2 * i:2 * i + 2, :].rearrange("p a b -> p (a b)"), in1=x1t[i][:, :], op=ALU.add),
                            [pstok[2 * i], pstok[2 * i + 1], t_x1t[i]], [t_x1t[i]])
                        act(lambda e, i=i: e.activation(out=ot[i][:, :], in_=x1t[i][:, :], func=AF.Square, accum_out=st[:, 0:1]), [t_x1t[i]], [t_ot[i], t_st])
                        dve(lambda e: e.tensor_scalar(out=st[:, 1:2], in0=st[:, 0:1], scalar1=1.0 / D, scalar2=EPS, op0=ALU.mult, op1=ALU.add), [t_st], [t_st])
                        rsqrt(st[:, 2:3], st[:, 1:2], [t_st], [t_st])
                        dve(lambda e, i=i: e.scalar_tensor_tensor(out=ot[i][:, :], in0=x1t[i][:, :], scalar=st[:, 2:3], in1=fgb[:, :], op0=ALU.mult, op1=ALU.mult),
                            [t_x1t[i], t_st, t_fgb], [t_ot[i]])
                        finals.append(P.op("pool", lambda e, i=i, r0=r0: e.dma_start(out=out_d[r0:r0 + 128, :], in_=ot[i][:, :]),
                                           reads=[t_ot[i]], writes=[], dsem=ds_ot[i]))

        for _k, _f in ((1, _sweep1), (2, _sweep2), (3, _sweep3), (4, _sweep4)):
            if _k in sweeps:
                _f()
        P.emit(final_waits=finals)
    nc.input_names = in_names
    return nc


def prep_shared(inp):
    hc = host_consts()
    f = lambda a: np.ascontiguousarray(np.asarray(a, dtype=np.float32))
    sh = {
        "w_in": f(inp["w_in"][0]),
        "norm1_gc": f(inp["norm1_g"][0].reshape(8, 128).T),
        "gmlp_ln_g": f(inp["gmlp_ln_g"][0]),
        "gmlp_ln_b": f(inp["gmlp_ln_b"][0]),
        "gmlp_wsT": f(inp["gmlp_ws"][0].transpose(2, 0, 1)),
        "gmlp_bsT": f(inp["gmlp_bs"][0].T),
        "consts": f(np.concatenate([hc[n] for n in CONST_NAMES], axis=1)),
        "conv_w": f(inp["conv_w"][0].reshape(-1)),
        "conv_b": f(inp["conv_b"][0].reshape(1, -1)),
        "dt_bias": f(inp["dt_bias"][0]),
        "a_log": f(inp["a_log"][0]),
        "d_skip": f(inp["d_skip"][0]),
        "w_out": f(inp["w_out"][0]),
        "gcat": f(np.concatenate([inp["gmlp_out_g"][0], inp["ssd_norm_g"][0]]).reshape(16, 128).T),
        "w_queryT": f(inp["w_query"][0].T),
        "sub_keysT": f(np.stack([inp["sub_keys1"][0].T, inp["sub_keys2"][0].T], axis=1)),
        "norm2_gc": f(inp["norm2_g"][0].reshape(8, 128).T),
    }
    return sh


def prep_experts(inp):
    f = lambda a: np.ascontiguousarray(np.asarray(a, dtype=np.float32))
    return {
        "expert_uT": f(np.asarray(inp["expert_u"][0]).T),
        "expert_v": f(inp["expert_v"][0]),
        "final_g": f(inp["final_g"]),
    }


_NC_CACHE = {}


def kernel(**inputs):
    x = np.asarray(inputs["x"], dtype=np.float32)
    B, L, _ = x.shape
    nseq = B // NCORES
    shared = prep_shared(inputs)
    shared.update(prep_experts(inputs))
    key = (nseq, L)
    if key not in _NC_CACHE:
        _NC_CACHE[key] = build(NSEQ=nseq, L=L)
    nc = _NC_CACHE[key]
    in_maps = []
    for c in range(NCORES):
        m = dict(shared)
        m["x"] = np.ascontiguousarray(x[c * nseq:(c + 1) * nseq].reshape(nseq * L, D))
        in_maps.append(m)
    res = run_bass_kernel_spmd(nc, in_maps, core_ids=list(range(NCORES)))
    outs = [np.asarray(res.results[c]["out"], dtype=np.float32).reshape(nseq, L, D) for c in range(NCORES)]
    return np.concatenate(outs, axis=0)
```
